# Optimizing a Trainium2 kernel written in Bass

```python
import math
import jax
import jax.numpy as jnp
from jax import lax
import numpy as np

D_MODEL = 2048
BATCH = 1
SEQ = 8192
DEPTH = 2

F32 = jnp.float32
CHUNK = 64
Q_BLOCK = 128
FOX_HEADS = 6
FOX_HD = 128
FOX_W = FOX_HEADS * FOX_HD
LRU_W = 768
LRU_BLOCKS = 6
LRU_BW = LRU_W // LRU_BLOCKS
CONV_W = 4
LRU_C = 8.0
DIFF_HEADS = 4
DIFF_HD = 64
DIFF_VD = 2 * DIFF_HD
DIFF_W = DIFF_HEADS * DIFF_VD
ROT_DIM = DIFF_HD // 4
ROPE_THETA = 500000.0
D_MIX = FOX_W + LRU_W + DIFF_W
N_IN = 3 * FOX_W + FOX_HEADS + 2 * LRU_W + 3 * DIFF_W
N_GROUPS = 4
EXPERTS_PER_GROUP = 8
TOP_K = 2
D_FF_EXPERT = 512
ALPHA = (2.0 * DEPTH) ** 0.25
BETA = (8.0 * DEPTH) ** -0.25
LN_EPS = 1e-5

kernel_name = 'hybrid_fox_rglru_diffattn_hmoe_deepnorm'


def _layer_norm(x, g, b):
    xf = x.astype(F32)
    mu = jnp.mean(xf, -1, keepdims=True)
    xc = xf - mu
    var = jnp.mean(xc * xc, -1, keepdims=True)
    return (xc * lax.rsqrt(var + LN_EPS) * g.astype(F32) + b.astype(F32)).astype(x.dtype)


def _rms_norm(x, g):
    xf = x.astype(F32)
    return (xf * lax.rsqrt(jnp.mean(xf * xf, -1, keepdims=True) + LN_EPS) * g.astype(F32)).astype(x.dtype)


def _rope_tables(positions):
    half = ROT_DIM // 2
    inv_freq = ROPE_THETA ** (-jnp.arange(half, dtype=F32) * 2.0 / ROT_DIM)
    ang = positions.astype(F32)[..., None] * inv_freq
    return jnp.cos(ang)[:, :, None, :], jnp.sin(ang)[:, :, None, :]


def _apply_partial_rope(t, cos, sin):
    half = ROT_DIM // 2
    t1 = t[..., :half].astype(F32)
    t2 = t[..., half:ROT_DIM].astype(F32)
    rot = jnp.concatenate([t1 * cos - t2 * sin, t2 * cos + t1 * sin], -1).astype(t.dtype)
    return jnp.concatenate([rot, t[..., ROT_DIM:]], -1)


def _to_blocks(t):
    b, s = t.shape[:2]
    return jnp.moveaxis(t.reshape((b, s // Q_BLOCK, Q_BLOCK) + t.shape[2:]), 1, 0)


def _from_blocks(t):
    nb, b, q = t.shape[:3]
    return jnp.moveaxis(t, 0, 1).reshape((b, nb * q) + t.shape[3:])


def _forgetting_attention(q, k, v, log_f):
    s_len = q.shape[1]
    cum = jnp.moveaxis(jnp.cumsum(log_f, axis=1), -1, 1)
    k_pos = jnp.arange(s_len)
    q_pos = k_pos.reshape(-1, Q_BLOCK)
    cum_q = jnp.moveaxis(cum.reshape(cum.shape[0], cum.shape[1], -1, Q_BLOCK), 2, 0)
    scale = FOX_HD ** -0.5

    def block(args):
        qb, cq, pq = args
        logits = jnp.einsum('bqhd,bkhd->bhqk', qb, k, preferred_element_type=F32) * scale
        logits = logits + cq[..., None] - cum[:, :, None, :]
        logits = jnp.where(k_pos[None, :] <= pq[:, None], logits, -jnp.inf)
        p = jax.nn.softmax(logits, axis=-1)
        return jnp.einsum('bhqk,bkhd->bqhd', p.astype(v.dtype), v)

    return _from_blocks(lax.map(block, (_to_blocks(q), cum_q, q_pos)))


def _differential_attention(q, k, v, lam):
    s_len = q.shape[1]
    chunk = jnp.arange(s_len) // CHUNK
    chunk_q = chunk.reshape(-1, Q_BLOCK)
    scale = DIFF_HD ** -0.5

    def block(args):
        qb, cq = args
        logits = jnp.einsum('bqhmd,bkhmd->bhmqk', qb, k, preferred_element_type=F32) * scale
        logits = jnp.where(chunk[None, :] <= cq[:, None], logits, -jnp.inf)
        p = jax.nn.softmax(logits, axis=-1)
        w = p[:, :, 0] - lam * p[:, :, 1]
        return jnp.einsum('bhqk,bkhd->bqhd', w.astype(v.dtype), v)

    return _from_blocks(lax.map(block, (_to_blocks(q), chunk_q)))


def _causal_depthwise_conv(x, w, b):
    y = lax.conv_general_dilated(x, w[:, None, :], (1,), [(CONV_W - 1, 0)],
                                 dimension_numbers=('NWC', 'WIO', 'NWC'),
                                 feature_group_count=x.shape[-1])
    return y + b


def _rg_lru(x, w_a, b_a, w_i, b_i, lam):
    bsz, s_len, _ = x.shape
    xb = x.reshape(bsz, s_len, LRU_BLOCKS, LRU_BW)
    r = jax.nn.sigmoid((jnp.einsum('bsnc,ncd->bsnd', xb, w_a).reshape(bsz, s_len, LRU_W) + b_a).astype(F32))
    i = jax.nn.sigmoid((jnp.einsum('bsnc,ncd->bsnd', xb, w_i).reshape(bsz, s_len, LRU_W) + b_i).astype(F32))
    log_a = LRU_C * r * jax.nn.log_sigmoid(lam.astype(F32))
    a = jnp.exp(log_a)
    u = jnp.sqrt(-jnp.expm1(2.0 * log_a)) * (i * x.astype(F32))

    def combine(left, right):
        a_l, u_l = left
        a_r, u_r = right
        return a_l * a_r, a_r * u_l + u_r

    _, h = lax.associative_scan(combine, (a, u), axis=1)
    return h.astype(x.dtype)


def _hierarchical_moe(x, w_group, b_group, w_router, b_router, w_gate, w_up, w_down):
    bsz, s_len, d = x.shape
    t = x.reshape(-1, d)
    g_logits = jnp.matmul(t, w_group, preferred_element_type=F32) + b_group.astype(F32)
    g_prob = jax.nn.softmax(g_logits, axis=-1)
    g_onehot = jax.nn.one_hot(jnp.argmax(g_logits, axis=-1), N_GROUPS, dtype=F32)
    g_weight = jnp.sum(g_prob * g_onehot, -1, keepdims=True)
    e_logits = (jnp.matmul(t, w_router, preferred_element_type=F32) + b_router.astype(F32)
                ).reshape(-1, N_GROUPS, EXPERTS_PER_GROUP)
    e_logits = jnp.einsum('tg,tge->te', g_onehot, e_logits)
    e_prob = jax.nn.softmax(e_logits, axis=-1)
    top_w, top_i = lax.top_k(e_prob, TOP_K)
    top_w = top_w / jnp.sum(top_w, -1, keepdims=True)
    e_gate = jnp.einsum('tk,tke->te', top_w, jax.nn.one_hot(top_i, EXPERTS_PER_GROUP, dtype=F32))
    gate = g_onehot[:, :, None] * (g_weight * e_gate)[:, None, :]
    out = jnp.zeros((t.shape[0], d), F32)
    for g in range(N_GROUPS):
        hg = jnp.einsum('td,edf->tef', t, w_gate[g])
        hu = jnp.einsum('td,edf->tef', t, w_up[g])
        h = jax.nn.silu(hg) * hu * gate[:, g, :, None].astype(t.dtype)
        out = out + jnp.einsum('tef,efd->td', h, w_down[g], preferred_element_type=F32)
    return out.reshape(bsz, s_len, d).astype(x.dtype)


def _mixer(x, cos, sin, layer, w_in, b_f, conv_w, conv_b, w_a, b_a, w_i, b_i, lru_lambda,
           lam_q1, lam_k1, lam_q2, lam_k2, subln_g, w_o):
    bsz, s_len, _ = x.shape
    proj = x @ w_in
    sizes = (FOX_W, FOX_W, FOX_W, FOX_HEADS, LRU_W, LRU_W, DIFF_W, DIFF_W, DIFF_W)
    offs = [sum(sizes[:j + 1]) for j in range(len(sizes) - 1)]
    qa, ka, va, fa, xr, gr, qc, kc, vc = jnp.split(proj, offs, axis=-1)

    def heads(t, h):
        return t.reshape(bsz, s_len, h, -1)

    log_f = jax.nn.log_sigmoid(fa.astype(F32) + b_f.astype(F32))
    out_a = _forgetting_attention(heads(qa, FOX_HEADS), heads(ka, FOX_HEADS), heads(va, FOX_HEADS),
                                  log_f).reshape(bsz, s_len, FOX_W)
    xr = _causal_depthwise_conv(xr, conv_w, conv_b)
    out_b = jax.nn.gelu(gr, approximate=True) * _rg_lru(xr, w_a, b_a, w_i, b_i, lru_lambda)
    qc = _apply_partial_rope(heads(qc, 2 * DIFF_HEADS), cos, sin).reshape(bsz, s_len, DIFF_HEADS, 2, DIFF_HD)
    kc = _apply_partial_rope(heads(kc, 2 * DIFF_HEADS), cos, sin).reshape(bsz, s_len, DIFF_HEADS, 2, DIFF_HD)
    lam_init = 0.8 - 0.6 * math.exp(-0.3 * layer)
    lam = (jnp.exp(jnp.sum(lam_q1.astype(F32) * lam_k1.astype(F32)))
           - jnp.exp(jnp.sum(lam_q2.astype(F32) * lam_k2.astype(F32))) + lam_init)
    oc = _differential_attention(qc, kc, heads(vc, DIFF_HEADS), lam)
    out_c = (_rms_norm(oc, subln_g) * (1.0 - lam_init)).reshape(bsz, s_len, DIFF_W)
    return jnp.concatenate([out_a, out_b, out_c], axis=-1) @ w_o


def setup_inputs(seed: int = 0) -> dict:
    key = jax.random.key(seed)
    ks = jax.random.split(key, 32)

    def nrm(k, shape, scale):
        return jax.random.normal(k, shape, F32) * scale

    G, E = N_GROUPS, EXPERTS_PER_GROUP
    x = nrm(ks[0], (BATCH, SEQ, D_MODEL), 1.0)
    offset = jax.random.randint(ks[1], (BATCH, 1), 0, 64) * CHUNK
    positions = (offset + jnp.arange(SEQ)[None, :]).astype(jnp.int32)
    a_init = jax.random.uniform(ks[10], (DEPTH, LRU_W), F32, 0.9, 0.999) ** (1.0 / LRU_C)
    lru_lambda = jnp.log(a_init) - jnp.log1p(-a_init)
    return {
        'x': x,
        'positions': positions,
        'w_in': nrm(ks[2], (DEPTH, D_MODEL, N_IN), D_MODEL ** -0.5),
        'b_f': 1.0 + nrm(ks[3], (DEPTH, FOX_HEADS), 0.1),
        'conv_w': nrm(ks[4], (DEPTH, CONV_W, LRU_W), CONV_W ** -0.5),
        'conv_b': nrm(ks[5], (DEPTH, LRU_W), 0.02),
        'w_a': nrm(ks[6], (DEPTH, LRU_BLOCKS, LRU_BW, LRU_BW), LRU_BW ** -0.5),
        'b_a': nrm(ks[7], (DEPTH, LRU_W), 0.1),
        'w_i': nrm(ks[8], (DEPTH, LRU_BLOCKS, LRU_BW, LRU_BW), LRU_BW ** -0.5),
        'b_i': nrm(ks[9], (DEPTH, LRU_W), 0.1),
        'lru_lambda': lru_lambda,
        'lam_q1': nrm(ks[11], (DEPTH, DIFF_HD), 0.1),
        'lam_k1': nrm(ks[12], (DEPTH, DIFF_HD), 0.1),
        'lam_q2': nrm(ks[13], (DEPTH, DIFF_HD), 0.1),
        'lam_k2': nrm(ks[14], (DEPTH, DIFF_HD), 0.1),
        'subln_g': 1.0 + nrm(ks[15], (DEPTH, DIFF_VD), 0.02),
        'w_o': nrm(ks[16], (DEPTH, D_MIX, D_MODEL), BETA * D_MIX ** -0.5),
        'ln1_g': 1.0 + nrm(ks[17], (DEPTH, D_MODEL), 0.02),
        'ln1_b': nrm(ks[18], (DEPTH, D_MODEL), 0.02),
        'w_group': nrm(ks[19], (DEPTH, D_MODEL, G), D_MODEL ** -0.5),
        'b_group': nrm(ks[20], (DEPTH, G), 0.01),
        'w_router': nrm(ks[21], (DEPTH, D_MODEL, G * E), D_MODEL ** -0.5),
        'b_router': nrm(ks[22], (DEPTH, G * E), 0.01),
        'w_gate': nrm(ks[23], (DEPTH, G, E, D_MODEL, D_FF_EXPERT), D_MODEL ** -0.5),
        'w_up': nrm(ks[24], (DEPTH, G, E, D_MODEL, D_FF_EXPERT), D_MODEL ** -0.5),
        'w_down': nrm(ks[25], (DEPTH, G, E, D_FF_EXPERT, D_MODEL), BETA * D_FF_EXPERT ** -0.5),
        'ln2_g': 1.0 + nrm(ks[26], (DEPTH, D_MODEL), 0.02),
        'ln2_b': nrm(ks[27], (DEPTH, D_MODEL), 0.02),
    }


def reference(x, positions, w_in, b_f, conv_w, conv_b, w_a, b_a, w_i, b_i, lru_lambda,
              lam_q1, lam_k1, lam_q2, lam_k2, subln_g, w_o, ln1_g, ln1_b,
              w_group, b_group, w_router, b_router, w_gate, w_up, w_down, ln2_g, ln2_b):
    cos, sin = _rope_tables(positions)
    h = x
    for l in range(DEPTH):
        mix = _mixer(h, cos, sin, l, w_in[l], b_f[l], conv_w[l], conv_b[l], w_a[l], b_a[l],
                     w_i[l], b_i[l], lru_lambda[l], lam_q1[l], lam_k1[l], lam_q2[l], lam_k2[l],
                     subln_g[l], w_o[l])
        h = _layer_norm(ALPHA * h + mix, ln1_g[l], ln1_b[l])
        ffn = _hierarchical_moe(h, w_group[l], b_group[l], w_router[l], b_router[l],
                                w_gate[l], w_up[l], w_down[l])
        h = _layer_norm(ALPHA * h + ffn, ln2_g[l], ln2_b[l])
    return h
```

```python
import math
from contextlib import ExitStack
import numpy as np
import ml_dtypes
import concourse.bass as bass
import concourse.mybir as mybir
from concourse.bass_utils import run_bass_kernel_spmd

F32 = mybir.dt.float32
BF16 = mybir.dt.bfloat16
I32 = mybir.dt.int32
AF = mybir.ActivationFunctionType
ALU = mybir.AluOpType
AX = mybir.AxisListType

NC = 8
SEQ = 8192
D = 2048
TL = 1024
N_IN = 5382
ALPHA = (2.0 * 2) ** 0.25
LN_EPS = 1e-5
PI = math.pi

SAME_ENGINE_SYNC = True
NEXP = 32
CSTOP = 9
LOOK = 2


class Res:
    __slots__ = ("last_w", "readers")

    def __init__(self):
        self.last_w = None
        self.readers = []


class Tile:
    __slots__ = ("t", "r")

    def __init__(self, t):
        self.t = t
        self.r = Res()


class Op:
    __slots__ = ("eng", "fn", "deps", "sig", "is_dma", "dsem", "dtarget", "has_dep", "waits")

    def __init__(self, eng, fn, is_dma):
        self.eng = eng
        self.fn = fn
        self.deps = []
        self.sig = 0
        self.is_dma = is_dma
        self.dsem = None
        self.dtarget = 0
        self.has_dep = False
        self.waits = []


class Prog:
    ENGS = ("pe", "act", "dve", "pool", "sp")

    def __init__(self, nc, st, n_dma_sems=8):
        self.nc = nc
        self.st = st
        self.ops = []
        self.n_dma_sems = n_dma_sems
        self._n = 0

    def tile(self, shape, dtype, name=None):
        self._n += 1
        return Tile(self.st.enter_context(self.nc.sbuf_tensor("%s_%d" % (name or "t", self._n), shape, dtype)))

    def psum(self, shape, dtype=F32, name=None):
        self._n += 1
        return Tile(self.st.enter_context(self.nc.psum_tensor("%s_%d" % (name or "p", self._n), shape, dtype)))

    def emit(self, eng, fn, reads=(), writes=(), is_dma=False):
        op = Op(eng, fn, is_dma)
        deps = {}
        for r in reads:
            if r.last_w is not None:
                deps[id(r.last_w)] = r.last_w
        for w in writes:
            if w.last_w is not None:
                deps[id(w.last_w)] = w.last_w
            for rd in w.readers:
                deps[id(rd)] = rd
        op.deps = list(deps.values())
        for d in op.deps:
            d.has_dep = True
        for r in reads:
            r.readers.append(op)
        for w in writes:
            w.last_w = op
            w.readers = []
        self.ops.append(op)
        return op

    def dma(self, eng, out, in_, reads=(), writes=()):
        return self.emit(eng, lambda e: e.dma_start(out=out, in_=in_), reads, writes, is_dma=True)

    def finalize(self):
        nc, st = self.nc, self.st
        esem = {e: st.enter_context(nc.semaphore("s_" + e)) for e in self.ENGS}
        dsems = {q: [st.enter_context(nc.semaphore("d_%s%d" % (q, i))) for i in range(self.n_dma_sems)]
                 for q in ("sp", "pool")}
        dcount = {q: [0] * self.n_dma_sems for q in dsems}
        drr = {q: 0 for q in dsems}
        ecount = {e: 0 for e in self.ENGS}
        known = {e: {} for e in self.ENGS}
        per_eng = {e: [] for e in self.ENGS}
        for op in self.ops:
            waits = {}
            k = known[op.eng]

            def need(sem, val):
                key = id(sem)
                if k.get(key, 0) >= val:
                    return
                if key not in waits or waits[key][1] < val:
                    waits[key] = (sem, val)

            for d in op.deps:
                if d.is_dma:
                    need(d.dsem, d.dtarget)
                else:
                    if d.eng == op.eng and (d.eng == "pe" or not SAME_ENGINE_SYNC):
                        continue
                    need(esem[d.eng], d.sig)
            if op.is_dma:
                q = op.eng
                i = drr[q]
                drr[q] = (i + 1) % self.n_dma_sems
                sem = dsems[q][i]
                if dcount[q][i] > 0:
                    need(sem, dcount[q][i] * 16)
                dcount[q][i] += 1
                op.dsem = sem
                op.dtarget = dcount[q][i] * 16
            elif op.has_dep:
                ecount[op.eng] += 1
                op.sig = ecount[op.eng]
            op.waits = list(waits.values())
            for sem, val in op.waits:
                k[id(sem)] = val
            per_eng[op.eng].append(op)
        fin = []
        for q in dsems:
            for i, sem in enumerate(dsems[q]):
                if dcount[q][i] > 0:
                    fin.append((sem, dcount[q][i] * 16))
        for e in self.ENGS:
            if ecount[e] > 0:
                fin.append((esem[e], ecount[e]))
        block = st.enter_context(nc.Block())

        def replay(name, last=False):
            def body(e):
                for op in per_eng[name]:
                    for sem, val in op.waits:
                        e.wait_ge(sem, val)
                    inst = op.fn(e)
                    if op.is_dma:
                        inst.then_inc(op.dsem, 16)
                    elif op.sig:
                        inst.then_inc(esem[name], 1)
                if last:
                    for sem, val in fin:
                        e.wait_ge(sem, val)
            return body

        block.tensor(replay("pe"))
        block.scalar(replay("act"))
        block.vector(replay("dve"))
        block.gpsimd(replay("pool"))
        block.sync(replay("sp", True))


class Ring:
    def __init__(self, tiles):
        self.tiles = tiles
        self.i = 0

    def next(self):
        t = self.tiles[self.i % len(self.tiles)]
        self.i += 1
        return t


def din(nc, name, shape, dt=F32):
    return nc.dram_tensor(name, list(shape), dt, kind="ExternalInput").ap()


def dout(nc, name, shape, dt=F32):
    return nc.dram_tensor(name, list(shape), dt, kind="ExternalOutput").ap()


_ev = [0]


def evac_eng():
    _ev[0] += 1
    return "act" if _ev[0] % 2 else "dve"


def copy_op(P, eng, out, in_, reads, writes, scale=None):
    if eng == "act":
        if scale is None:
            P.emit("act", lambda e: e.activation(out=out, in_=in_, func=AF.Copy), reads, writes)
        else:
            P.emit("act", lambda e: e.activation(out=out, in_=in_, func=AF.Copy, scale=float(scale)), reads, writes)
    else:
        if scale is None:
            P.emit("dve", lambda e: e.tensor_copy(out=out, in_=in_), reads, writes)
        else:
            P.emit("dve", lambda e: e.tensor_scalar_mul(out=out, in0=in_, scalar1=float(scale)), reads, writes)


def transpose_to_xT(P, src_ap_fn, ident, xT, nchunks=16):
    hts = Ring([P.tile([128, nchunks * 128], F32, "ht") for _ in range(2)])
    pst = Ring([P.psum([128, 512], F32, "pst") for _ in range(2)])
    for j in range(8):
        ht = hts.next()
        P.dma("sp", ht.t[:], src_ap_fn(j), writes=[ht.r])
        for g in range(nchunks // 4):
            ps = pst.next()
            for q in range(4):
                kc = g * 4 + q
                P.emit("pe", lambda e, ps=ps, q=q, ht=ht, kc=kc: e.transpose(
                    ps.t[:, q * 128:(q + 1) * 128], ht.t[:, kc * 128:(kc + 1) * 128], ident.t[:]),
                    reads=[ht.r, ident.r], writes=[ps.r])
            copy_op(P, evac_eng(), xT.t[:, g * 4:(g + 1) * 4, j * 128:(j + 1) * 128],
                    ps.t[:].rearrange("p (a b) -> p a b", a=4), [ps.r], [xT.r])


def phase_A(nc, io):
    with ExitStack() as st:
        P = Prog(nc, st)
        ident = P.tile([128, 128], F32, "ident")
        P.dma("sp", ident.t[:], io["ident"], writes=[ident.r])
        xT = P.tile([128, 16, TL], BF16, "xT")
        transpose_to_xT(P, lambda j: io["hin"][j * 128:(j + 1) * 128, :], ident, xT)

        posi = P.tile([128, 8], I32, "posi")
        posf = P.tile([128, 8], F32, "posf")
        invf = P.tile([128, 8], F32, "invf")
        ang = P.tile([128, 8, 8], F32, "ang")
        red = P.tile([128, 8, 8], F32, "red")
        cost = P.tile([128, 8, 8], F32, "cos")
        sint = P.tile([128, 8, 8], F32, "sin")
        bfb = P.tile([128, 6], F32, "bfb")
        P.dma("sp", posi.t[:], io["pos"], writes=[posi.r])
        P.dma("sp", invf.t[:], io["invf"], writes=[invf.r])
        P.dma("sp", bfb.t[:], io["b_f"].partition_broadcast(128), writes=[bfb.r])
        P.emit("dve", lambda e: e.tensor_copy(out=posf.t[:], in_=posi.t[:]), [posi.r], [posf.r])
        P.emit("dve", lambda e: e.tensor_tensor(out=ang.t[:], in0=posf.t[:].unsqueeze(2).to_broadcast([128, 8, 8]),
                                                in1=invf.t[:].unsqueeze(1).to_broadcast([128, 8, 8]), op=ALU.mult),
               [posf.r, invf.r], [ang.r])
        angi = P.tile([128, 8, 8], I32, "angi")
        angf = P.tile([128, 8, 8], F32, "angf")
        ang2 = P.tile([128, 8, 8], F32, "ang2")
        for (shift, dst) in ((0.0, sint), (0.5 * PI, cost)):
            P.emit("dve", lambda e, shift=shift: e.tensor_scalar_add(out=ang2.t[:], in0=ang.t[:], scalar1=float(shift)), [ang.r], [ang2.r])
            P.emit("dve", lambda e: e.tensor_scalar_mul(out=red.t[:], in0=ang2.t[:], scalar1=float(1.0 / (2 * PI))), [ang2.r], [red.r])
            P.emit("dve", lambda e: e.tensor_copy(out=angi.t[:], in_=red.t[:]), [red.r], [angi.r])
            P.emit("dve", lambda e: e.tensor_copy(out=angf.t[:], in_=angi.t[:]), [angi.r], [angf.r])
            P.emit("dve", lambda e: e.scalar_tensor_tensor(out=red.t[:], in0=angf.t[:], scalar=float(-2 * PI), in1=ang2.t[:], op0=ALU.mult, op1=ALU.add),
                   [angf.r, ang2.r], [red.r])
            P.emit("dve", lambda e: e.tensor_single_scalar(out=angf.t[:], in_=red.t[:], scalar=float(PI), op=ALU.is_gt), [red.r], [angf.r])
            P.emit("dve", lambda e: e.scalar_tensor_tensor(out=red.t[:], in0=angf.t[:], scalar=float(-2 * PI), in1=red.t[:], op0=ALU.mult, op1=ALU.add),
                   [angf.r, red.r], [red.r])
            P.emit("dve", lambda e: e.tensor_single_scalar(out=angf.t[:], in_=red.t[:], scalar=float(-PI), op=ALU.is_lt), [red.r], [angf.r])
            P.emit("dve", lambda e: e.scalar_tensor_tensor(out=red.t[:], in0=angf.t[:], scalar=float(2 * PI), in1=red.t[:], op0=ALU.mult, op1=ALU.add),
                   [angf.r, red.r], [red.r])
            P.emit("act", lambda e, dst=dst: e.activation(out=dst.t[:], in_=red.t[:], func=AF.Sin), [red.r], [dst.r])

        wbufs = Ring([P.tile([128, 16, 512], BF16, "w") for _ in range(3)])
        psA = Ring([P.psum([128, 512], F32, "psA") for _ in range(3)])

        def load_w(c0, ncols):
            wb = wbufs.next()
            P.dma("pool", wb.t[:, :, 0:ncols], io["w_in"][:, c0:c0 + ncols].rearrange("(kc p) n -> p kc n", p=128),
                  writes=[wb.r])
            return wb

        otf = Ring([P.tile([128, TL], F32, "otf") for _ in range(2)])
        otb = Ring([P.tile([128, TL], BF16, "otb") for _ in range(2)])

        def feat_chunk(c0, nblk, out_ap, scale, is_bf):
            wb = load_w(c0, nblk * 128)
            for b in range(nblk):
                ot = (otb if is_bf else otf).next()
                for th in range(2):
                    ps = psA.next()
                    for kc in range(16):
                        P.emit("pe", lambda e, ps=ps, wb=wb, kc=kc, b=b, th=th: e.matmul(
                            ps.t[:], lhsT=wb.t[:, kc, b * 128:(b + 1) * 128], rhs=xT.t[:, kc, th * 512:(th + 1) * 512],
                            start=(kc == 0), stop=(kc == 15)), reads=[wb.r, xT.r], writes=[ps.r])
                    copy_op(P, evac_eng(), ot.t[:, th * 512:(th + 1) * 512], ps.t[:], [ps.r], [ot.r], scale)
                P.dma("sp", out_ap(b), ot.t[:], reads=[ot.r])

        def tok_chunk(c0, ncols, handler):
            wb = load_w(c0, ncols)
            for j in range(8):
                ps = psA.next()
                for kc in range(16):
                    P.emit("pe", lambda e, ps=ps, wb=wb, kc=kc, j=j: e.matmul(
                        ps.t[:, 0:ncols], lhsT=xT.t[:, kc, j * 128:(j + 1) * 128], rhs=wb.t[:, kc, 0:ncols],
                        start=(kc == 0), stop=(kc == 15)), reads=[wb.r, xT.r], writes=[ps.r])
                handler(j, ps)

        feat_chunk(0, 4, lambda b: io["qTf"][b * 128:(b + 1) * 128, :], 128 ** -0.5, True)
        feat_chunk(512, 2, lambda b: io["qTf"][(4 + b) * 128:(5 + b) * 128, :], 128 ** -0.5, True)
        feat_chunk(768, 4, lambda b: io["kTf"][b * 128:(b + 1) * 128, :], None, True)
        feat_chunk(1280, 2, lambda b: io["kTf"][(4 + b) * 128:(5 + b) * 128, :], None, True)
        feat_chunk(2310, 4, lambda b: io["xrT"][b * 128:(b + 1) * 128, :], None, False)
        feat_chunk(2310 + 512, 2, lambda b: io["xrT"][(4 + b) * 128:(5 + b) * 128, :], None, False)
        feat_chunk(3078, 4, lambda b: io["grT"][b * 128:(b + 1) * 128, :], None, False)
        feat_chunk(3078 + 512, 2, lambda b: io["grT"][(4 + b) * 128:(5 + b) * 128, :], None, False)

        vts = Ring([P.tile([128, 512], BF16, "vt") for _ in range(2)])

        def h_v(key, col0, ncols):
            def h(j, ps):
                vt = vts.next()
                copy_op(P, evac_eng(), vt.t[:, 0:ncols], ps.t[:, 0:ncols], [ps.r], [vt.r])
                P.dma("sp", io[key][j * 128:(j + 1) * 128, col0:col0 + ncols], vt.t[:, 0:ncols], reads=[vt.r])
            return h

        tok_chunk(1536, 512, h_v("vf", 0, 512))
        logf = P.tile([128, 8, 6], F32, "logf")
        zt = P.tile([128, 6], F32, "zt")
        h_v2 = h_v("vf", 512, 256)

        def h_vfa(j, ps):
            P.emit("dve", lambda e: e.tensor_tensor(out=zt.t[:], in0=ps.t[:, 256:262], in1=bfb.t[:], op=ALU.add),
                   [ps.r, bfb.r], [zt.r])
            P.emit("act", lambda e: e.activation(out=zt.t[:], in_=zt.t[:], func=AF.Exp, scale=-1.0), [zt.r], [zt.r])
            P.emit("act", lambda e: e.activation(out=zt.t[:], in_=zt.t[:], func=AF.Ln, bias=1.0), [zt.r], [zt.r])
            P.emit("dve", lambda e: e.tensor_scalar_mul(out=logf.t[:, j, :], in0=zt.t[:], scalar1=-1.0), [zt.r], [logf.r])
            h_v2(j, ps)

        tok_chunk(2048, 262, h_vfa)
        P.dma("sp", io["logf"].rearrange("(j p) h -> p j h", p=128), logf.t[:], reads=[logf.r])

        qk = P.tile([128, 512], F32, "qk")
        ta, tb, tc_, td = [P.tile([128, 8, 8], F32, "rt") for _ in range(4)]
        pstr = Ring([P.psum([128, 512], F32, "pstr") for _ in range(2)])

        def h_qk(key, scale):
            qTd = P.tile([128, 4, TL], BF16, "qTd")

            def h(j, ps):
                copy_op(P, "act", qk.t[:], ps.t[:], [ps.r], [qk.r], scale)
                v3 = qk.t[:].rearrange("p (s d) -> p s d", s=8)
                t1, t2 = v3[:, :, 0:8], v3[:, :, 8:16]
                cb = cost.t[:, j, :].unsqueeze(1).to_broadcast([128, 8, 8])
                sb = sint.t[:, j, :].unsqueeze(1).to_broadcast([128, 8, 8])
                rd = [qk.r, cost.r, sint.r]
                P.emit("dve", lambda e: e.tensor_tensor(out=ta.t[:], in0=t1, in1=cb, op=ALU.mult), rd, [ta.r])
                P.emit("dve", lambda e: e.tensor_tensor(out=tb.t[:], in0=t2, in1=sb, op=ALU.mult), rd, [tb.r])
                P.emit("dve", lambda e: e.tensor_tensor(out=tc_.t[:], in0=t2, in1=cb, op=ALU.mult), rd, [tc_.r])
                P.emit("dve", lambda e: e.tensor_tensor(out=td.t[:], in0=t1, in1=sb, op=ALU.mult), rd, [td.r])
                P.emit("dve", lambda e: e.tensor_tensor(out=t1, in0=ta.t[:], in1=tb.t[:], op=ALU.subtract), [ta.r, tb.r], [qk.r])
                P.emit("dve", lambda e: e.tensor_tensor(out=t2, in0=tc_.t[:], in1=td.t[:], op=ALU.add), [tc_.r, td.r], [qk.r])
                pt = pstr.next()
                for b in range(4):
                    P.emit("pe", lambda e, b=b, pt=pt: e.transpose(pt.t[:, b * 128:(b + 1) * 128], qk.t[:, b * 128:(b + 1) * 128], ident.t[:]),
                           [qk.r, ident.r], [pt.r])
                copy_op(P, evac_eng(), qTd.t[:, :, j * 128:(j + 1) * 128], pt.t[:].rearrange("p (a b) -> p a b", a=4), [pt.r], [qTd.r])
                if j == 7:
                    P.dma("sp", io[key].rearrange("(b p) t -> p b t", p=128), qTd.t[:], reads=[qTd.r])
            return h

        tok_chunk(3846, 512, h_qk("qTd", 64 ** -0.5))
        tok_chunk(4358, 512, h_qk("kTd", None))
        tok_chunk(4870, 512, h_v("vd", 0, 512))
        P.finalize()
    nc.all_engine_barrier()


def build_A():
    nc = bass.Bass("TRN2", target_bir_lowering=False)
    io = dict(
        hin=din(nc, "hin", [TL, D]), w_in=din(nc, "w_in", [D, N_IN]), b_f=din(nc, "b_f", [6]),
        pos=din(nc, "pos", [128, 8], I32), invf=din(nc, "invf", [128, 8]), ident=din(nc, "ident", [128, 128]),
        qTf=dout(nc, "qTf", [768, TL], BF16), kTf=dout(nc, "kTf", [768, TL], BF16), vf=dout(nc, "vf", [TL, 768], BF16),
        logf=dout(nc, "logf", [TL, 6]), xrT=dout(nc, "xrT", [768, TL]), grT=dout(nc, "grT", [768, TL]),
        qTd=dout(nc, "qTd", [512, TL], BF16), kTd=dout(nc, "kTd", [512, TL], BF16), vd=dout(nc, "vd", [TL, 512], BF16),
    )
    phase_A(nc, io)
    return nc


def own_rows(c):
    return np.concatenate([np.arange((8 * j + c) * 128, (8 * j + c + 1) * 128) for j in range(8)])


IDENT = np.eye(128, dtype=np.float32)
INVF = np.broadcast_to((np.float32(500000.0) ** (-np.arange(8, dtype=np.float32) * np.float32(2.0) / np.float32(16))).astype(np.float32), (128, 8)).copy()


def run(nc, in_maps):
    res = run_bass_kernel_spmd(nc, in_maps, core_ids=list(range(NC)))
    return res.results


def launch_A(h_shards, w_in_l, b_f_l, positions):
    nc = build_A()
    in_maps = []
    for c in range(NC):
        in_maps.append(dict(hin=h_shards[c], w_in=w_in_l, b_f=b_f_l,
                            pos=np.ascontiguousarray(positions[own_rows(c)].reshape(8, 128).T), invf=INVF, ident=IDENT))
    return run(nc, in_maps)


def split3(P, src, cur, back, n, npart, sign):
    outs = []
    P.emit("dve", lambda e: e.tensor_scalar_mul(out=cur.t[:], in0=src.t[:], scalar1=float(sign)), [src.r], [cur.r])
    for i in range(3):
        o = P.tile([npart, n], BF16, "s3o")
        P.emit("dve", lambda e, o=o: e.tensor_copy(out=o.t[:], in_=cur.t[:]), [cur.r], [o.r])
        if i < 2:
            P.emit("dve", lambda e, o=o: e.tensor_copy(out=back.t[:], in_=o.t[:]), [o.r], [back.r])
            P.emit("dve", lambda e: e.tensor_tensor(out=cur.t[:], in0=cur.t[:], in1=back.t[:], op=ALU.subtract), [cur.r, back.r], [cur.r])
        outs.append(o)
    return outs


def phase_B(nc, io, lam_init):
    with ExitStack() as st:
        P = Prog(nc, st)
        CH = 2048
        cw = P.tile([128, 4], F32, "cw"); cb = P.tile([128, 1], F32, "cb")
        wa32 = P.tile([128, 128], F32, "wa32"); wi32 = P.tile([128, 128], F32, "wi32")
        wa = P.tile([128, 128], BF16, "wa"); wi = P.tile([128, 128], BF16, "wi")
        ba = P.tile([128, 1], F32, "ba"); bi = P.tile([128, 1], F32, "bi"); lm = P.tile([128, 1], F32, "lm")
        one = P.tile([128, 1], F32, "one"); c8 = P.tile([128, 1], F32, "c8")
        for t_, k_ in ((cw, "conv_w"), (cb, "conv_b"), (wa32, "w_a"), (wi32, "w_i"), (ba, "b_a"), (bi, "b_i"), (lm, "lru_lam")):
            P.dma("sp", t_.t[:], io[k_], writes=[t_.r])
        P.emit("dve", lambda e: e.tensor_copy(out=wa.t[:], in_=wa32.t[:]), [wa32.r], [wa.r])
        P.emit("dve", lambda e: e.tensor_copy(out=wi.t[:], in_=wi32.t[:]), [wi32.r], [wi.r])
        P.emit("dve", lambda e: e.memset(one.t[:], 1.0), [], [one.r])
        P.emit("act", lambda e: e.activation(out=c8.t[:], in_=lm.t[:], func=AF.Exp, scale=-1.0), [lm.r], [c8.r])
        P.emit("act", lambda e: e.activation(out=c8.t[:], in_=c8.t[:], func=AF.Ln, bias=one.t[:, 0:1]), [c8.r, one.r], [c8.r])
        P.emit("dve", lambda e: e.tensor_scalar_mul(out=c8.t[:], in0=c8.t[:], scalar1=-8.0), [c8.r], [c8.r])
        xpad = P.tile([128, CH + 3], F32, "xpad"); xc = P.tile([128, CH], F32, "xc"); xcb = P.tile([128, CH], BF16, "xcb")
        rr = P.tile([128, CH], F32, "rr"); ii = P.tile([128, CH], F32, "ii"); tmp = P.tile([128, CH], F32, "tmp")
        hh = Ring([P.tile([128, CH], F32, "hh") for _ in range(2)]); gg = P.tile([128, CH], F32, "gg")
        psl = Ring([P.psum([128, 512], F32, "psl") for _ in range(2)])
        hprev = None
        for ck in range(SEQ // CH):
            t0 = ck * CH
            P.dma("sp", xpad.t[:], io["xr"][:, t0:t0 + CH + 3], writes=[xpad.r])
            P.dma("sp", gg.t[:], io["gr"][:, t0:t0 + CH], writes=[gg.r])
            P.emit("dve", lambda e: e.tensor_scalar(out=xc.t[:], in0=xpad.t[:, 0:CH], scalar1=cw.t[:, 0:1], scalar2=cb.t[:, 0:1],
                                                    op0=ALU.mult, op1=ALU.add), [xpad.r, cw.r, cb.r], [xc.r])
            for j in range(1, 4):
                P.emit("dve", lambda e, j=j: e.scalar_tensor_tensor(out=xc.t[:], in0=xpad.t[:, j:j + CH], scalar=cw.t[:, j:j + 1], in1=xc.t[:],
                                                                    op0=ALU.mult, op1=ALU.add), [xpad.r, cw.r, xc.r], [xc.r])
            P.emit("dve", lambda e: e.tensor_copy(out=xcb.t[:], in_=xc.t[:]), [xc.r], [xcb.r])
            for s in range(CH // 512):
                sl = slice(s * 512, (s + 1) * 512)
                for (wt, bt, dst) in ((wa, ba, rr), (wi, bi, ii)):
                    ps = psl.next()
                    P.emit("pe", lambda e, ps=ps, wt=wt, sl=sl: e.matmul(ps.t[:], lhsT=wt.t[:], rhs=xcb.t[:, sl], start=True, stop=True),
                           [wt.r, xcb.r], [ps.r])
                    P.emit("act", lambda e, ps=ps, bt=bt, dst=dst, sl=sl: e.activation(out=dst.t[:, sl], in_=ps.t[:], func=AF.Sigmoid, bias=bt.t[:, 0:1]),
                           [ps.r, bt.r], [dst.r])
            P.emit("act", lambda e: e.activation(out=rr.t[:], in_=rr.t[:], func=AF.Exp, scale=c8.t[:, 0:1]), [rr.r, c8.r], [rr.r])
            P.emit("dve", lambda e: e.tensor_tensor(out=tmp.t[:], in0=rr.t[:], in1=rr.t[:], op=ALU.mult), [rr.r], [tmp.r])
            P.emit("act", lambda e: e.activation(out=tmp.t[:], in_=tmp.t[:], func=AF.Sqrt, scale=-1.0, bias=one.t[:, 0:1]), [tmp.r, one.r], [tmp.r])
            P.emit("dve", lambda e: e.tensor_tensor(out=ii.t[:], in0=ii.t[:], in1=xc.t[:], op=ALU.mult), [ii.r, xc.r], [ii.r])
            P.emit("dve", lambda e: e.tensor_tensor(out=ii.t[:], in0=ii.t[:], in1=tmp.t[:], op=ALU.mult), [ii.r, tmp.r], [ii.r])
            hcur = hh.next()
            if hprev is None:
                P.emit("dve", lambda e, hcur=hcur: e.tensor_tensor_scan(out=hcur.t[:], data0=rr.t[:], data1=ii.t[:], initial=0.0,
                                                                        op0=ALU.mult, op1=ALU.add), [rr.r, ii.r], [hcur.r])
            else:
                P.emit("dve", lambda e, hcur=hcur, hp=hprev: e.tensor_tensor_scan(out=hcur.t[:], data0=rr.t[:], data1=ii.t[:],
                                                                                  initial=hp.t[:, CH - 1:CH], op0=ALU.mult, op1=ALU.add),
                       [rr.r, ii.r, hprev.r], [hcur.r])
            hprev = hcur
            P.emit("dve", lambda e: e.tensor_tensor(out=tmp.t[:], in0=gg.t[:], in1=gg.t[:], op=ALU.mult), [gg.r], [tmp.r])
            P.emit("dve", lambda e: e.tensor_scalar(out=tmp.t[:], in0=tmp.t[:], scalar1=0.044715, scalar2=1.0, op0=ALU.mult, op1=ALU.add), [tmp.r], [tmp.r])
            P.emit("dve", lambda e: e.tensor_tensor(out=tmp.t[:], in0=tmp.t[:], in1=gg.t[:], op=ALU.mult), [tmp.r, gg.r], [tmp.r])
            P.emit("act", lambda e: e.activation(out=tmp.t[:], in_=tmp.t[:], func=AF.Sigmoid, scale=2.0 * math.sqrt(2.0 / math.pi)), [tmp.r], [tmp.r])
            P.emit("dve", lambda e: e.tensor_tensor(out=tmp.t[:], in0=tmp.t[:], in1=gg.t[:], op=ALU.mult), [tmp.r, gg.r], [tmp.r])
            P.emit("dve", lambda e, hcur=hcur: e.tensor_tensor(out=tmp.t[:], in0=tmp.t[:], in1=hcur.t[:], op=ALU.mult), [tmp.r, hcur.r], [tmp.r])
            P.dma("sp", io["lru"][:, t0:t0 + CH], tmp.t[:], reads=[tmp.r])
        P.finalize()
    nc.all_engine_barrier()

    with ExitStack() as st:
        P = Prog(nc, st)
        kaug_d = nc.dram_tensor("kaug_d", [6, 6, SEQ], BF16).ap()
        qaug_d = nc.dram_tensor("qaug_d", [6, 6, TL], BF16).ap()
        lf = P.tile([6, SEQ], F32, "lf"); ones6 = P.tile([6, SEQ], BF16, "ones6"); cum = P.tile([6, SEQ], F32, "cum")
        om = P.tile([6, SEQ], F32, "om"); cown = P.tile([6, TL], F32, "cown")
        qcur = P.tile([6, TL], F32, "qcur"); qback = P.tile([6, TL], F32, "qback")
        P.dma("sp", lf.t[:], io["logfT"], writes=[lf.r])
        P.dma("sp", om.t[:], io["ownmask"], writes=[om.r])
        P.emit("dve", lambda e: e.memset(ones6.t[:], 1.0), [], [ones6.r])
        P.emit("dve", lambda e: e.tensor_tensor_scan(out=cum.t[:], data0=ones6.t[:], data1=lf.t[:], initial=0.0, op0=ALU.mult, op1=ALU.add),
               [ones6.r, lf.r], [cum.r])
        P.emit("dve", lambda e: e.tensor_tensor(out=om.t[:], in0=om.t[:], in1=cum.t[:], op=ALU.mult), [om.r, cum.r], [om.r])
        P.emit("dve", lambda e: e.tensor_reduce(out=cown.t[:].rearrange("p (j i) -> p j i", j=8),
                                                in_=om.t[:].rearrange("p (j r i) -> p j i r", j=8, r=8, i=128), axis=AX.X, op=ALU.add),
               [om.r], [cown.r])
        q3 = split3(P, cown, qcur, qback, TL, 6, 1.0)
        k3 = split3(P, cum, lf, om, SEQ, 6, -1.0)
        for i in range(3):
            P.dma("sp", kaug_d[:, i, :], ones6.t[:], reads=[ones6.r])
            P.dma("sp", kaug_d[:, 3 + i, :], k3[i].t[:], reads=[k3[i].r])
            P.dma("sp", qaug_d[:, i, :], q3[i].t[:], reads=[q3[i].r])
            P.dma("sp", qaug_d[:, 3 + i, :], ones6.t[:, 0:TL], reads=[ones6.r])
        P.finalize()
    nc.all_engine_barrier()

    with ExitStack() as st:
        P = Prog(nc, st)
        mF = P.tile([128, 8, 128], BF16, "mF"); mD = P.tile([128, 8, 128], BF16, "mD")
        P.dma("sp", mF.t[:], io["maskF"], writes=[mF.r])
        P.dma("sp", mD.t[:], io["maskD"], writes=[mD.r])
        lq = [P.tile([128, 64], F32, "lq") for _ in range(4)]
        for t_, k_ in zip(lq, ("lam_q1", "lam_k1", "lam_q2", "lam_k2")):
            P.dma("sp", t_.t[:], io[k_].partition_broadcast(128), writes=[t_.r])
        s1 = P.tile([128, 1], F32, "s1"); s2 = P.tile([128, 1], F32, "s2"); nlam = P.tile([128, 1], F32, "nlam")
        epst = P.tile([128, 1], F32, "eps")
        P.emit("dve", lambda e: e.memset(epst.t[:], LN_EPS), [], [epst.r])
        for (a_, b_, s_) in ((lq[0], lq[1], s1), (lq[2], lq[3], s2)):
            P.emit("dve", lambda e, a_=a_, b_=b_: e.tensor_tensor(out=a_.t[:], in0=a_.t[:], in1=b_.t[:], op=ALU.mult), [a_.r, b_.r], [a_.r])
            P.emit("dve", lambda e, a_=a_, s_=s_: e.reduce_sum(out=s_.t[:], in_=a_.t[:], axis=AX.X), [a_.r], [s_.r])
            P.emit("act", lambda e, s_=s_: e.activation(out=s_.t[:], in_=s_.t[:], func=AF.Exp), [s_.r], [s_.r])
        P.emit("dve", lambda e: e.tensor_tensor(out=nlam.t[:], in0=s2.t[:], in1=s1.t[:], op=ALU.subtract), [s1.r, s2.r], [nlam.r])
        P.emit("dve", lambda e: e.tensor_scalar_add(out=nlam.t[:], in0=nlam.t[:], scalar1=-float(lam_init)), [nlam.r], [nlam.r])
        gsb = P.tile([128, 128], F32, "gsb")
        P.dma("sp", gsb.t[:], io["subln_g"].partition_broadcast(128), writes=[gsb.r])
        P.emit("dve", lambda e: e.tensor_scalar_mul(out=gsb.t[:], in0=gsb.t[:], scalar1=float(1.0 - lam_init)), [gsb.r], [gsb.r])

        kTs = Ring([P.tile([128, SEQ], BF16, "kT") for _ in range(2)])
        kaugs = Ring([P.tile([6, SEQ], BF16, "kaug") for _ in range(2)])
        qaugs = Ring([P.tile([6, TL], BF16, "qaug") for _ in range(2)])
        vs = Ring([P.tile([128, 64, 130], BF16, "v") for _ in range(2)])
        qTs = Ring([P.tile([128, TL], BF16, "qT") for _ in range(2)])
        for _ in range(2):
            v_ = vs.next()
            P.emit("dve", lambda e, v_=v_: e.memset(v_.t[:, :, 128:130], 1.0), [], [v_.r])
        pss = Ring([P.psum([128, 512], F32, "pss") for _ in range(LOOK + 1)])
        pos_ = Ring([P.psum([128, 512], F32, "po") for _ in range(4)])
        pTs = Ring([P.tile([128, 512], BF16, "pT") for _ in range(4)])
        o1s = Ring([P.tile([128, 128], F32, "o1") for _ in range(3)])
        rcs = Ring([P.tile([128, 1], F32, "rc") for _ in range(4)])
        junk = P.tile([128, 128], F32, "junk")

        tasks = []
        head_loads = []

        def sel_head(kall, vall, qown, hidx, aug=None):
            kT, v_, qT = kTs.next(), vs.next(), qTs.next()
            ka = qa = None
            if aug is not None:
                ka, qa = kaugs.next(), qaugs.next()

            def load():
                P.dma("sp", kT.t[:], kall[hidx * 128:(hidx + 1) * 128, :], writes=[kT.r])
                P.dma("sp", v_.t[:, :, 0:128], vall.rearrange("(t p) d -> p t d", p=128)[:, :, hidx * 128:(hidx + 1) * 128], writes=[v_.r])
                P.dma("sp", qT.t[:], qown[hidx * 128:(hidx + 1) * 128, :], writes=[qT.r])
                if aug is not None:
                    P.dma("sp", ka.t[:], kaug_d[aug], writes=[ka.r])
                    P.dma("sp", qa.t[:], qaug_d[aug], writes=[qa.r])
            head_loads.append(load)
            return kT, v_, qT, ka, qa

        def add_unit(j, qk_mm, v_, mask, po, clamp, fin=None, head_first=None):
            nk = 8 * j + 8
            gl = list(range(0, nk, 4))
            for gi, g0 in enumerate(gl):
                tasks.append(dict(j=j, g0=g0, nk=nk, qk=qk_mm, v=v_, mask=mask, po=po, clamp=clamp,
                                  head_first=head_first if gi == 0 else None, fin=fin if gi == len(gl) - 1 else None))

        def fox_fin(po, h, j):
            def fin():
                rc = rcs.next(); o1 = o1s.next()
                P.emit("dve", lambda e: e.reciprocal(out=rc.t[:], in_=po.t[:, 128:129]), [po.r], [rc.r])
                P.emit("dve", lambda e: e.tensor_scalar_mul(out=o1.t[:], in0=po.t[:, 0:128], scalar1=rc.t[:, 0:1]), [po.r, rc.r], [o1.r])
                P.dma("sp", io["attn"][j * 128:(j + 1) * 128, h * 128:(h + 1) * 128], o1.t[:], reads=[o1.r])
            return fin

        def diff_fin(po1, po2, hd, j):
            def fin():
                rc1, rc2, ss = rcs.next(), rcs.next(), rcs.next()
                o1 = o1s.next(); oc = o1s.next()
                P.emit("dve", lambda e: e.reciprocal(out=rc1.t[:], in_=po1.t[:, 128:129]), [po1.r], [rc1.r])
                P.emit("dve", lambda e: e.tensor_scalar_mul(out=o1.t[:], in0=po1.t[:, 0:128], scalar1=rc1.t[:, 0:1]), [po1.r, rc1.r], [o1.r])
                P.emit("dve", lambda e: e.reciprocal(out=rc2.t[:], in_=po2.t[:, 128:129]), [po2.r], [rc2.r])
                P.emit("dve", lambda e: e.tensor_tensor(out=rc2.t[:], in0=rc2.t[:], in1=nlam.t[:], op=ALU.mult), [rc2.r, nlam.r], [rc2.r])
                P.emit("dve", lambda e: e.scalar_tensor_tensor(out=oc.t[:], in0=po2.t[:, 0:128], scalar=rc2.t[:, 0:1], in1=o1.t[:],
                                                               op0=ALU.mult, op1=ALU.add), [po2.r, rc2.r, o1.r], [oc.r])
                P.emit("act", lambda e: e.activation(out=junk.t[:], in_=oc.t[:], func=AF.Square, accum_out=ss.t[:, 0:1]), [oc.r], [junk.r, ss.r])
                P.emit("act", lambda e: e.activation(out=ss.t[:], in_=ss.t[:], func=AF.Sqrt, scale=1.0 / 128.0, bias=epst.t[:, 0:1]), [ss.r, epst.r], [ss.r])
                P.emit("dve", lambda e: e.reciprocal(out=ss.t[:], in_=ss.t[:]), [ss.r], [ss.r])
                P.emit("dve", lambda e: e.scalar_tensor_tensor(out=oc.t[:], in0=oc.t[:], scalar=ss.t[:, 0:1], in1=gsb.t[:], op0=ALU.mult, op1=ALU.mult),
                       [oc.r, ss.r, gsb.r], [oc.r])
                P.dma("sp", io["attn"][j * 128:(j + 1) * 128, 768 + hd * 128:768 + (hd + 1) * 128], oc.t[:], reads=[oc.r])
            return fin

        hcount = 0
        for h in range(6):
            kT, v_, qT, ka, qa = sel_head(io["kTf_all"], io["vf_all"], io["qTf"], h, aug=h)
            for j in range(8):
                po = pos_.next()

                def qk_mm(ps, g, kt, kT=kT, qT=qT, ka=ka, qa=qa, j=j):
                    P.emit("pe", lambda e: e.matmul(ps.t[:, g * 128:(g + 1) * 128], lhsT=kT.t[:, kt * 128:(kt + 1) * 128], rhs=qT.t[:, j * 128:(j + 1) * 128],
                                                    start=True, stop=False), [kT.r, qT.r], [ps.r])
                    P.emit("pe", lambda e: e.matmul(ps.t[:, g * 128:(g + 1) * 128], lhsT=ka.t[:, kt * 128:(kt + 1) * 128], rhs=qa.t[:, j * 128:(j + 1) * 128],
                                                    start=False, stop=True), [ka.r, qa.r], [ps.r])
                add_unit(j, qk_mm, v_, mF, po, True, fin=fox_fin(po, h, j), head_first=(hcount if j == 0 else None))
            hcount += 1
        for hd in range(4):
            kT, v_, qT, _, _ = sel_head(io["kTd_all"], io["vd_all"], io["qTd"], hd)
            for j in range(8):
                po1, po2 = pos_.next(), pos_.next()
                for m, po in ((0, po1), (1, po2)):
                    def qk_mm(ps, g, kt, kT=kT, qT=qT, m=m, j=j):
                        P.emit("pe", lambda e: e.matmul(ps.t[:, g * 128:(g + 1) * 128], lhsT=kT.t[m * 64:(m + 1) * 64, kt * 128:(kt + 1) * 128],
                                                        rhs=qT.t[m * 64:(m + 1) * 64, j * 128:(j + 1) * 128], start=True, stop=True), [kT.r, qT.r], [ps.r])
                    add_unit(j, qk_mm, v_, mD, po, False, fin=(diff_fin(po1, po2, hd, j) if m == 1 else None),
                             head_first=(hcount if (j == 0 and m == 0) else None))
            hcount += 1

        def emit_qk(t):
            ps = pss.next()
            t["ps"] = ps
            for g in range(4):
                t["qk"](ps, g, t["g0"] + g)

        def emit_rest(t):
            ps, j, g0, nk, po, v_, mask = t["ps"], t["j"], t["g0"], t["nk"], t["po"], t["v"], t["mask"]
            hf = t["head_first"]
            if hf is not None and hf + 1 < len(head_loads):
                head_loads[hf + 1]()
            pT = pTs.next()
            if t["clamp"] and g0 >= 8 * j:
                P.emit("dve", lambda e: e.tensor_scalar_min(out=ps.t[:], in0=ps.t[:], scalar1=80.0), [ps.r], [ps.r])
            P.emit("act", lambda e: e.activation(out=pT.t[:], in_=ps.t[:], func=AF.Exp), [ps.r], [pT.r])
            if g0 >= 8 * j:
                r0 = g0 - 8 * j
                P.emit("dve", lambda e: e.tensor_tensor(out=pT.t[:].rearrange("p (a b) -> p a b", a=4), in0=pT.t[:].rearrange("p (a b) -> p a b", a=4),
                                                        in1=mask.t[:, r0:r0 + 4, :], op=ALU.mult), [pT.r, mask.r], [pT.r])
            for g in range(4):
                kt = g0 + g
                P.emit("pe", lambda e, g=g, kt=kt: e.matmul(po.t[:, 0:130], lhsT=pT.t[:, g * 128:(g + 1) * 128], rhs=v_.t[:, kt, :],
                                                            start=(kt == 0), stop=(kt == nk - 1)), [pT.r, v_.r], [po.r])
            if t["fin"] is not None:
                t["fin"]()

        head_loads[0]()
        for i in range(min(LOOK, len(tasks))):
            emit_qk(tasks[i])
        for i, t in enumerate(tasks):
            if i + LOOK < len(tasks):
                emit_qk(tasks[i + LOOK])
            emit_rest(t)
        P.finalize()
    nc.all_engine_barrier()


def build_B(lam_init):
    nc = bass.Bass("TRN2", target_bir_lowering=False)
    io = dict(
        qTf=din(nc, "qTf", [768, TL], BF16), kTf_all=din(nc, "kTf_all", [768, SEQ], BF16), vf_all=din(nc, "vf_all", [SEQ, 768], BF16),
        logfT=din(nc, "logfT", [6, SEQ]), ownmask=din(nc, "ownmask", [6, SEQ]),
        maskF=din(nc, "maskF", [128, 8, 128], BF16), maskD=din(nc, "maskD", [128, 8, 128], BF16),
        qTd=din(nc, "qTd", [512, TL], BF16), kTd_all=din(nc, "kTd_all", [512, SEQ], BF16), vd_all=din(nc, "vd_all", [SEQ, 512], BF16),
        lam_q1=din(nc, "lam_q1", [64]), lam_k1=din(nc, "lam_k1", [64]), lam_q2=din(nc, "lam_q2", [64]), lam_k2=din(nc, "lam_k2", [64]),
        subln_g=din(nc, "subln_g", [128]),
        xr=din(nc, "xr", [128, SEQ + 3]), gr=din(nc, "gr", [128, SEQ]), conv_w=din(nc, "conv_w", [128, 4]), conv_b=din(nc, "conv_b", [128, 1]),
        w_a=din(nc, "w_a", [128, 128]), w_i=din(nc, "w_i", [128, 128]), b_a=din(nc, "b_a", [128, 1]), b_i=din(nc, "b_i", [128, 1]),
        lru_lam=din(nc, "lru_lam", [128, 1]),
        attn=dout(nc, "attn", [TL, 1280]), lru=dout(nc, "lru", [128, SEQ]),
    )
    phase_B(nc, io, lam_init)
    return nc


def global_order(parts, axis):
    full = np.concatenate(parts, axis=axis)
    perm = np.concatenate([own_rows(c) for c in range(NC)])
    inv = np.empty_like(perm)
    inv[perm] = np.arange(perm.size)
    return np.take(full, inv, axis=axis)


def core_masks(c):
    i = np.arange(128)
    tri = (i[:, None] <= i[None, :])
    blk = ((i[:, None] // 64) <= (i[None, :] // 64))
    mF = np.zeros((128, 8, 128), np.float32)
    mD = np.zeros((128, 8, 128), np.float32)
    for r in range(8):
        if r < c:
            mF[:, r, :] = 1.0
            mD[:, r, :] = 1.0
        elif r == c:
            mF[:, r, :] = tri
            mD[:, r, :] = blk
    om = np.zeros((6, SEQ), np.float32)
    om[:, own_rows(c)] = 1.0
    return mF.astype(ml_dtypes.bfloat16), mD.astype(ml_dtypes.bfloat16), om


def launch_B(resA, prm, l):
    lam_init = 0.8 - 0.6 * math.exp(-0.3 * l)
    nc = build_B(lam_init)
    kTf_all = global_order([r["kTf"] for r in resA], 1)
    vf_all = global_order([r["vf"] for r in resA], 0)
    kTd_all = global_order([r["kTd"] for r in resA], 1)
    vd_all = global_order([r["vd"] for r in resA], 0)
    logfT = np.ascontiguousarray(global_order([r["logf"] for r in resA], 0).T)
    xr_all = global_order([r["xrT"] for r in resA], 1)
    gr_all = global_order([r["grT"] for r in resA], 1)
    in_maps = []
    for c in range(NC):
        mF, mD, om = core_masks(c)
        n = c % 6
        sl = slice(n * 128, (n + 1) * 128)
        xr = np.zeros((128, SEQ + 3), np.float32)
        xr[:, 3:] = xr_all[sl]
        in_maps.append(dict(
            qTf=resA[c]["qTf"], kTf_all=kTf_all, vf_all=vf_all, logfT=logfT, ownmask=om, maskF=mF, maskD=mD,
            qTd=resA[c]["qTd"], kTd_all=kTd_all, vd_all=vd_all,
            lam_q1=prm["lam_q1"][l], lam_k1=prm["lam_k1"][l], lam_q2=prm["lam_q2"][l], lam_k2=prm["lam_k2"][l],
            subln_g=prm["subln_g"][l], xr=xr, gr=np.ascontiguousarray(gr_all[sl]),
            conv_w=np.ascontiguousarray(prm["conv_w"][l][:, sl].T), conv_b=np.ascontiguousarray(prm["conv_b"][l][sl, None]),
            w_a=np.ascontiguousarray(prm["w_a"][l][n]), w_i=np.ascontiguousarray(prm["w_i"][l][n]),
            b_a=np.ascontiguousarray(prm["b_a"][l][sl, None]), b_i=np.ascontiguousarray(prm["b_i"][l][sl, None]),
            lru_lam=np.ascontiguousarray(prm["lru_lambda"][l][sl, None])))
    return run(nc, in_maps)


def layer_norm_tiles(P, acc, gt, bt, epst, junk_ap, sm):
    for j in range(8):
        a = acc.t[:, j, :]
        mu, ss = sm.next(), sm.next()
        P.emit("dve", lambda e, a=a, mu=mu: e.reduce_sum(out=mu.t[:], in_=a, axis=AX.X), [acc.r], [mu.r])
        P.emit("dve", lambda e, mu=mu: e.tensor_scalar_mul(out=mu.t[:], in0=mu.t[:], scalar1=-1.0 / D), [mu.r], [mu.r])
        P.emit("dve", lambda e, a=a, mu=mu: e.tensor_scalar_add(out=a, in0=a, scalar1=mu.t[:, 0:1]), [acc.r, mu.r], [acc.r])
        P.emit("act", lambda e, a=a, ss=ss: e.activation(out=junk_ap[0], in_=a, func=AF.Square, accum_out=ss.t[:, 0:1]), [acc.r], [junk_ap[1], ss.r])
        P.emit("act", lambda e, ss=ss: e.activation(out=ss.t[:], in_=ss.t[:], func=AF.Sqrt, scale=1.0 / D, bias=epst.t[:, 0:1]), [ss.r, epst.r], [ss.r])
        P.emit("dve", lambda e, ss=ss: e.reciprocal(out=ss.t[:], in_=ss.t[:]), [ss.r], [ss.r])
        P.emit("dve", lambda e, a=a, ss=ss: e.scalar_tensor_tensor(out=a, in0=a, scalar=ss.t[:, 0:1], in1=gt.t[:], op0=ALU.mult, op1=ALU.mult),
               [acc.r, ss.r, gt.r], [acc.r])
        P.emit("dve", lambda e, a=a: e.tensor_tensor(out=a, in0=a, in1=bt.t[:], op=ALU.add), [acc.r, bt.r], [acc.r])


def phase_C(nc, io):
    with ExitStack() as st:
        P = Prog(nc, st)
        ident = P.tile([128, 128], F32, "ident")
        P.dma("sp", ident.t[:], io["ident"], writes=[ident.r])
        epst = P.tile([128, 1], F32, "eps")
        P.emit("dve", lambda e: e.memset(epst.t[:], LN_EPS), [], [epst.r])
        acc = P.tile([128, 8, D], F32, "acc")
        xT = P.tile([128, 16, TL], BF16, "xT")
        lng = P.tile([128, D], F32, "lng"); lnb = P.tile([128, D], F32, "lnb")
        hid = P.tile([128, 4, TL], BF16, "hid")
        junk_ap = (hid.t[:].rearrange("p a b -> p (a b)")[:, 0:D], hid.r)
        wgs = Ring([P.tile([128, 16, 256], BF16, "wg") for _ in range(2)])
        wus = Ring([P.tile([128, 16, 256], BF16, "wu") for _ in range(2)])
        wds = Ring([P.tile([128, 4, D], BF16, "wd") for _ in range(2)])
        psr = Ring([P.psum([128, 512], F32, "ps") for _ in range(7)])
        sm = Ring([P.tile([128, 1], F32, "sm") for _ in range(12)])
        gate = P.tile([128, 8, 32], F32, "gate")
        wgr = P.tile([128, 16, 36], F32, "wgr")
        bgr = P.tile([128, 36], F32, "bgr")
        sgs = Ring([P.tile([128, 512], F32, "sg") for _ in range(2)])
        P.dma("sp", wgr.t[:], io["w_gr"].rearrange("(kc p) n -> p kc n", p=128), writes=[wgr.r])
        P.dma("sp", bgr.t[:], io["b_gr"].partition_broadcast(128), writes=[bgr.r])
        P.dma("sp", lng.t[:], io["ln1_g"].partition_broadcast(128), writes=[lng.r])
        P.dma("sp", lnb.t[:], io["ln1_b"].partition_broadcast(128), writes=[lnb.r])

        att = Ring([P.tile([128, 512], F32, "att") for _ in range(2)])
        for j in range(8):
            P.dma("sp", acc.t[:, j, :], io["hres"][j * 128:(j + 1) * 128, :], writes=[acc.r])
            for (c0, n, d0) in ((0, 4, 0), (512, 2, 4), (768, 4, 12)):
                at = att.next()
                P.dma("sp", at.t[:, 0:n * 128], io["attn"][j * 128:(j + 1) * 128, c0:c0 + n * 128], writes=[at.r])
                ps = psr.next()
                for q in range(n):
                    P.emit("pe", lambda e, ps=ps, at=at, q=q: e.transpose(ps.t[:, q * 128:(q + 1) * 128], at.t[:, q * 128:(q + 1) * 128], ident.t[:]),
                           [at.r, ident.r], [ps.r])
                copy_op(P, evac_eng(), xT.t[:, d0:d0 + n, j * 128:(j + 1) * 128], ps.t[:, 0:n * 128].rearrange("p (a b) -> p a b", a=n), [ps.r], [xT.r])
        for b in range(6):
            P.dma("pool", xT.t[:, 6 + b, :], io["lruT"][b * 128:(b + 1) * 128, :], writes=[xT.r])

        wos = wgs.tiles + wus.tiles
        wi_ = 0
        for n in range(8):
            wt = wos[wi_ % len(wos)]; wi_ += 1
            P.dma("pool", wt.t[:], io["w_o"][:, n * 256:(n + 1) * 256].rearrange("(kc p) n -> p kc n", p=128), writes=[wt.r])
            for j in range(8):
                ps = psr.next()
                for kc in range(16):
                    P.emit("pe", lambda e, ps=ps, wt=wt, kc=kc, j=j: e.matmul(ps.t[:, 0:256], lhsT=xT.t[:, kc, j * 128:(j + 1) * 128], rhs=wt.t[:, kc, :],
                                                                            start=(kc == 0), stop=(kc == 15)), [xT.r, wt.r], [ps.r])
                a = acc.t[:, j, n * 256:(n + 1) * 256]
                P.emit("dve", lambda e, a=a, ps=ps: e.scalar_tensor_tensor(out=a, in0=a, scalar=float(ALPHA), in1=ps.t[:, 0:256], op0=ALU.mult, op1=ALU.add),
                       [acc.r, ps.r], [acc.r])
        if CSTOP >= 2:
            layer_norm_tiles(P, acc, lng, lnb, epst, junk_ap, sm)

        h1f = lng
        h1f_v = h1f.t[:].rearrange("p (a b) -> p a b", a=16)
        hlo = P.tile([128, 16, 128], BF16, "hlo"); whi = P.tile([128, 16, 36], BF16, "whi"); wlo = P.tile([128, 16, 36], BF16, "wlo")
        P.emit("dve", lambda e: e.tensor_copy(out=whi.t[:], in_=wgr.t[:]), [wgr.r], [whi.r])
        P.emit("dve", lambda e: e.tensor_tensor(out=wlo.t[:], in0=wgr.t[:], in1=whi.t[:], op=ALU.subtract), [wgr.r, whi.r], [wlo.r])
        lg = P.tile([128, 36], F32, "lg"); goh = P.tile([128, 4], F32, "goh"); ge = P.tile([128, 4], F32, "ge")
        esel = P.tile([128, 8], F32, "esel"); oh1 = P.tile([128, 8], F32, "oh1"); oh2 = P.tile([128, 8], F32, "oh2")
        msk = P.tile([128, 8], F32, "msk"); eg = P.tile([128, 8], F32, "eg")
        for j in range(8 if CSTOP >= 3.1 else 0):
            for g in range(4):
                ps = psr.next()
                for q in range(4):
                    kc = g * 4 + q
                    P.emit("pe", lambda e, ps=ps, q=q, kc=kc, j=j: e.transpose(ps.t[:, q * 128:(q + 1) * 128], acc.t[:, j, kc * 128:(kc + 1) * 128], ident.t[:]),
                           [acc.r, ident.r], [ps.r])
                P.emit("act", lambda e, ps=ps, g=g, j=j: e.activation(out=xT.t[:, g * 4:(g + 1) * 4, j * 128:(j + 1) * 128],
                                                                    in_=ps.t[:].rearrange("p (a b) -> p a b", a=4), func=AF.Copy), [ps.r], [xT.r])
                ps2 = psr.next()
                for q in range(4):
                    kc = g * 4 + q
                    P.emit("pe", lambda e, ps2=ps2, q=q, kc=kc, j=j: e.transpose(ps2.t[:, q * 128:(q + 1) * 128], acc.t[:, j, kc * 128:(kc + 1) * 128], ident.t[:]),
                           [acc.r, ident.r], [ps2.r])
                P.emit("dve", lambda e, ps2=ps2, g=g: e.tensor_copy(out=h1f_v[:, g * 4:(g + 1) * 4, :], in_=ps2.t[:].rearrange("p (a b) -> p a b", a=4)),
                       [ps2.r], [h1f.r])
            if CSTOP < 3.2:
                continue
            P.emit("dve", lambda e, j=j: e.tensor_tensor(out=hlo.t[:], in0=h1f_v, in1=xT.t[:, :, j * 128:(j + 1) * 128], op=ALU.subtract), [h1f.r, xT.r], [hlo.r])
            ps = psr.next()
            for kc in range(16):
                for ti, (lt_, lr_, rt_, rr_) in enumerate(((xT.t[:, kc, j * 128:(j + 1) * 128], xT.r, whi.t[:, kc, :], whi.r),
                                                           (xT.t[:, kc, j * 128:(j + 1) * 128], xT.r, wlo.t[:, kc, :], wlo.r),
                                                           (hlo.t[:, kc, :], hlo.r, whi.t[:, kc, :], whi.r))):
                    P.emit("pe", lambda e, ps=ps, lt_=lt_, rt_=rt_, kc=kc, ti=ti: e.matmul(ps.t[:, 0:36], lhsT=lt_, rhs=rt_, start=(kc == 0 and ti == 0), stop=(kc == 15 and ti == 2)),
                           [lr_, rr_], [ps.r])
            P.emit("dve", lambda e, ps=ps: e.tensor_tensor(out=lg.t[:], in0=ps.t[:, 0:36], in1=bgr.t[:], op=ALU.add), [ps.r, bgr.r], [lg.r])
            if CSTOP < 3.3:
                continue
            gmax, ngmax, gsum, m1, m2, dd, w1, w2 = [sm.next() for _ in range(8)]
            P.emit("dve", lambda e, gmax=gmax: e.reduce_max(out=gmax.t[:], in_=lg.t[:, 0:4], axis=AX.X), [lg.r], [gmax.r])
            P.emit("dve", lambda e, gmax=gmax: e.tensor_scalar(out=goh.t[:], in0=lg.t[:, 0:4], scalar1=gmax.t[:, 0:1], scalar2=None, op0=ALU.is_equal), [lg.r, gmax.r], [goh.r])
            P.emit("dve", lambda e, gmax=gmax, ngmax=ngmax: e.tensor_scalar_mul(out=ngmax.t[:], in0=gmax.t[:], scalar1=-1.0), [gmax.r], [ngmax.r])
            P.emit("act", lambda e, ngmax=ngmax, gsum=gsum: e.activation(out=ge.t[:], in_=lg.t[:, 0:4], func=AF.Exp, bias=ngmax.t[:, 0:1], accum_out=gsum.t[:, 0:1]),
                   [lg.r, ngmax.r], [ge.r, gsum.r])
            P.emit("dve", lambda e, gsum=gsum: e.reciprocal(out=gsum.t[:], in_=gsum.t[:]), [gsum.r], [gsum.r])
            if CSTOP < 3.4:
                continue
            P.emit("dve", lambda e: e.tensor_scalar_mul(out=esel.t[:], in0=lg.t[:, 4:12], scalar1=goh.t[:, 0:1]), [lg.r, goh.r], [esel.r])
            for g in range(1, 4):
                P.emit("dve", lambda e, g=g: e.scalar_tensor_tensor(out=esel.t[:], in0=lg.t[:, 4 + 8 * g:12 + 8 * g], scalar=goh.t[:, g:g + 1], in1=esel.t[:],
                                                                    op0=ALU.mult, op1=ALU.add), [lg.r, goh.r, esel.r], [esel.r])
            P.emit("dve", lambda e, m1=m1: e.reduce_max(out=m1.t[:], in_=esel.t[:], axis=AX.X), [esel.r], [m1.r])
            P.emit("dve", lambda e, m1=m1: e.tensor_scalar(out=oh1.t[:], in0=esel.t[:], scalar1=m1.t[:, 0:1], scalar2=None, op0=ALU.is_equal), [esel.r, m1.r], [oh1.r])
            P.emit("dve", lambda e: e.scalar_tensor_tensor(out=msk.t[:], in0=oh1.t[:], scalar=-1e30, in1=esel.t[:], op0=ALU.mult, op1=ALU.add), [oh1.r, esel.r], [msk.r])
            P.emit("dve", lambda e, m2=m2: e.reduce_max(out=m2.t[:], in_=msk.t[:], axis=AX.X), [msk.r], [m2.r])
            P.emit("dve", lambda e, m2=m2: e.tensor_scalar(out=oh2.t[:], in0=msk.t[:], scalar1=m2.t[:, 0:1], scalar2=None, op0=ALU.is_equal), [msk.r, m2.r], [oh2.r])
            if CSTOP < 3.5:
                continue
            P.emit("dve", lambda e, m1=m1, m2=m2, dd=dd: e.tensor_tensor(out=dd.t[:], in0=m2.t[:], in1=m1.t[:], op=ALU.subtract), [m1.r, m2.r], [dd.r])
            P.emit("act", lambda e, dd=dd: e.activation(out=dd.t[:], in_=dd.t[:], func=AF.Exp), [dd.r], [dd.r])
            P.emit("dve", lambda e, dd=dd, w1=w1: e.tensor_scalar_add(out=w1.t[:], in0=dd.t[:], scalar1=1.0), [dd.r], [w1.r])
            P.emit("dve", lambda e, w1=w1: e.reciprocal(out=w1.t[:], in_=w1.t[:]), [w1.r], [w1.r])
            P.emit("dve", lambda e, w1=w1, w2=w2, dd=dd: e.tensor_tensor(out=w2.t[:], in0=dd.t[:], in1=w1.t[:], op=ALU.mult), [dd.r, w1.r], [w2.r])
            P.emit("dve", lambda e, w1=w1, gsum=gsum: e.tensor_tensor(out=w1.t[:], in0=w1.t[:], in1=gsum.t[:], op=ALU.mult), [w1.r, gsum.r], [w1.r])
            P.emit("dve", lambda e, w2=w2, gsum=gsum: e.tensor_tensor(out=w2.t[:], in0=w2.t[:], in1=gsum.t[:], op=ALU.mult), [w2.r, gsum.r], [w2.r])
            P.emit("dve", lambda e, w1=w1: e.tensor_scalar_mul(out=eg.t[:], in0=oh1.t[:], scalar1=w1.t[:, 0:1]), [oh1.r, w1.r], [eg.r])
            P.emit("dve", lambda e, w2=w2: e.scalar_tensor_tensor(out=eg.t[:], in0=oh2.t[:], scalar=w2.t[:, 0:1], in1=eg.t[:], op0=ALU.mult, op1=ALU.add),
                   [oh2.r, w2.r, eg.r], [eg.r])
            for g in range(4):
                P.emit("dve", lambda e, g=g, j=j: e.tensor_scalar_mul(out=gate.t[:, j, g * 8:(g + 1) * 8], in0=eg.t[:], scalar1=goh.t[:, g:g + 1]), [eg.r, goh.r], [gate.r])
        for j in range(8 if CSTOP >= 3 else 0):
            P.emit("dve", lambda e, j=j: e.tensor_scalar_mul(out=acc.t[:, j, :], in0=acc.t[:, j, :], scalar1=float(ALPHA)), [acc.r], [acc.r])
        if CSTOP >= 3.02:
            P.dma("sp", lng.t[:], io["ln2_g"].partition_broadcast(128), reads=[h1f.r], writes=[lng.r])
            P.dma("sp", lnb.t[:], io["ln2_b"].partition_broadcast(128), writes=[lnb.r])

        for ex in range(NEXP if CSTOP >= 4 else 0):
            wd = wds.next()
            P.dma("pool", wd.t[:], io["w_down"][ex].rearrange("(fc p) d -> p fc d", p=128), writes=[wd.r])
            for fh in range(2):
                wg, wu = wgs.next(), wus.next()
                P.dma("pool", wg.t[:], io["w_gate"][ex][:, fh * 256:(fh + 1) * 256].rearrange("(kc p) f -> p kc f", p=128), writes=[wg.r])
                P.dma("pool", wu.t[:], io["w_up"][ex][:, fh * 256:(fh + 1) * 256].rearrange("(kc p) f -> p kc f", p=128), writes=[wu.r])
                for fc in range(2):
                    for th in range(2):
                        pg, pu = psr.next(), psr.next()
                        for (wt, ps) in ((wg, pg), (wu, pu)):
                            for kc in range(16):
                                P.emit("pe", lambda e, ps=ps, wt=wt, kc=kc, fc=fc, th=th: e.matmul(
                                    ps.t[:], lhsT=wt.t[:, kc, fc * 128:(fc + 1) * 128], rhs=xT.t[:, kc, th * 512:(th + 1) * 512],
                                    start=(kc == 0), stop=(kc == 15)), [wt.r, xT.r], [ps.r])
                        sg = sgs.next()
                        P.emit("act", lambda e, pg=pg, sg=sg: e.activation(out=sg.t[:], in_=pg.t[:], func=AF.Silu), [pg.r], [sg.r])
                        P.emit("dve", lambda e, pu=pu, sg=sg, fh=fh, fc=fc, th=th: e.tensor_tensor(out=hid.t[:, fh * 2 + fc, th * 512:(th + 1) * 512], in0=sg.t[:], in1=pu.t[:], op=ALU.mult),
                               [sg.r, pu.r], [hid.r])
            for j in range(8):
                for n in range(4):
                    ps = psr.next()
                    for fc in range(4):
                        P.emit("pe", lambda e, ps=ps, fc=fc, j=j, n=n, wd=wd: e.matmul(ps.t[:], lhsT=hid.t[:, fc, j * 128:(j + 1) * 128], rhs=wd.t[:, fc, n * 512:(n + 1) * 512],
                                                                                   start=(fc == 0), stop=(fc == 3)), [hid.r, wd.r], [ps.r])
                    a = acc.t[:, j, n * 512:(n + 1) * 512]
                    P.emit("dve", lambda e, a=a, ps=ps, j=j, ex=ex: e.scalar_tensor_tensor(out=a, in0=ps.t[:], scalar=gate.t[:, j, ex:ex + 1], in1=a, op0=ALU.mult, op1=ALU.add),
                           [ps.r, gate.r, acc.r], [acc.r])
        if CSTOP >= 5:
            layer_norm_tiles(P, acc, lng, lnb, epst, junk_ap, sm)
        for j in range(8):
            P.dma("sp", io["hout"][j * 128:(j + 1) * 128, :], acc.t[:, j, :], reads=[acc.r])
        P.finalize()
    nc.all_engine_barrier()


def build_C():
    nc = bass.Bass("TRN2", target_bir_lowering=False)
    io = dict(
        hres=din(nc, "hres", [TL, D]), attn=din(nc, "attn", [TL, 1280]), lruT=din(nc, "lruT", [768, TL]),
        w_o=din(nc, "w_o", [D, D]), ln1_g=din(nc, "ln1_g", [D]), ln1_b=din(nc, "ln1_b", [D]),
        w_gr=din(nc, "w_gr", [D, 36]), b_gr=din(nc, "b_gr", [36]),
        w_gate=din(nc, "w_gate", [NEXP, D, 512]), w_up=din(nc, "w_up", [NEXP, D, 512]), w_down=din(nc, "w_down", [NEXP, 512, D]),
        ln2_g=din(nc, "ln2_g", [D]), ln2_b=din(nc, "ln2_b", [D]), ident=din(nc, "ident", [128, 128]),
        hout=dout(nc, "hout", [TL, D]),
    )
    phase_C(nc, io)
    return nc


def launch_C(h_shards, resB, prm, l):
    nc = build_C()
    lru_all = np.concatenate([resB[n]["lru"] for n in range(6)], axis=0)
    w_gr = np.ascontiguousarray(np.concatenate([prm["w_group"][l], prm["w_router"][l]], axis=1))
    b_gr = np.ascontiguousarray(np.concatenate([prm["b_group"][l], prm["b_router"][l]], axis=0))
    wg = prm["w_gate"][l].reshape(32, D, 512)[:NEXP]
    wu = prm["w_up"][l].reshape(32, D, 512)[:NEXP]
    wdn = prm["w_down"][l].reshape(32, 512, D)[:NEXP]
    in_maps = []
    for c in range(NC):
        in_maps.append(dict(
            hres=h_shards[c], attn=resB[c]["attn"], lruT=np.ascontiguousarray(lru_all[:, own_rows(c)]),
            w_o=prm["w_o"][l], ln1_g=prm["ln1_g"][l], ln1_b=prm["ln1_b"][l], w_gr=w_gr, b_gr=b_gr,
            w_gate=wg, w_up=wu, w_down=wdn, ln2_g=prm["ln2_g"][l], ln2_b=prm["ln2_b"][l], ident=IDENT))
    return run(nc, in_maps)


def kernel(**inputs):
    prm = {k: np.asarray(v) for k, v in inputs.items()}
    x = prm["x"][0]
    pos = prm["positions"][0]
    h_shards = [np.ascontiguousarray(x[own_rows(c)]) for c in range(NC)]
    for l in range(2):
        resA = launch_A(h_shards, prm["w_in"][l], prm["b_f"][l], pos)
        resB = launch_B(resA, prm, l)
        resC = launch_C(h_shards, resB, prm, l)
        h_shards = [np.ascontiguousarray(r["hout"]) for r in resC]
    out = np.empty((1, SEQ, D), np.float32)
    for c in range(NC):
        out[0, own_rows(c)] = h_shards[c]
    return out
```

```python
import math
from contextlib import ExitStack
import numpy as np
import ml_dtypes
import concourse.bass as bass
import concourse.mybir as mybir
from concourse.bass_utils import run_bass_kernel_spmd

F32 = mybir.dt.float32
BF16 = mybir.dt.bfloat16
I32 = mybir.dt.int32
AF = mybir.ActivationFunctionType
ALU = mybir.AluOpType
AX = mybir.AxisListType

NC = 8
SEQ = 8192
D = 2048
TL = 1024
N_IN = 5382
ALPHA = (2.0 * 2) ** 0.25
LN_EPS = 1e-5
PI = math.pi

SAME_ENGINE_SYNC = True
NEXP = 32
CSTOP = 9
LOOK = 3


class Res:
    __slots__ = ("last_w", "readers")

    def __init__(self):
        self.last_w = None
        self.readers = []


class Tile:
    __slots__ = ("t", "r")

    def __init__(self, t):
        self.t = t
        self.r = Res()


class Op:
    __slots__ = ("eng", "fn", "deps", "sig", "is_dma", "dsem", "dtarget", "has_dep", "waits")

    def __init__(self, eng, fn, is_dma):
        self.eng = eng
        self.fn = fn
        self.deps = []
        self.sig = 0
        self.is_dma = is_dma
        self.dsem = None
        self.dtarget = 0
        self.has_dep = False
        self.waits = []


class Prog:
    ENGS = ("pe", "act", "dve", "pool", "sp")

    def __init__(self, nc, st, n_dma_sems=8):
        self.nc = nc
        self.st = st
        self.ops = []
        self.n_dma_sems = n_dma_sems
        self._n = 0

    def tile(self, shape, dtype, name=None):
        self._n += 1
        return Tile(self.st.enter_context(self.nc.sbuf_tensor("%s_%d" % (name or "t", self._n), shape, dtype)))

    def psum(self, shape, dtype=F32, name=None):
        self._n += 1
        return Tile(self.st.enter_context(self.nc.psum_tensor("%s_%d" % (name or "p", self._n), shape, dtype)))

    def emit(self, eng, fn, reads=(), writes=(), is_dma=False):
        op = Op(eng, fn, is_dma)
        deps = {}
        for r in reads:
            if r.last_w is not None:
                deps[id(r.last_w)] = r.last_w
        for w in writes:
            if w.last_w is not None:
                deps[id(w.last_w)] = w.last_w
            for rd in w.readers:
                deps[id(rd)] = rd
        op.deps = list(deps.values())
        for d in op.deps:
            d.has_dep = True
        for r in reads:
            r.readers.append(op)
        for w in writes:
            w.last_w = op
            w.readers = []
        self.ops.append(op)
        return op

    def dma(self, eng, out, in_, reads=(), writes=()):
        return self.emit(eng, lambda e: e.dma_start(out=out, in_=in_), reads, writes, is_dma=True)

    def finalize(self):
        nc, st = self.nc, self.st
        esem = {e: st.enter_context(nc.semaphore("s_" + e)) for e in self.ENGS}
        dsems = {q: [st.enter_context(nc.semaphore("d_%s%d" % (q, i))) for i in range(self.n_dma_sems)]
                 for q in ("sp", "pool")}
        dcount = {q: [0] * self.n_dma_sems for q in dsems}
        drr = {q: 0 for q in dsems}
        ecount = {e: 0 for e in self.ENGS}
        known = {e: {} for e in self.ENGS}
        per_eng = {e: [] for e in self.ENGS}
        for op in self.ops:
            waits = {}
            k = known[op.eng]

            def need(sem, val):
                key = id(sem)
                if k.get(key, 0) >= val:
                    return
                if key not in waits or waits[key][1] < val:
                    waits[key] = (sem, val)

            for d in op.deps:
                if d.is_dma:
                    need(d.dsem, d.dtarget)
                else:
                    if d.eng == op.eng and (d.eng == "pe" or not SAME_ENGINE_SYNC):
                        continue
                    need(esem[d.eng], d.sig)
            if op.is_dma:
                q = op.eng
                i = drr[q]
                drr[q] = (i + 1) % self.n_dma_sems
                sem = dsems[q][i]
                if dcount[q][i] > 0:
                    need(sem, dcount[q][i] * 16)
                dcount[q][i] += 1
                op.dsem = sem
                op.dtarget = dcount[q][i] * 16
            elif op.has_dep:
                ecount[op.eng] += 1
                op.sig = ecount[op.eng]
            op.waits = list(waits.values())
            for sem, val in op.waits:
                k[id(sem)] = val
            per_eng[op.eng].append(op)
        fin = []
        for q in dsems:
            for i, sem in enumerate(dsems[q]):
                if dcount[q][i] > 0:
                    fin.append((sem, dcount[q][i] * 16))
        for e in self.ENGS:
            if ecount[e] > 0:
                fin.append((esem[e], ecount[e]))
        block = st.enter_context(nc.Block())

        def replay(name, last=False):
            def body(e):
                for op in per_eng[name]:
                    for sem, val in op.waits:
                        e.wait_ge(sem, val)
                    inst = op.fn(e)
                    if op.is_dma:
                        inst.then_inc(op.dsem, 16)
                    elif op.sig:
                        inst.then_inc(esem[name], 1)
                if last:
                    for sem, val in fin:
                        e.wait_ge(sem, val)
            return body

        block.tensor(replay("pe"))
        block.scalar(replay("act"))
        block.vector(replay("dve"))
        block.gpsimd(replay("pool"))
        block.sync(replay("sp", True))


class Ring:
    def __init__(self, tiles):
        self.tiles = tiles
        self.i = 0

    def next(self):
        t = self.tiles[self.i % len(self.tiles)]
        self.i += 1
        return t


def din(nc, name, shape, dt=F32):
    return nc.dram_tensor(name, list(shape), dt, kind="ExternalInput").ap()


def dout(nc, name, shape, dt=F32):
    return nc.dram_tensor(name, list(shape), dt, kind="ExternalOutput").ap()


_ev = [0]


def evac_eng():
    _ev[0] += 1
    return "act" if _ev[0] % 2 else "dve"


def copy_op(P, eng, out, in_, reads, writes, scale=None):
    if eng == "act":
        if scale is None:
            P.emit("act", lambda e: e.activation(out=out, in_=in_, func=AF.Copy), reads, writes)
        else:
            P.emit("act", lambda e: e.activation(out=out, in_=in_, func=AF.Copy, scale=float(scale)), reads, writes)
    else:
        if scale is None:
            P.emit("dve", lambda e: e.tensor_copy(out=out, in_=in_), reads, writes)
        else:
            P.emit("dve", lambda e: e.tensor_scalar_mul(out=out, in0=in_, scalar1=float(scale)), reads, writes)


def transpose_to_xT(P, src_ap_fn, ident, xT, nchunks=16):
    hts = Ring([P.tile([128, nchunks * 128], F32, "ht") for _ in range(2)])
    pst = Ring([P.psum([128, 512], F32, "pst") for _ in range(2)])
    for j in range(8):
        ht = hts.next()
        P.dma("sp", ht.t[:], src_ap_fn(j), writes=[ht.r])
        for g in range(nchunks // 4):
            ps = pst.next()
            for q in range(4):
                kc = g * 4 + q
                P.emit("pe", lambda e, ps=ps, q=q, ht=ht, kc=kc: e.transpose(
                    ps.t[:, q * 128:(q + 1) * 128], ht.t[:, kc * 128:(kc + 1) * 128], ident.t[:]),
                    reads=[ht.r, ident.r], writes=[ps.r])
            copy_op(P, evac_eng(), xT.t[:, g * 4:(g + 1) * 4, j * 128:(j + 1) * 128],
                    ps.t[:].rearrange("p (a b) -> p a b", a=4), [ps.r], [xT.r])


def phase_A(nc, io):
    with ExitStack() as st:
        P = Prog(nc, st)
        ident = P.tile([128, 128], F32, "ident")
        P.dma("sp", ident.t[:], io["ident"], writes=[ident.r])
        xT = P.tile([128, 16, TL], BF16, "xT")
        transpose_to_xT(P, lambda j: io["hin"][j * 128:(j + 1) * 128, :], ident, xT)

        posi = P.tile([128, 8], I32, "posi")
        posf = P.tile([128, 8], F32, "posf")
        invf = P.tile([128, 8], F32, "invf")
        ang = P.tile([128, 8, 8], F32, "ang")
        red = P.tile([128, 8, 8], F32, "red")
        cost = P.tile([128, 8, 8], F32, "cos")
        sint = P.tile([128, 8, 8], F32, "sin")
        bfb = P.tile([128, 6], F32, "bfb")
        P.dma("sp", posi.t[:], io["pos"], writes=[posi.r])
        P.dma("sp", invf.t[:], io["invf"], writes=[invf.r])
        P.dma("sp", bfb.t[:], io["b_f"].partition_broadcast(128), writes=[bfb.r])
        P.emit("dve", lambda e: e.tensor_copy(out=posf.t[:], in_=posi.t[:]), [posi.r], [posf.r])
        P.emit("dve", lambda e: e.tensor_tensor(out=ang.t[:], in0=posf.t[:].unsqueeze(2).to_broadcast([128, 8, 8]),
                                                in1=invf.t[:].unsqueeze(1).to_broadcast([128, 8, 8]), op=ALU.mult),
               [posf.r, invf.r], [ang.r])
        angi = P.tile([128, 8, 8], I32, "angi")
        angf = P.tile([128, 8, 8], F32, "angf")
        ang2 = P.tile([128, 8, 8], F32, "ang2")
        for (shift, dst) in ((0.0, sint), (0.5 * PI, cost)):
            P.emit("dve", lambda e, shift=shift: e.tensor_scalar_add(out=ang2.t[:], in0=ang.t[:], scalar1=float(shift)), [ang.r], [ang2.r])
            P.emit("dve", lambda e: e.tensor_scalar_mul(out=red.t[:], in0=ang2.t[:], scalar1=float(1.0 / (2 * PI))), [ang2.r], [red.r])
            P.emit("dve", lambda e: e.tensor_copy(out=angi.t[:], in_=red.t[:]), [red.r], [angi.r])
            P.emit("dve", lambda e: e.tensor_copy(out=angf.t[:], in_=angi.t[:]), [angi.r], [angf.r])
            P.emit("dve", lambda e: e.scalar_tensor_tensor(out=red.t[:], in0=angf.t[:], scalar=float(-2 * PI), in1=ang2.t[:], op0=ALU.mult, op1=ALU.add),
                   [angf.r, ang2.r], [red.r])
            P.emit("dve", lambda e: e.tensor_single_scalar(out=angf.t[:], in_=red.t[:], scalar=float(PI), op=ALU.is_gt), [red.r], [angf.r])
            P.emit("dve", lambda e: e.scalar_tensor_tensor(out=red.t[:], in0=angf.t[:], scalar=float(-2 * PI), in1=red.t[:], op0=ALU.mult, op1=ALU.add),
                   [angf.r, red.r], [red.r])
            P.emit("dve", lambda e: e.tensor_single_scalar(out=angf.t[:], in_=red.t[:], scalar=float(-PI), op=ALU.is_lt), [red.r], [angf.r])
            P.emit("dve", lambda e: e.scalar_tensor_tensor(out=red.t[:], in0=angf.t[:], scalar=float(2 * PI), in1=red.t[:], op0=ALU.mult, op1=ALU.add),
                   [angf.r, red.r], [red.r])
            P.emit("act", lambda e, dst=dst: e.activation(out=dst.t[:], in_=red.t[:], func=AF.Sin), [red.r], [dst.r])

        wbufs = Ring([P.tile([128, 16, 512], BF16, "w") for _ in range(3)])
        psA = Ring([P.psum([128, 512], F32, "psA") for _ in range(3)])

        def load_w(c0, ncols):
            wb = wbufs.next()
            P.dma("pool", wb.t[:, :, 0:ncols], io["w_in"][:, c0:c0 + ncols].rearrange("(kc p) n -> p kc n", p=128),
                  writes=[wb.r])
            return wb

        otf = Ring([P.tile([128, TL], F32, "otf") for _ in range(2)])
        otb = Ring([P.tile([128, TL], BF16, "otb") for _ in range(2)])

        def feat_chunk(c0, nblk, out_ap, scale, is_bf):
            wb = load_w(c0, nblk * 128)
            for b in range(nblk):
                ot = (otb if is_bf else otf).next()
                for th in range(2):
                    ps = psA.next()
                    for kc in range(16):
                        P.emit("pe", lambda e, ps=ps, wb=wb, kc=kc, b=b, th=th: e.matmul(
                            ps.t[:], lhsT=wb.t[:, kc, b * 128:(b + 1) * 128], rhs=xT.t[:, kc, th * 512:(th + 1) * 512],
                            start=(kc == 0), stop=(kc == 15)), reads=[wb.r, xT.r], writes=[ps.r])
                    copy_op(P, evac_eng(), ot.t[:, th * 512:(th + 1) * 512], ps.t[:], [ps.r], [ot.r], scale)
                P.dma("sp", out_ap(b), ot.t[:], reads=[ot.r])

        def tok_chunk(c0, ncols, handler):
            wb = load_w(c0, ncols)
            for j in range(8):
                ps = psA.next()
                for kc in range(16):
                    P.emit("pe", lambda e, ps=ps, wb=wb, kc=kc, j=j: e.matmul(
                        ps.t[:, 0:ncols], lhsT=xT.t[:, kc, j * 128:(j + 1) * 128], rhs=wb.t[:, kc, 0:ncols],
                        start=(kc == 0), stop=(kc == 15)), reads=[wb.r, xT.r], writes=[ps.r])
                handler(j, ps)

        feat_chunk(0, 4, lambda b: io["qTf"][b * 128:(b + 1) * 128, :], 128 ** -0.5, True)
        feat_chunk(512, 2, lambda b: io["qTf"][(4 + b) * 128:(5 + b) * 128, :], 128 ** -0.5, True)
        feat_chunk(768, 4, lambda b: io["kTf"][b * 128:(b + 1) * 128, :], None, True)
        feat_chunk(1280, 2, lambda b: io["kTf"][(4 + b) * 128:(5 + b) * 128, :], None, True)
        feat_chunk(2310, 4, lambda b: io["xrT"][b * 128:(b + 1) * 128, :], None, False)
        feat_chunk(2310 + 512, 2, lambda b: io["xrT"][(4 + b) * 128:(5 + b) * 128, :], None, False)
        feat_chunk(3078, 4, lambda b: io["grT"][b * 128:(b + 1) * 128, :], None, False)
        feat_chunk(3078 + 512, 2, lambda b: io["grT"][(4 + b) * 128:(5 + b) * 128, :], None, False)

        vts = Ring([P.tile([128, 512], BF16, "vt") for _ in range(2)])

        def h_v(key, col0, ncols):
            def h(j, ps):
                vt = vts.next()
                copy_op(P, evac_eng(), vt.t[:, 0:ncols], ps.t[:, 0:ncols], [ps.r], [vt.r])
                P.dma("sp", io[key][j * 128:(j + 1) * 128, col0:col0 + ncols], vt.t[:, 0:ncols], reads=[vt.r])
            return h

        tok_chunk(1536, 512, h_v("vf", 0, 512))
        logf = P.tile([128, 8, 6], F32, "logf")
        zt = P.tile([128, 6], F32, "zt")
        h_v2 = h_v("vf", 512, 256)

        def h_vfa(j, ps):
            P.emit("dve", lambda e: e.tensor_tensor(out=zt.t[:], in0=ps.t[:, 256:262], in1=bfb.t[:], op=ALU.add),
                   [ps.r, bfb.r], [zt.r])
            P.emit("act", lambda e: e.activation(out=zt.t[:], in_=zt.t[:], func=AF.Exp, scale=-1.0), [zt.r], [zt.r])
            P.emit("act", lambda e: e.activation(out=zt.t[:], in_=zt.t[:], func=AF.Ln, bias=1.0), [zt.r], [zt.r])
            P.emit("dve", lambda e: e.tensor_scalar_mul(out=logf.t[:, j, :], in0=zt.t[:], scalar1=-1.0), [zt.r], [logf.r])
            h_v2(j, ps)

        tok_chunk(2048, 262, h_vfa)
        P.dma("sp", io["logf"].rearrange("(j p) h -> p j h", p=128), logf.t[:], reads=[logf.r])

        qk = P.tile([128, 512], F32, "qk")
        ta, tb, tc_, td = [P.tile([128, 8, 8], F32, "rt") for _ in range(4)]
        pstr = Ring([P.psum([128, 512], F32, "pstr") for _ in range(2)])

        def h_qk(key, scale):
            qTd = P.tile([128, 4, TL], BF16, "qTd")

            def h(j, ps):
                copy_op(P, "act", qk.t[:], ps.t[:], [ps.r], [qk.r], scale)
                v3 = qk.t[:].rearrange("p (s d) -> p s d", s=8)
                t1, t2 = v3[:, :, 0:8], v3[:, :, 8:16]
                cb = cost.t[:, j, :].unsqueeze(1).to_broadcast([128, 8, 8])
                sb = sint.t[:, j, :].unsqueeze(1).to_broadcast([128, 8, 8])
                rd = [qk.r, cost.r, sint.r]
                P.emit("dve", lambda e: e.tensor_tensor(out=ta.t[:], in0=t1, in1=cb, op=ALU.mult), rd, [ta.r])
                P.emit("dve", lambda e: e.tensor_tensor(out=tb.t[:], in0=t2, in1=sb, op=ALU.mult), rd, [tb.r])
                P.emit("dve", lambda e: e.tensor_tensor(out=tc_.t[:], in0=t2, in1=cb, op=ALU.mult), rd, [tc_.r])
                P.emit("dve", lambda e: e.tensor_tensor(out=td.t[:], in0=t1, in1=sb, op=ALU.mult), rd, [td.r])
                P.emit("dve", lambda e: e.tensor_tensor(out=t1, in0=ta.t[:], in1=tb.t[:], op=ALU.subtract), [ta.r, tb.r], [qk.r])
                P.emit("dve", lambda e: e.tensor_tensor(out=t2, in0=tc_.t[:], in1=td.t[:], op=ALU.add), [tc_.r, td.r], [qk.r])
                pt = pstr.next()
                for b in range(4):
                    P.emit("pe", lambda e, b=b, pt=pt: e.transpose(pt.t[:, b * 128:(b + 1) * 128], qk.t[:, b * 128:(b + 1) * 128], ident.t[:]),
                           [qk.r, ident.r], [pt.r])
                copy_op(P, evac_eng(), qTd.t[:, :, j * 128:(j + 1) * 128], pt.t[:].rearrange("p (a b) -> p a b", a=4), [pt.r], [qTd.r])
                if j == 7:
                    P.dma("sp", io[key].rearrange("(b p) t -> p b t", p=128), qTd.t[:], reads=[qTd.r])
            return h

        tok_chunk(3846, 512, h_qk("qTd", 64 ** -0.5))
        tok_chunk(4358, 512, h_qk("kTd", None))
        tok_chunk(4870, 512, h_v("vd", 0, 512))
        P.finalize()
    nc.all_engine_barrier()


def build_A():
    nc = bass.Bass("TRN2", target_bir_lowering=False)
    io = dict(
        hin=din(nc, "hin", [TL, D]), w_in=din(nc, "w_in", [D, N_IN]), b_f=din(nc, "b_f", [6]),
        pos=din(nc, "pos", [128, 8], I32), invf=din(nc, "invf", [128, 8]), ident=din(nc, "ident", [128, 128]),
        qTf=dout(nc, "qTf", [768, TL], BF16), kTf=dout(nc, "kTf", [768, TL], BF16), vf=dout(nc, "vf", [TL, 768], BF16),
        logf=dout(nc, "logf", [TL, 6]), xrT=dout(nc, "xrT", [768, TL]), grT=dout(nc, "grT", [768, TL]),
        qTd=dout(nc, "qTd", [512, TL], BF16), kTd=dout(nc, "kTd", [512, TL], BF16), vd=dout(nc, "vd", [TL, 512], BF16),
    )
    phase_A(nc, io)
    return nc


def own_rows(c):
    return np.concatenate([np.arange((8 * j + c) * 128, (8 * j + c + 1) * 128) for j in range(8)])


IDENT = np.eye(128, dtype=np.float32)
INVF = np.broadcast_to((np.float32(500000.0) ** (-np.arange(8, dtype=np.float32) * np.float32(2.0) / np.float32(16))).astype(np.float32), (128, 8)).copy()


def run(nc, in_maps):
    res = run_bass_kernel_spmd(nc, in_maps, core_ids=list(range(NC)))
    return res.results


def launch_A(h_shards, w_in_l, b_f_l, positions):
    nc = build_A()
    in_maps = []
    for c in range(NC):
        in_maps.append(dict(hin=h_shards[c], w_in=w_in_l, b_f=b_f_l,
                            pos=np.ascontiguousarray(positions[own_rows(c)].reshape(8, 128).T), invf=INVF, ident=IDENT))
    return run(nc, in_maps)


def split3(P, src, cur, back, n, npart, sign):
    outs = []
    P.emit("dve", lambda e: e.tensor_scalar_mul(out=cur.t[:], in0=src.t[:], scalar1=float(sign)), [src.r], [cur.r])
    for i in range(3):
        o = P.tile([npart, n], BF16, "s3o")
        P.emit("dve", lambda e, o=o: e.tensor_copy(out=o.t[:], in_=cur.t[:]), [cur.r], [o.r])
        if i < 2:
            P.emit("dve", lambda e, o=o: e.tensor_copy(out=back.t[:], in_=o.t[:]), [o.r], [back.r])
            P.emit("dve", lambda e: e.tensor_tensor(out=cur.t[:], in0=cur.t[:], in1=back.t[:], op=ALU.subtract), [cur.r, back.r], [cur.r])
        outs.append(o)
    return outs


def phase_B(nc, io, lam_init):
    with ExitStack() as st:
        P = Prog(nc, st)
        CH = 2048
        cw = P.tile([128, 4], F32, "cw"); cb = P.tile([128, 1], F32, "cb")
        wa32 = P.tile([128, 128], F32, "wa32"); wi32 = P.tile([128, 128], F32, "wi32")
        wa = P.tile([128, 128], BF16, "wa"); wi = P.tile([128, 128], BF16, "wi")
        ba = P.tile([128, 1], F32, "ba"); bi = P.tile([128, 1], F32, "bi"); lm = P.tile([128, 1], F32, "lm")
        one = P.tile([128, 1], F32, "one"); c8 = P.tile([128, 1], F32, "c8")
        for t_, k_ in ((cw, "conv_w"), (cb, "conv_b"), (wa32, "w_a"), (wi32, "w_i"), (ba, "b_a"), (bi, "b_i"), (lm, "lru_lam")):
            P.dma("sp", t_.t[:], io[k_], writes=[t_.r])
        P.emit("dve", lambda e: e.tensor_copy(out=wa.t[:], in_=wa32.t[:]), [wa32.r], [wa.r])
        P.emit("dve", lambda e: e.tensor_copy(out=wi.t[:], in_=wi32.t[:]), [wi32.r], [wi.r])
        P.emit("dve", lambda e: e.memset(one.t[:], 1.0), [], [one.r])
        P.emit("act", lambda e: e.activation(out=c8.t[:], in_=lm.t[:], func=AF.Exp, scale=-1.0), [lm.r], [c8.r])
        P.emit("act", lambda e: e.activation(out=c8.t[:], in_=c8.t[:], func=AF.Ln, bias=one.t[:, 0:1]), [c8.r, one.r], [c8.r])
        P.emit("dve", lambda e: e.tensor_scalar_mul(out=c8.t[:], in0=c8.t[:], scalar1=-8.0), [c8.r], [c8.r])
        xpad = P.tile([128, CH + 3], F32, "xpad"); xc = P.tile([128, CH], F32, "xc"); xcb = P.tile([128, CH], BF16, "xcb")
        rr = P.tile([128, CH], F32, "rr"); ii = P.tile([128, CH], F32, "ii"); tmp = P.tile([128, CH], F32, "tmp")
        hh = Ring([P.tile([128, CH], F32, "hh") for _ in range(2)]); gg = P.tile([128, CH], F32, "gg"); gt2 = P.tile([128, CH], F32, "gt2")
        psl = Ring([P.psum([128, 512], F32, "psl") for _ in range(2)])
        hprev = None
        for ck in range(SEQ // CH):
            t0 = ck * CH
            P.dma("sp", xpad.t[:], io["xr"][:, t0:t0 + CH + 3], writes=[xpad.r])
            P.dma("sp", gg.t[:], io["gr"][:, t0:t0 + CH], writes=[gg.r])
            P.emit("dve", lambda e: e.tensor_scalar(out=xc.t[:], in0=xpad.t[:, 0:CH], scalar1=cw.t[:, 0:1], scalar2=cb.t[:, 0:1],
                                                    op0=ALU.mult, op1=ALU.add), [xpad.r, cw.r, cb.r], [xc.r])
            for j in range(1, 4):
                P.emit("dve", lambda e, j=j: e.scalar_tensor_tensor(out=xc.t[:], in0=xpad.t[:, j:j + CH], scalar=cw.t[:, j:j + 1], in1=xc.t[:],
                                                                    op0=ALU.mult, op1=ALU.add), [xpad.r, cw.r, xc.r], [xc.r])
            P.emit("dve", lambda e: e.tensor_copy(out=xcb.t[:], in_=xc.t[:]), [xc.r], [xcb.r])
            for s in range(CH // 512):
                sl = slice(s * 512, (s + 1) * 512)
                for (wt, bt, dst) in ((wa, ba, rr), (wi, bi, ii)):
                    ps = psl.next()
                    P.emit("pe", lambda e, ps=ps, wt=wt, sl=sl: e.matmul(ps.t[:], lhsT=wt.t[:], rhs=xcb.t[:, sl], start=True, stop=True),
                           [wt.r, xcb.r], [ps.r])
                    P.emit("act", lambda e, ps=ps, bt=bt, dst=dst, sl=sl: e.activation(out=dst.t[:, sl], in_=ps.t[:], func=AF.Sigmoid, bias=bt.t[:, 0:1]),
                           [ps.r, bt.r], [dst.r])
            P.emit("act", lambda e: e.activation(out=rr.t[:], in_=rr.t[:], func=AF.Exp, scale=c8.t[:, 0:1]), [rr.r, c8.r], [rr.r])
            P.emit("dve", lambda e: e.tensor_tensor(out=tmp.t[:], in0=rr.t[:], in1=rr.t[:], op=ALU.mult), [rr.r], [tmp.r])
            P.emit("act", lambda e: e.activation(out=tmp.t[:], in_=tmp.t[:], func=AF.Sqrt, scale=-1.0, bias=one.t[:, 0:1]), [tmp.r, one.r], [tmp.r])
            P.emit("dve", lambda e: e.tensor_tensor(out=ii.t[:], in0=ii.t[:], in1=xc.t[:], op=ALU.mult), [ii.r, xc.r], [ii.r])
            P.emit("dve", lambda e: e.tensor_tensor(out=ii.t[:], in0=ii.t[:], in1=tmp.t[:], op=ALU.mult), [ii.r, tmp.r], [ii.r])
            hcur = hh.next()
            if hprev is None:
                P.emit("dve", lambda e, hcur=hcur: e.tensor_tensor_scan(out=hcur.t[:], data0=rr.t[:], data1=ii.t[:], initial=0.0,
                                                                        op0=ALU.mult, op1=ALU.add), [rr.r, ii.r], [hcur.r])
            else:
                P.emit("dve", lambda e, hcur=hcur, hp=hprev: e.tensor_tensor_scan(out=hcur.t[:], data0=rr.t[:], data1=ii.t[:],
                                                                                  initial=hp.t[:, CH - 1:CH], op0=ALU.mult, op1=ALU.add),
                       [rr.r, ii.r, hprev.r], [hcur.r])
            hprev = hcur
            P.emit("pool", lambda e: e.tensor_tensor(out=gt2.t[:], in0=gg.t[:], in1=gg.t[:], op=ALU.mult), [gg.r], [gt2.r])
            P.emit("pool", lambda e: e.tensor_scalar(out=gt2.t[:], in0=gt2.t[:], scalar1=0.044715, scalar2=1.0, op0=ALU.mult, op1=ALU.add), [gt2.r], [gt2.r])
            P.emit("pool", lambda e: e.tensor_tensor(out=gt2.t[:], in0=gt2.t[:], in1=gg.t[:], op=ALU.mult), [gt2.r, gg.r], [gt2.r])
            P.emit("act", lambda e: e.activation(out=gt2.t[:], in_=gt2.t[:], func=AF.Sigmoid, scale=2.0 * math.sqrt(2.0 / math.pi)), [gt2.r], [gt2.r])
            P.emit("pool", lambda e: e.tensor_tensor(out=gt2.t[:], in0=gt2.t[:], in1=gg.t[:], op=ALU.mult), [gt2.r, gg.r], [gt2.r])
            P.emit("dve", lambda e, hcur=hcur: e.tensor_tensor(out=tmp.t[:], in0=gt2.t[:], in1=hcur.t[:], op=ALU.mult), [gt2.r, hcur.r], [tmp.r])
            P.dma("sp", io["lru"][:, t0:t0 + CH], tmp.t[:], reads=[tmp.r])
        P.finalize()
    nc.all_engine_barrier()

    with ExitStack() as st:
        P = Prog(nc, st)
        kaug_d = nc.dram_tensor("kaug_d", [6, 6, SEQ], BF16).ap()
        qaug_d = nc.dram_tensor("qaug_d", [6, 6, TL], BF16).ap()
        lf = P.tile([6, SEQ], F32, "lf"); ones6 = P.tile([6, SEQ], BF16, "ones6"); cum = P.tile([6, SEQ], F32, "cum")
        om = P.tile([6, SEQ], F32, "om"); cown = P.tile([6, TL], F32, "cown")
        qcur = P.tile([6, TL], F32, "qcur"); qback = P.tile([6, TL], F32, "qback")
        P.dma("sp", lf.t[:], io["logfT"], writes=[lf.r])
        P.dma("sp", om.t[:], io["ownmask"], writes=[om.r])
        P.emit("dve", lambda e: e.memset(ones6.t[:], 1.0), [], [ones6.r])
        P.emit("dve", lambda e: e.tensor_tensor_scan(out=cum.t[:], data0=ones6.t[:], data1=lf.t[:], initial=0.0, op0=ALU.mult, op1=ALU.add),
               [ones6.r, lf.r], [cum.r])
        P.emit("dve", lambda e: e.tensor_tensor(out=om.t[:], in0=om.t[:], in1=cum.t[:], op=ALU.mult), [om.r, cum.r], [om.r])
        P.emit("dve", lambda e: e.tensor_reduce(out=cown.t[:].rearrange("p (j i) -> p j i", j=8),
                                                in_=om.t[:].rearrange("p (j r i) -> p j i r", j=8, r=8, i=128), axis=AX.X, op=ALU.add),
               [om.r], [cown.r])
        q3 = split3(P, cown, qcur, qback, TL, 6, 1.0)
        k3 = split3(P, cum, lf, om, SEQ, 6, -1.0)
        for i in range(3):
            P.dma("sp", kaug_d[:, i, :], ones6.t[:], reads=[ones6.r])
            P.dma("sp", kaug_d[:, 3 + i, :], k3[i].t[:], reads=[k3[i].r])
            P.dma("sp", qaug_d[:, i, :], q3[i].t[:], reads=[q3[i].r])
            P.dma("sp", qaug_d[:, 3 + i, :], ones6.t[:, 0:TL], reads=[ones6.r])
        P.finalize()
    nc.all_engine_barrier()

    with ExitStack() as st:
        P = Prog(nc, st)
        mF = P.tile([128, 8, 128], BF16, "mF"); mD = P.tile([128, 8, 128], BF16, "mD")
        P.dma("sp", mF.t[:], io["maskF"], writes=[mF.r])
        P.dma("sp", mD.t[:], io["maskD"], writes=[mD.r])
        lq = [P.tile([128, 64], F32, "lq") for _ in range(4)]
        for t_, k_ in zip(lq, ("lam_q1", "lam_k1", "lam_q2", "lam_k2")):
            P.dma("sp", t_.t[:], io[k_].partition_broadcast(128), writes=[t_.r])
        s1 = P.tile([128, 1], F32, "s1"); s2 = P.tile([128, 1], F32, "s2"); nlam = P.tile([128, 1], F32, "nlam")
        epst = P.tile([128, 1], F32, "eps")
        P.emit("dve", lambda e: e.memset(epst.t[:], LN_EPS), [], [epst.r])
        for (a_, b_, s_) in ((lq[0], lq[1], s1), (lq[2], lq[3], s2)):
            P.emit("dve", lambda e, a_=a_, b_=b_: e.tensor_tensor(out=a_.t[:], in0=a_.t[:], in1=b_.t[:], op=ALU.mult), [a_.r, b_.r], [a_.r])
            P.emit("dve", lambda e, a_=a_, s_=s_: e.reduce_sum(out=s_.t[:], in_=a_.t[:], axis=AX.X), [a_.r], [s_.r])
            P.emit("act", lambda e, s_=s_: e.activation(out=s_.t[:], in_=s_.t[:], func=AF.Exp), [s_.r], [s_.r])
        P.emit("dve", lambda e: e.tensor_tensor(out=nlam.t[:], in0=s2.t[:], in1=s1.t[:], op=ALU.subtract), [s1.r, s2.r], [nlam.r])
        P.emit("dve", lambda e: e.tensor_scalar_add(out=nlam.t[:], in0=nlam.t[:], scalar1=-float(lam_init)), [nlam.r], [nlam.r])
        gsb = P.tile([128, 128], F32, "gsb")
        P.dma("sp", gsb.t[:], io["subln_g"].partition_broadcast(128), writes=[gsb.r])
        P.emit("dve", lambda e: e.tensor_scalar_mul(out=gsb.t[:], in0=gsb.t[:], scalar1=float(1.0 - lam_init)), [gsb.r], [gsb.r])

        kTs = Ring([P.tile([128, SEQ], BF16, "kT") for _ in range(2)])
        kaugs = Ring([P.tile([128, SEQ], BF16, "kaug") for _ in range(2)])
        qaugs = Ring([P.tile([128, TL], BF16, "qaug") for _ in range(2)])
        for t_ in kaugs.tiles + qaugs.tiles:
            P.emit("pool", lambda e, t_=t_: e.memset(t_.t[:], 0.0), [], [t_.r])
        vs = Ring([P.tile([128, 64, 130], BF16, "v") for _ in range(2)])
        qTs = Ring([P.tile([128, TL], BF16, "qT") for _ in range(2)])
        for _ in range(2):
            v_ = vs.next()
            P.emit("dve", lambda e, v_=v_: e.memset(v_.t[:, :, 128:130], 1.0), [], [v_.r])
        pss = Ring([P.psum([128, 512], F32, "pss") for _ in range(LOOK + 1)])
        pTs = Ring([P.tile([128, 512], BF16, "pT") for _ in range(LOOK + 2)])
        o1s = Ring([P.tile([128, 128], F32, "o1") for _ in range(3)])
        rcs = Ring([P.tile([128, 1], F32, "rc") for _ in range(6)])
        junk = P.tile([128, 128], F32, "junk")

        class Acc:
            pass
        accbanks = [P.psum([128, 512], F32, "accb") for _ in range(4)]
        accs = []
        for j in range(8):
            a_ = Acc()
            a_.ap = accbanks[j % 4].t[:, 0:130]
            a_.r = accbanks[j % 4].r
            accs.append(a_)
        o1all = P.tile([128, 8, 128], F32, "o1all")
        o1res = [Res() for _ in range(8)]

        tasks = []
        head_loads = []

        def sel_head(kall, vall, qown, hidx, aug=None):
            kT, v_, qT = kTs.next(), vs.next(), qTs.next()
            ka = qa = None
            if aug is not None:
                ka, qa = kaugs.next(), qaugs.next()

            def load():
                P.dma("sp", kT.t[:], kall[hidx * 128:(hidx + 1) * 128, :], writes=[kT.r])
                P.dma("sp", v_.t[:, :, 0:128], vall.rearrange("(t p) d -> p t d", p=128)[:, :, hidx * 128:(hidx + 1) * 128], writes=[v_.r])
                P.dma("sp", qT.t[:], qown[hidx * 128:(hidx + 1) * 128, :], writes=[qT.r])
                if aug is not None:
                    P.dma("sp", ka.t[0:6, :], kaug_d[aug], writes=[ka.r])
                    P.dma("sp", qa.t[0:6, :], qaug_d[aug], writes=[qa.r])
            head_loads.append(load)
            return kT, v_, qT, ka, qa

        def add_map(qk_mm, v_, mask, clamp, fin_fn, head_first):
            first = True
            for ps_ in range(2):
                for kt in range(32 * (ps_ + 1)):
                    G = kt // 8
                    jlo, jhi = max(G, 4 * ps_), 4 * (ps_ + 1)
                    if jlo >= jhi:
                        continue
                    tasks.append(dict(kt=kt, G=G, jlo=jlo, jhi=jhi, qk=qk_mm, v=v_, mask=mask, clamp=clamp, fin=fin_fn,
                                      head_first=head_first if first else None))
                    first = False

        def fox_fin(h):
            def fin(j):
                acc = accs[j]
                rc = rcs.next(); o1 = o1s.next()
                P.emit("dve", lambda e: e.reciprocal(out=rc.t[:], in_=acc.ap[:, 128:129]), [acc.r], [rc.r])
                P.emit("dve", lambda e: e.tensor_scalar_mul(out=o1.t[:], in0=acc.ap[:, 0:128], scalar1=rc.t[:, 0:1]), [acc.r, rc.r], [o1.r])
                P.dma("sp", io["attn"][j * 128:(j + 1) * 128, h * 128:(h + 1) * 128], o1.t[:], reads=[o1.r])
            return fin

        def diff_fin0():
            def fin(j):
                acc = accs[j]
                rc1 = rcs.next()
                P.emit("dve", lambda e: e.reciprocal(out=rc1.t[:], in_=acc.ap[:, 128:129]), [acc.r], [rc1.r])
                P.emit("dve", lambda e: e.tensor_scalar_mul(out=o1all.t[:, j, :], in0=acc.ap[:, 0:128], scalar1=rc1.t[:, 0:1]), [acc.r, rc1.r], [o1res[j]])
            return fin

        def diff_fin1(hd):
            def fin(j):
                acc = accs[j]
                rc2, ss = rcs.next(), rcs.next()
                oc = o1s.next()
                P.emit("dve", lambda e: e.reciprocal(out=rc2.t[:], in_=acc.ap[:, 128:129]), [acc.r], [rc2.r])
                P.emit("dve", lambda e: e.tensor_tensor(out=rc2.t[:], in0=rc2.t[:], in1=nlam.t[:], op=ALU.mult), [rc2.r, nlam.r], [rc2.r])
                P.emit("dve", lambda e: e.scalar_tensor_tensor(out=oc.t[:], in0=acc.ap[:, 0:128], scalar=rc2.t[:, 0:1], in1=o1all.t[:, j, :],
                                                               op0=ALU.mult, op1=ALU.add), [acc.r, rc2.r, o1res[j]], [oc.r])
                P.emit("act", lambda e: e.activation(out=junk.t[:], in_=oc.t[:], func=AF.Square, accum_out=ss.t[:, 0:1]), [oc.r], [junk.r, ss.r])
                P.emit("act", lambda e: e.activation(out=ss.t[:], in_=ss.t[:], func=AF.Sqrt, scale=1.0 / 128.0, bias=epst.t[:, 0:1]), [ss.r, epst.r], [ss.r])
                P.emit("dve", lambda e: e.reciprocal(out=ss.t[:], in_=ss.t[:]), [ss.r], [ss.r])
                P.emit("dve", lambda e: e.scalar_tensor_tensor(out=oc.t[:], in0=oc.t[:], scalar=ss.t[:, 0:1], in1=gsb.t[:], op0=ALU.mult, op1=ALU.mult),
                       [oc.r, ss.r, gsb.r], [oc.r])
                P.dma("sp", io["attn"][j * 128:(j + 1) * 128, 768 + hd * 128:768 + (hd + 1) * 128], oc.t[:], reads=[oc.r])
            return fin

        hcount = 0
        for h in range(6):
            kT, v_, qT, ka, qa = sel_head(io["kTf_all"], io["vf_all"], io["qTf"], h, aug=h)

            def qk_mm(ps, kt, jlo, jhi, kT=kT, qT=qT, ka=ka, qa=qa):
                n = (jhi - jlo) * 128
                P.emit("pe", lambda e: e.matmul(ps.t[:, 0:n], lhsT=kT.t[:, kt * 128:(kt + 1) * 128], rhs=qT.t[:, jlo * 128:jhi * 128],
                                                start=True, stop=False), [kT.r, qT.r], [ps.r])
                P.emit("pe", lambda e: e.matmul(ps.t[:, 0:n], lhsT=ka.t[:, kt * 128:(kt + 1) * 128], rhs=qa.t[:, jlo * 128:jhi * 128],
                                                start=False, stop=True), [ka.r, qa.r], [ps.r])
            add_map(qk_mm, v_, mF, True, fox_fin(h), hcount)
            hcount += 1
        for hd in range(4):
            kT, v_, qT, _, _ = sel_head(io["kTd_all"], io["vd_all"], io["qTd"], hd)
            for m in range(2):
                def qk_mm(ps, kt, jlo, jhi, kT=kT, qT=qT, m=m):
                    n = (jhi - jlo) * 128
                    P.emit("pe", lambda e: e.matmul(ps.t[:, 0:n], lhsT=kT.t[m * 64:(m + 1) * 64, kt * 128:(kt + 1) * 128],
                                                    rhs=qT.t[m * 64:(m + 1) * 64, jlo * 128:jhi * 128], start=True, stop=True), [kT.r, qT.r], [ps.r])
                add_map(qk_mm, v_, mD, False, diff_fin0() if m == 0 else diff_fin1(hd), hcount if m == 0 else None)
            hcount += 1

        def emit_qk(t):
            ps = pss.next()
            t["ps"] = ps
            t["qk"](ps, t["kt"], t["jlo"], t["jhi"])

        def emit_rest(t):
            ps, kt, G, jlo, jhi, v_, mask = t["ps"], t["kt"], t["G"], t["jlo"], t["jhi"], t["v"], t["mask"]
            n = (jhi - jlo) * 128
            hf = t["head_first"]
            if hf is not None and hf + 1 < len(head_loads):
                head_loads[hf + 1]()
            pT = pTs.next()
            diag = (jlo == G)
            if t["clamp"] and diag:
                P.emit("dve", lambda e: e.tensor_scalar_min(out=ps.t[:, 0:128], in0=ps.t[:, 0:128], scalar1=80.0), [ps.r], [ps.r])
            P.emit("act", lambda e: e.activation(out=pT.t[:, 0:n], in_=ps.t[:, 0:n], func=AF.Exp), [ps.r], [pT.r])
            if diag:
                r = kt - 8 * G
                P.emit("dve", lambda e: e.tensor_tensor(out=pT.t[:, 0:128], in0=pT.t[:, 0:128], in1=mask.t[:, r, :], op=ALU.mult), [pT.r, mask.r], [pT.r])
            while pending:
                pending.pop(0)()
            for jj in range(jlo, jhi):
                acc = accs[jj]
                c0 = (jj - jlo) * 128
                P.emit("pe", lambda e, acc=acc, c0=c0, jj=jj: e.matmul(acc.ap, lhsT=pT.t[:, c0:c0 + 128], rhs=v_.t[:, kt, :],
                                                                   start=(kt == 0), stop=(kt == 8 * jj + 7)), [pT.r, v_.r], [acc.r])
            if kt % 8 == 7 and diag:
                pending.append(lambda f=t["fin"], G=G: f(G))

        pending = []
        pending_prev = []
        head_loads[0]()
        for i in range(min(LOOK, len(tasks))):
            emit_qk(tasks[i])
        for i, t in enumerate(tasks):
            if i + LOOK < len(tasks):
                emit_qk(tasks[i + LOOK])
            emit_rest(t)
        for f_ in pending_prev + pending:
            f_()
        P.finalize()
    nc.all_engine_barrier()


def build_B(lam_init):
    nc = bass.Bass("TRN2", target_bir_lowering=False)
    io = dict(
        qTf=din(nc, "qTf", [768, TL], BF16), kTf_all=din(nc, "kTf_all", [768, SEQ], BF16), vf_all=din(nc, "vf_all", [SEQ, 768], BF16),
        logfT=din(nc, "logfT", [6, SEQ]), ownmask=din(nc, "ownmask", [6, SEQ]),
        maskF=din(nc, "maskF", [128, 8, 128], BF16), maskD=din(nc, "maskD", [128, 8, 128], BF16),
        qTd=din(nc, "qTd", [512, TL], BF16), kTd_all=din(nc, "kTd_all", [512, SEQ], BF16), vd_all=din(nc, "vd_all", [SEQ, 512], BF16),
        lam_q1=din(nc, "lam_q1", [64]), lam_k1=din(nc, "lam_k1", [64]), lam_q2=din(nc, "lam_q2", [64]), lam_k2=din(nc, "lam_k2", [64]),
        subln_g=din(nc, "subln_g", [128]),
        xr=din(nc, "xr", [128, SEQ + 3]), gr=din(nc, "gr", [128, SEQ]), conv_w=din(nc, "conv_w", [128, 4]), conv_b=din(nc, "conv_b", [128, 1]),
        w_a=din(nc, "w_a", [128, 128]), w_i=din(nc, "w_i", [128, 128]), b_a=din(nc, "b_a", [128, 1]), b_i=din(nc, "b_i", [128, 1]),
        lru_lam=din(nc, "lru_lam", [128, 1]),
        attn=dout(nc, "attn", [TL, 1280]), lru=dout(nc, "lru", [128, SEQ]),
    )
    phase_B(nc, io, lam_init)
    return nc


def global_order(parts, axis):
    full = np.concatenate(parts, axis=axis)
    perm = np.concatenate([own_rows(c) for c in range(NC)])
    inv = np.empty_like(perm)
    inv[perm] = np.arange(perm.size)
    return np.take(full, inv, axis=axis)


def core_masks(c):
    i = np.arange(128)
    tri = (i[:, None] <= i[None, :])
    blk = ((i[:, None] // 64) <= (i[None, :] // 64))
    mF = np.zeros((128, 8, 128), np.float32)
    mD = np.zeros((128, 8, 128), np.float32)
    for r in range(8):
        if r < c:
            mF[:, r, :] = 1.0
            mD[:, r, :] = 1.0
        elif r == c:
            mF[:, r, :] = tri
            mD[:, r, :] = blk
    om = np.zeros((6, SEQ), np.float32)
    om[:, own_rows(c)] = 1.0
    return mF.astype(ml_dtypes.bfloat16), mD.astype(ml_dtypes.bfloat16), om


def launch_B(resA, prm, l):
    lam_init = 0.8 - 0.6 * math.exp(-0.3 * l)
    nc = build_B(lam_init)
    kTf_all = global_order([r["kTf"] for r in resA], 1)
    vf_all = global_order([r["vf"] for r in resA], 0)
    kTd_all = global_order([r["kTd"] for r in resA], 1)
    vd_all = global_order([r["vd"] for r in resA], 0)
    logfT = np.ascontiguousarray(global_order([r["logf"] for r in resA], 0).T)
    xr_all = global_order([r["xrT"] for r in resA], 1)
    gr_all = global_order([r["grT"] for r in resA], 1)
    in_maps = []
    for c in range(NC):
        mF, mD, om = core_masks(c)
        n = c % 6
        sl = slice(n * 128, (n + 1) * 128)
        xr = np.zeros((128, SEQ + 3), np.float32)
        xr[:, 3:] = xr_all[sl]
        in_maps.append(dict(
            qTf=resA[c]["qTf"], kTf_all=kTf_all, vf_all=vf_all, logfT=logfT, ownmask=om, maskF=mF, maskD=mD,
            qTd=resA[c]["qTd"], kTd_all=kTd_all, vd_all=vd_all,
            lam_q1=prm["lam_q1"][l], lam_k1=prm["lam_k1"][l], lam_q2=prm["lam_q2"][l], lam_k2=prm["lam_k2"][l],
            subln_g=prm["subln_g"][l], xr=xr, gr=np.ascontiguousarray(gr_all[sl]),
            conv_w=np.ascontiguousarray(prm["conv_w"][l][:, sl].T), conv_b=np.ascontiguousarray(prm["conv_b"][l][sl, None]),
            w_a=np.ascontiguousarray(prm["w_a"][l][n]), w_i=np.ascontiguousarray(prm["w_i"][l][n]),
            b_a=np.ascontiguousarray(prm["b_a"][l][sl, None]), b_i=np.ascontiguousarray(prm["b_i"][l][sl, None]),
            lru_lam=np.ascontiguousarray(prm["lru_lambda"][l][sl, None])))
    return run(nc, in_maps)


def layer_norm_tiles(P, acc, gt, bt, epst, junk_ap, sm):
    for j in range(8):
        a = acc.t[:, j, :]
        mu, ss = sm.next(), sm.next()
        P.emit("dve", lambda e, a=a, mu=mu: e.reduce_sum(out=mu.t[:], in_=a, axis=AX.X), [acc.r], [mu.r])
        P.emit("dve", lambda e, mu=mu: e.tensor_scalar_mul(out=mu.t[:], in0=mu.t[:], scalar1=-1.0 / D), [mu.r], [mu.r])
        P.emit("dve", lambda e, a=a, mu=mu: e.tensor_scalar_add(out=a, in0=a, scalar1=mu.t[:, 0:1]), [acc.r, mu.r], [acc.r])
        P.emit("act", lambda e, a=a, ss=ss: e.activation(out=junk_ap[0], in_=a, func=AF.Square, accum_out=ss.t[:, 0:1]), [acc.r], [junk_ap[1], ss.r])
        P.emit("act", lambda e, ss=ss: e.activation(out=ss.t[:], in_=ss.t[:], func=AF.Sqrt, scale=1.0 / D, bias=epst.t[:, 0:1]), [ss.r, epst.r], [ss.r])
        P.emit("dve", lambda e, ss=ss: e.reciprocal(out=ss.t[:], in_=ss.t[:]), [ss.r], [ss.r])
        P.emit("dve", lambda e, a=a, ss=ss: e.scalar_tensor_tensor(out=a, in0=a, scalar=ss.t[:, 0:1], in1=gt.t[:], op0=ALU.mult, op1=ALU.mult),
               [acc.r, ss.r, gt.r], [acc.r])
        P.emit("dve", lambda e, a=a: e.tensor_tensor(out=a, in0=a, in1=bt.t[:], op=ALU.add), [acc.r, bt.r], [acc.r])


def phase_C(nc, io):
    with ExitStack() as st:
        P = Prog(nc, st)
        ident = P.tile([128, 128], F32, "ident")
        P.dma("sp", ident.t[:], io["ident"], writes=[ident.r])
        epst = P.tile([128, 1], F32, "eps")
        P.emit("dve", lambda e: e.memset(epst.t[:], LN_EPS), [], [epst.r])
        acc = P.tile([128, 8, D], F32, "acc")
        xT = P.tile([128, 16, TL], BF16, "xT")
        lng = P.tile([128, D], F32, "lng"); lnb = P.tile([128, D], F32, "lnb")
        hid = P.tile([128, 4, TL], BF16, "hid")
        junk_ap = (hid.t[:].rearrange("p a b -> p (a b)")[:, 0:D], hid.r)
        wgs = Ring([P.tile([128, 16, 256], BF16, "wg") for _ in range(2)])
        wus = Ring([P.tile([128, 16, 256], BF16, "wu") for _ in range(2)])
        wds = Ring([P.tile([128, 4, D], BF16, "wd") for _ in range(2)])
        psr = Ring([P.psum([128, 512], F32, "ps") for _ in range(7)])
        sm = Ring([P.tile([128, 1], F32, "sm") for _ in range(12)])
        gate = P.tile([128, 8, 32], F32, "gate")
        wgr = P.tile([128, 16, 36], F32, "wgr")
        bgr = P.tile([128, 36], F32, "bgr")
        sgs = Ring([P.tile([128, 512], F32, "sg") for _ in range(2)])
        P.dma("sp", wgr.t[:], io["w_gr"].rearrange("(kc p) n -> p kc n", p=128), writes=[wgr.r])
        P.dma("sp", bgr.t[:], io["b_gr"].partition_broadcast(128), writes=[bgr.r])
        P.dma("sp", lng.t[:], io["ln1_g"].partition_broadcast(128), writes=[lng.r])
        P.dma("sp", lnb.t[:], io["ln1_b"].partition_broadcast(128), writes=[lnb.r])

        att = Ring([P.tile([128, 512], F32, "att") for _ in range(2)])
        for j in range(8):
            P.dma("sp", acc.t[:, j, :], io["hres"][j * 128:(j + 1) * 128, :], writes=[acc.r])
            for (c0, n, d0) in ((0, 4, 0), (512, 2, 4), (768, 4, 12)):
                at = att.next()
                P.dma("sp", at.t[:, 0:n * 128], io["attn"][j * 128:(j + 1) * 128, c0:c0 + n * 128], writes=[at.r])
                ps = psr.next()
                for q in range(n):
                    P.emit("pe", lambda e, ps=ps, at=at, q=q: e.transpose(ps.t[:, q * 128:(q + 1) * 128], at.t[:, q * 128:(q + 1) * 128], ident.t[:]),
                           [at.r, ident.r], [ps.r])
                copy_op(P, evac_eng(), xT.t[:, d0:d0 + n, j * 128:(j + 1) * 128], ps.t[:, 0:n * 128].rearrange("p (a b) -> p a b", a=n), [ps.r], [xT.r])
        for b in range(6):
            P.dma("pool", xT.t[:, 6 + b, :], io["lruT"][b * 128:(b + 1) * 128, :], writes=[xT.r])

        wos = wgs.tiles + wus.tiles
        wi_ = 0
        for n in range(8):
            wt = wos[wi_ % len(wos)]; wi_ += 1
            P.dma("pool", wt.t[:], io["w_o"][:, n * 256:(n + 1) * 256].rearrange("(kc p) n -> p kc n", p=128), writes=[wt.r])
            for j in range(8):
                ps = psr.next()
                for kc in range(16):
                    P.emit("pe", lambda e, ps=ps, wt=wt, kc=kc, j=j: e.matmul(ps.t[:, 0:256], lhsT=xT.t[:, kc, j * 128:(j + 1) * 128], rhs=wt.t[:, kc, :],
                                                                            start=(kc == 0), stop=(kc == 15)), [xT.r, wt.r], [ps.r])
                a = acc.t[:, j, n * 256:(n + 1) * 256]
                P.emit("dve", lambda e, a=a, ps=ps: e.scalar_tensor_tensor(out=a, in0=a, scalar=float(ALPHA), in1=ps.t[:, 0:256], op0=ALU.mult, op1=ALU.add),
                       [acc.r, ps.r], [acc.r])
        if CSTOP >= 2:
            layer_norm_tiles(P, acc, lng, lnb, epst, junk_ap, sm)

        h1f = lng
        h1f_v = h1f.t[:].rearrange("p (a b) -> p a b", a=16)
        hlo = P.tile([128, 16, 128], BF16, "hlo"); whi = P.tile([128, 16, 36], BF16, "whi"); wlo = P.tile([128, 16, 36], BF16, "wlo")
        P.emit("dve", lambda e: e.tensor_copy(out=whi.t[:], in_=wgr.t[:]), [wgr.r], [whi.r])
        P.emit("dve", lambda e: e.tensor_tensor(out=wlo.t[:], in0=wgr.t[:], in1=whi.t[:], op=ALU.subtract), [wgr.r, whi.r], [wlo.r])
        lg = P.tile([128, 36], F32, "lg"); goh = P.tile([128, 4], F32, "goh"); ge = P.tile([128, 4], F32, "ge")
        esel = P.tile([128, 8], F32, "esel"); oh1 = P.tile([128, 8], F32, "oh1"); oh2 = P.tile([128, 8], F32, "oh2")
        msk = P.tile([128, 8], F32, "msk"); eg = P.tile([128, 8], F32, "eg")
        for j in range(8 if CSTOP >= 3.1 else 0):
            for g in range(4):
                ps = psr.next()
                for q in range(4):
                    kc = g * 4 + q
                    P.emit("pe", lambda e, ps=ps, q=q, kc=kc, j=j: e.transpose(ps.t[:, q * 128:(q + 1) * 128], acc.t[:, j, kc * 128:(kc + 1) * 128], ident.t[:]),
                           [acc.r, ident.r], [ps.r])
                P.emit("act", lambda e, ps=ps, g=g, j=j: e.activation(out=xT.t[:, g * 4:(g + 1) * 4, j * 128:(j + 1) * 128],
                                                                    in_=ps.t[:].rearrange("p (a b) -> p a b", a=4), func=AF.Copy), [ps.r], [xT.r])
                ps2 = psr.next()
                for q in range(4):
                    kc = g * 4 + q
                    P.emit("pe", lambda e, ps2=ps2, q=q, kc=kc, j=j: e.transpose(ps2.t[:, q * 128:(q + 1) * 128], acc.t[:, j, kc * 128:(kc + 1) * 128], ident.t[:]),
                           [acc.r, ident.r], [ps2.r])
                P.emit("dve", lambda e, ps2=ps2, g=g: e.tensor_copy(out=h1f_v[:, g * 4:(g + 1) * 4, :], in_=ps2.t[:].rearrange("p (a b) -> p a b", a=4)),
                       [ps2.r], [h1f.r])
            if CSTOP < 3.2:
                continue
            P.emit("dve", lambda e, j=j: e.tensor_tensor(out=hlo.t[:], in0=h1f_v, in1=xT.t[:, :, j * 128:(j + 1) * 128], op=ALU.subtract), [h1f.r, xT.r], [hlo.r])
            ps = psr.next()
            for kc in range(16):
                for ti, (lt_, lr_, rt_, rr_) in enumerate(((xT.t[:, kc, j * 128:(j + 1) * 128], xT.r, whi.t[:, kc, :], whi.r),
                                                           (xT.t[:, kc, j * 128:(j + 1) * 128], xT.r, wlo.t[:, kc, :], wlo.r),
                                                           (hlo.t[:, kc, :], hlo.r, whi.t[:, kc, :], whi.r))):
                    P.emit("pe", lambda e, ps=ps, lt_=lt_, rt_=rt_, kc=kc, ti=ti: e.matmul(ps.t[:, 0:36], lhsT=lt_, rhs=rt_, start=(kc == 0 and ti == 0), stop=(kc == 15 and ti == 2)),
                           [lr_, rr_], [ps.r])
            P.emit("dve", lambda e, ps=ps: e.tensor_tensor(out=lg.t[:], in0=ps.t[:, 0:36], in1=bgr.t[:], op=ALU.add), [ps.r, bgr.r], [lg.r])
            if CSTOP < 3.3:
                continue
            gmax, ngmax, gsum, m1, m2, dd, w1, w2 = [sm.next() for _ in range(8)]
            P.emit("dve", lambda e, gmax=gmax: e.reduce_max(out=gmax.t[:], in_=lg.t[:, 0:4], axis=AX.X), [lg.r], [gmax.r])
            P.emit("dve", lambda e, gmax=gmax: e.tensor_scalar(out=goh.t[:], in0=lg.t[:, 0:4], scalar1=gmax.t[:, 0:1], scalar2=None, op0=ALU.is_equal), [lg.r, gmax.r], [goh.r])
            P.emit("dve", lambda e, gmax=gmax, ngmax=ngmax: e.tensor_scalar_mul(out=ngmax.t[:], in0=gmax.t[:], scalar1=-1.0), [gmax.r], [ngmax.r])
            P.emit("act", lambda e, ngmax=ngmax, gsum=gsum: e.activation(out=ge.t[:], in_=lg.t[:, 0:4], func=AF.Exp, bias=ngmax.t[:, 0:1], accum_out=gsum.t[:, 0:1]),
                   [lg.r, ngmax.r], [ge.r, gsum.r])
            P.emit("dve", lambda e, gsum=gsum: e.reciprocal(out=gsum.t[:], in_=gsum.t[:]), [gsum.r], [gsum.r])
            if CSTOP < 3.4:
                continue
            P.emit("dve", lambda e: e.tensor_scalar_mul(out=esel.t[:], in0=lg.t[:, 4:12], scalar1=goh.t[:, 0:1]), [lg.r, goh.r], [esel.r])
            for g in range(1, 4):
                P.emit("dve", lambda e, g=g: e.scalar_tensor_tensor(out=esel.t[:], in0=lg.t[:, 4 + 8 * g:12 + 8 * g], scalar=goh.t[:, g:g + 1], in1=esel.t[:],
                                                                    op0=ALU.mult, op1=ALU.add), [lg.r, goh.r, esel.r], [esel.r])
            P.emit("dve", lambda e, m1=m1: e.reduce_max(out=m1.t[:], in_=esel.t[:], axis=AX.X), [esel.r], [m1.r])
            P.emit("dve", lambda e, m1=m1: e.tensor_scalar(out=oh1.t[:], in0=esel.t[:], scalar1=m1.t[:, 0:1], scalar2=None, op0=ALU.is_equal), [esel.r, m1.r], [oh1.r])
            P.emit("dve", lambda e: e.scalar_tensor_tensor(out=msk.t[:], in0=oh1.t[:], scalar=-1e30, in1=esel.t[:], op0=ALU.mult, op1=ALU.add), [oh1.r, esel.r], [msk.r])
            P.emit("dve", lambda e, m2=m2: e.reduce_max(out=m2.t[:], in_=msk.t[:], axis=AX.X), [msk.r], [m2.r])
            P.emit("dve", lambda e, m2=m2: e.tensor_scalar(out=oh2.t[:], in0=msk.t[:], scalar1=m2.t[:, 0:1], scalar2=None, op0=ALU.is_equal), [msk.r, m2.r], [oh2.r])
            if CSTOP < 3.5:
                continue
            P.emit("dve", lambda e, m1=m1, m2=m2, dd=dd: e.tensor_tensor(out=dd.t[:], in0=m2.t[:], in1=m1.t[:], op=ALU.subtract), [m1.r, m2.r], [dd.r])
            P.emit("act", lambda e, dd=dd: e.activation(out=dd.t[:], in_=dd.t[:], func=AF.Exp), [dd.r], [dd.r])
            P.emit("dve", lambda e, dd=dd, w1=w1: e.tensor_scalar_add(out=w1.t[:], in0=dd.t[:], scalar1=1.0), [dd.r], [w1.r])
            P.emit("dve", lambda e, w1=w1: e.reciprocal(out=w1.t[:], in_=w1.t[:]), [w1.r], [w1.r])
            P.emit("dve", lambda e, w1=w1, w2=w2, dd=dd: e.tensor_tensor(out=w2.t[:], in0=dd.t[:], in1=w1.t[:], op=ALU.mult), [dd.r, w1.r], [w2.r])
            P.emit("dve", lambda e, w1=w1, gsum=gsum: e.tensor_tensor(out=w1.t[:], in0=w1.t[:], in1=gsum.t[:], op=ALU.mult), [w1.r, gsum.r], [w1.r])
            P.emit("dve", lambda e, w2=w2, gsum=gsum: e.tensor_tensor(out=w2.t[:], in0=w2.t[:], in1=gsum.t[:], op=ALU.mult), [w2.r, gsum.r], [w2.r])
            P.emit("dve", lambda e, w1=w1: e.tensor_scalar_mul(out=eg.t[:], in0=oh1.t[:], scalar1=w1.t[:, 0:1]), [oh1.r, w1.r], [eg.r])
            P.emit("dve", lambda e, w2=w2: e.scalar_tensor_tensor(out=eg.t[:], in0=oh2.t[:], scalar=w2.t[:, 0:1], in1=eg.t[:], op0=ALU.mult, op1=ALU.add),
                   [oh2.r, w2.r, eg.r], [eg.r])
            for g in range(4):
                P.emit("dve", lambda e, g=g, j=j: e.tensor_scalar_mul(out=gate.t[:, j, g * 8:(g + 1) * 8], in0=eg.t[:], scalar1=goh.t[:, g:g + 1]), [eg.r, goh.r], [gate.r])
        for j in range(8 if CSTOP >= 3 else 0):
            P.emit("dve", lambda e, j=j: e.tensor_scalar_mul(out=acc.t[:, j, :], in0=acc.t[:, j, :], scalar1=float(ALPHA)), [acc.r], [acc.r])
        if CSTOP >= 3.02:
            P.dma("sp", lng.t[:], io["ln2_g"].partition_broadcast(128), reads=[h1f.r], writes=[lng.r])
            P.dma("sp", lnb.t[:], io["ln2_b"].partition_broadcast(128), writes=[lnb.r])

        for ex in range(NEXP if CSTOP >= 4 else 0):
            wd = wds.next()
            P.dma("pool", wd.t[:], io["w_down"][ex].rearrange("(fc p) d -> p fc d", p=128), writes=[wd.r])
            for fh in range(2):
                wg, wu = wgs.next(), wus.next()
                P.dma("pool", wg.t[:], io["w_gate"][ex][:, fh * 256:(fh + 1) * 256].rearrange("(kc p) f -> p kc f", p=128), writes=[wg.r])
                P.dma("pool", wu.t[:], io["w_up"][ex][:, fh * 256:(fh + 1) * 256].rearrange("(kc p) f -> p kc f", p=128), writes=[wu.r])
                for fc in range(2):
                    for th in range(2):
                        pg, pu = psr.next(), psr.next()
                        for (wt, ps) in ((wg, pg), (wu, pu)):
                            for kc in range(16):
                                P.emit("pe", lambda e, ps=ps, wt=wt, kc=kc, fc=fc, th=th: e.matmul(
                                    ps.t[:], lhsT=wt.t[:, kc, fc * 128:(fc + 1) * 128], rhs=xT.t[:, kc, th * 512:(th + 1) * 512],
                                    start=(kc == 0), stop=(kc == 15)), [wt.r, xT.r], [ps.r])
                        sg = sgs.next()
                        P.emit("act", lambda e, pg=pg, sg=sg: e.activation(out=sg.t[:], in_=pg.t[:], func=AF.Silu), [pg.r], [sg.r])
                        P.emit("dve", lambda e, pu=pu, sg=sg, fh=fh, fc=fc, th=th: e.tensor_tensor(out=hid.t[:, fh * 2 + fc, th * 512:(th + 1) * 512], in0=sg.t[:], in1=pu.t[:], op=ALU.mult),
                               [sg.r, pu.r], [hid.r])
            for j in range(8):
                for n in range(4):
                    ps = psr.next()
                    for fc in range(4):
                        P.emit("pe", lambda e, ps=ps, fc=fc, j=j, n=n, wd=wd: e.matmul(ps.t[:], lhsT=hid.t[:, fc, j * 128:(j + 1) * 128], rhs=wd.t[:, fc, n * 512:(n + 1) * 512],
                                                                                   start=(fc == 0), stop=(fc == 3)), [hid.r, wd.r], [ps.r])
                    a = acc.t[:, j, n * 512:(n + 1) * 512]
                    P.emit("dve", lambda e, a=a, ps=ps, j=j, ex=ex: e.scalar_tensor_tensor(out=a, in0=ps.t[:], scalar=gate.t[:, j, ex:ex + 1], in1=a, op0=ALU.mult, op1=ALU.add),
                           [ps.r, gate.r, acc.r], [acc.r])
        if CSTOP >= 5:
            layer_norm_tiles(P, acc, lng, lnb, epst, junk_ap, sm)
        for j in range(8):
            P.dma("sp", io["hout"][j * 128:(j + 1) * 128, :], acc.t[:, j, :], reads=[acc.r])
        P.finalize()
    nc.all_engine_barrier()


def build_C():
    nc = bass.Bass("TRN2", target_bir_lowering=False)
    io = dict(
        hres=din(nc, "hres", [TL, D]), attn=din(nc, "attn", [TL, 1280]), lruT=din(nc, "lruT", [768, TL]),
        w_o=din(nc, "w_o", [D, D]), ln1_g=din(nc, "ln1_g", [D]), ln1_b=din(nc, "ln1_b", [D]),
        w_gr=din(nc, "w_gr", [D, 36]), b_gr=din(nc, "b_gr", [36]),
        w_gate=din(nc, "w_gate", [NEXP, D, 512]), w_up=din(nc, "w_up", [NEXP, D, 512]), w_down=din(nc, "w_down", [NEXP, 512, D]),
        ln2_g=din(nc, "ln2_g", [D]), ln2_b=din(nc, "ln2_b", [D]), ident=din(nc, "ident", [128, 128]),
        hout=dout(nc, "hout", [TL, D]),
    )
    phase_C(nc, io)
    return nc


def launch_C(h_shards, resB, prm, l):
    nc = build_C()
    lru_all = np.concatenate([resB[n]["lru"] for n in range(6)], axis=0)
    w_gr = np.ascontiguousarray(np.concatenate([prm["w_group"][l], prm["w_router"][l]], axis=1))
    b_gr = np.ascontiguousarray(np.concatenate([prm["b_group"][l], prm["b_router"][l]], axis=0))
    wg = prm["w_gate"][l].reshape(32, D, 512)[:NEXP]
    wu = prm["w_up"][l].reshape(32, D, 512)[:NEXP]
    wdn = prm["w_down"][l].reshape(32, 512, D)[:NEXP]
    in_maps = []
    for c in range(NC):
        in_maps.append(dict(
            hres=h_shards[c], attn=resB[c]["attn"], lruT=np.ascontiguousarray(lru_all[:, own_rows(c)]),
            w_o=prm["w_o"][l], ln1_g=prm["ln1_g"][l], ln1_b=prm["ln1_b"][l], w_gr=w_gr, b_gr=b_gr,
            w_gate=wg, w_up=wu, w_down=wdn, ln2_g=prm["ln2_g"][l], ln2_b=prm["ln2_b"][l], ident=IDENT))
    return run(nc, in_maps)


def kernel(**inputs):
    prm = {k: np.asarray(v) for k, v in inputs.items()}
    x = prm["x"][0]
    pos = prm["positions"][0]
    h_shards = [np.ascontiguousarray(x[own_rows(c)]) for c in range(NC)]
    for l in range(2):
        resA = launch_A(h_shards, prm["w_in"][l], prm["b_f"][l], pos)
        resB = launch_B(resA, prm, l)
        resC = launch_C(h_shards, resB, prm, l)
        h_shards = [np.ascontiguousarray(r["hout"]) for r in resC]
    out = np.empty((1, SEQ, D), np.float32)
    for c in range(NC):
        out[0, own_rows(c)] = h_shards[c]
    return out
```

```python
import math
from contextlib import ExitStack
import numpy as np
import ml_dtypes
import concourse.bass as bass
import concourse.mybir as mybir
from concourse.bass_utils import run_bass_kernel_spmd

F32 = mybir.dt.float32
BF16 = mybir.dt.bfloat16
I32 = mybir.dt.int32
AF = mybir.ActivationFunctionType
ALU = mybir.AluOpType
AX = mybir.AxisListType

NC = 8
SEQ = 8192
D = 2048
TL = 1024
N_IN = 5382
ALPHA = (2.0 * 2) ** 0.25
LN_EPS = 1e-5
PI = math.pi

SAME_ENGINE_SYNC = True
NEXP = 32
CSTOP = 9
LOOK = 3
PVLAG = 2
NDUMMY = 0


class Res:
    __slots__ = ("last_w", "readers")

    def __init__(self):
        self.last_w = None
        self.readers = []


class Tile:
    __slots__ = ("t", "r")

    def __init__(self, t):
        self.t = t
        self.r = Res()


class Op:
    __slots__ = ("eng", "fn", "deps", "sig", "is_dma", "dsem", "dtarget", "has_dep", "waits")

    def __init__(self, eng, fn, is_dma):
        self.eng = eng
        self.fn = fn
        self.deps = []
        self.sig = 0
        self.is_dma = is_dma
        self.dsem = None
        self.dtarget = 0
        self.has_dep = False
        self.waits = []


class Prog:
    ENGS = ("pe", "act", "dve", "pool", "sp")

    def __init__(self, nc, st, n_dma_sems=8):
        self.nc = nc
        self.st = st
        self.ops = []
        self.n_dma_sems = n_dma_sems
        self._n = 0

    def tile(self, shape, dtype, name=None):
        self._n += 1
        return Tile(self.st.enter_context(self.nc.sbuf_tensor("%s_%d" % (name or "t", self._n), shape, dtype)))

    def psum(self, shape, dtype=F32, name=None):
        self._n += 1
        return Tile(self.st.enter_context(self.nc.psum_tensor("%s_%d" % (name or "p", self._n), shape, dtype)))

    def emit(self, eng, fn, reads=(), writes=(), is_dma=False):
        op = Op(eng, fn, is_dma)
        deps = {}
        for r in reads:
            if r.last_w is not None:
                deps[id(r.last_w)] = r.last_w
        for w in writes:
            if w.last_w is not None:
                deps[id(w.last_w)] = w.last_w
            for rd in w.readers:
                deps[id(rd)] = rd
        op.deps = list(deps.values())
        for d in op.deps:
            d.has_dep = True
        for r in reads:
            r.readers.append(op)
        for w in writes:
            w.last_w = op
            w.readers = []
        self.ops.append(op)
        return op

    def dma(self, eng, out, in_, reads=(), writes=()):
        return self.emit(eng, lambda e: e.dma_start(out=out, in_=in_), reads, writes, is_dma=True)

    def finalize(self):
        nc, st = self.nc, self.st
        esem = {e: st.enter_context(nc.semaphore("s_" + e)) for e in self.ENGS}
        dsems = {q: [st.enter_context(nc.semaphore("d_%s%d" % (q, i))) for i in range(self.n_dma_sems)]
                 for q in ("sp", "pool")}
        dcount = {q: [0] * self.n_dma_sems for q in dsems}
        drr = {q: 0 for q in dsems}
        ecount = {e: 0 for e in self.ENGS}
        known = {e: {} for e in self.ENGS}
        per_eng = {e: [] for e in self.ENGS}
        for op in self.ops:
            waits = {}
            k = known[op.eng]

            def need(sem, val):
                key = id(sem)
                if k.get(key, 0) >= val:
                    return
                if key not in waits or waits[key][1] < val:
                    waits[key] = (sem, val)

            for d in op.deps:
                if d.is_dma:
                    need(d.dsem, d.dtarget)
                else:
                    if d.eng == op.eng and (d.eng == "pe" or not SAME_ENGINE_SYNC):
                        continue
                    need(esem[d.eng], d.sig)
            if op.is_dma:
                q = op.eng
                i = drr[q]
                drr[q] = (i + 1) % self.n_dma_sems
                sem = dsems[q][i]
                if dcount[q][i] > 0:
                    need(sem, dcount[q][i] * 16)
                dcount[q][i] += 1
                op.dsem = sem
                op.dtarget = dcount[q][i] * 16
            elif op.has_dep:
                ecount[op.eng] += 1
                op.sig = ecount[op.eng]
            op.waits = list(waits.values())
            for sem, val in op.waits:
                k[id(sem)] = val
            per_eng[op.eng].append(op)
        fin = []
        for q in dsems:
            for i, sem in enumerate(dsems[q]):
                if dcount[q][i] > 0:
                    fin.append((sem, dcount[q][i] * 16))
        for e in self.ENGS:
            if ecount[e] > 0:
                fin.append((esem[e], ecount[e]))
        block = st.enter_context(nc.Block())

        def replay(name, last=False):
            def body(e):
                for op in per_eng[name]:
                    for sem, val in op.waits:
                        e.wait_ge(sem, val)
                    inst = op.fn(e)
                    if op.is_dma:
                        inst.then_inc(op.dsem, 16)
                    elif op.sig:
                        inst.then_inc(esem[name], 1)
                if last:
                    for sem, val in fin:
                        e.wait_ge(sem, val)
            return body

        block.tensor(replay("pe"))
        block.scalar(replay("act"))
        block.vector(replay("dve"))
        block.gpsimd(replay("pool"))
        block.sync(replay("sp", True))


class Ring:
    def __init__(self, tiles):
        self.tiles = tiles
        self.i = 0

    def next(self):
        t = self.tiles[self.i % len(self.tiles)]
        self.i += 1
        return t


def din(nc, name, shape, dt=F32):
    return nc.dram_tensor(name, list(shape), dt, kind="ExternalInput").ap()


def dout(nc, name, shape, dt=F32):
    return nc.dram_tensor(name, list(shape), dt, kind="ExternalOutput").ap()


_ev = [0]


def evac_eng():
    _ev[0] += 1
    return "act" if _ev[0] % 2 else "dve"


def copy_op(P, eng, out, in_, reads, writes, scale=None):
    if eng == "act":
        if scale is None:
            P.emit("act", lambda e: e.activation(out=out, in_=in_, func=AF.Copy), reads, writes)
        else:
            P.emit("act", lambda e: e.activation(out=out, in_=in_, func=AF.Copy, scale=float(scale)), reads, writes)
    else:
        if scale is None:
            P.emit("dve", lambda e: e.tensor_copy(out=out, in_=in_), reads, writes)
        else:
            P.emit("dve", lambda e: e.tensor_scalar_mul(out=out, in0=in_, scalar1=float(scale)), reads, writes)


def transpose_to_xT(P, src_ap_fn, ident, xT, nchunks=16):
    hts = Ring([P.tile([128, nchunks * 128], F32, "ht") for _ in range(2)])
    pst = Ring([P.psum([128, 512], F32, "pst") for _ in range(2)])
    for j in range(8):
        ht = hts.next()
        P.dma("sp", ht.t[:], src_ap_fn(j), writes=[ht.r])
        for g in range(nchunks // 4):
            ps = pst.next()
            for q in range(4):
                kc = g * 4 + q
                P.emit("pe", lambda e, ps=ps, q=q, ht=ht, kc=kc: e.transpose(
                    ps.t[:, q * 128:(q + 1) * 128], ht.t[:, kc * 128:(kc + 1) * 128], ident.t[:]),
                    reads=[ht.r, ident.r], writes=[ps.r])
            copy_op(P, evac_eng(), xT.t[:, g * 4:(g + 1) * 4, j * 128:(j + 1) * 128],
                    ps.t[:].rearrange("p (a b) -> p a b", a=4), [ps.r], [xT.r])


def phase_A(nc, io):
    with ExitStack() as st:
        P = Prog(nc, st)
        ident = P.tile([128, 128], F32, "ident")
        P.dma("sp", ident.t[:], io["ident"], writes=[ident.r])
        xT = P.tile([128, 16, TL], BF16, "xT")
        transpose_to_xT(P, lambda j: io["hin"][j * 128:(j + 1) * 128, :], ident, xT)

        posi = P.tile([128, 8], I32, "posi")
        posf = P.tile([128, 8], F32, "posf")
        invf = P.tile([128, 8], F32, "invf")
        ang = P.tile([128, 8, 8], F32, "ang")
        red = P.tile([128, 8, 8], F32, "red")
        cost = P.tile([128, 8, 8], F32, "cos")
        sint = P.tile([128, 8, 8], F32, "sin")
        bfb = P.tile([128, 6], F32, "bfb")
        P.dma("sp", posi.t[:], io["pos"], writes=[posi.r])
        P.dma("sp", invf.t[:], io["invf"], writes=[invf.r])
        P.dma("sp", bfb.t[:], io["b_f"].partition_broadcast(128), writes=[bfb.r])
        P.emit("dve", lambda e: e.tensor_copy(out=posf.t[:], in_=posi.t[:]), [posi.r], [posf.r])
        P.emit("dve", lambda e: e.tensor_tensor(out=ang.t[:], in0=posf.t[:].unsqueeze(2).to_broadcast([128, 8, 8]),
                                                in1=invf.t[:].unsqueeze(1).to_broadcast([128, 8, 8]), op=ALU.mult),
               [posf.r, invf.r], [ang.r])
        angi = P.tile([128, 8, 8], I32, "angi")
        angf = P.tile([128, 8, 8], F32, "angf")
        ang2 = P.tile([128, 8, 8], F32, "ang2")
        for (shift, dst) in ((0.0, sint), (0.5 * PI, cost)):
            P.emit("dve", lambda e, shift=shift: e.tensor_scalar_add(out=ang2.t[:], in0=ang.t[:], scalar1=float(shift)), [ang.r], [ang2.r])
            P.emit("dve", lambda e: e.tensor_scalar_mul(out=red.t[:], in0=ang2.t[:], scalar1=float(1.0 / (2 * PI))), [ang2.r], [red.r])
            P.emit("dve", lambda e: e.tensor_copy(out=angi.t[:], in_=red.t[:]), [red.r], [angi.r])
            P.emit("dve", lambda e: e.tensor_copy(out=angf.t[:], in_=angi.t[:]), [angi.r], [angf.r])
            P.emit("dve", lambda e: e.scalar_tensor_tensor(out=red.t[:], in0=angf.t[:], scalar=float(-2 * PI), in1=ang2.t[:], op0=ALU.mult, op1=ALU.add),
                   [angf.r, ang2.r], [red.r])
            P.emit("dve", lambda e: e.tensor_single_scalar(out=angf.t[:], in_=red.t[:], scalar=float(PI), op=ALU.is_gt), [red.r], [angf.r])
            P.emit("dve", lambda e: e.scalar_tensor_tensor(out=red.t[:], in0=angf.t[:], scalar=float(-2 * PI), in1=red.t[:], op0=ALU.mult, op1=ALU.add),
                   [angf.r, red.r], [red.r])
            P.emit("dve", lambda e: e.tensor_single_scalar(out=angf.t[:], in_=red.t[:], scalar=float(-PI), op=ALU.is_lt), [red.r], [angf.r])
            P.emit("dve", lambda e: e.scalar_tensor_tensor(out=red.t[:], in0=angf.t[:], scalar=float(2 * PI), in1=red.t[:], op0=ALU.mult, op1=ALU.add),
                   [angf.r, red.r], [red.r])
            P.emit("act", lambda e, dst=dst: e.activation(out=dst.t[:], in_=red.t[:], func=AF.Sin), [red.r], [dst.r])

        wbufs = Ring([P.tile([128, 16, 512], BF16, "w") for _ in range(3)])
        psA = Ring([P.psum([128, 512], F32, "psA") for _ in range(3)])

        def load_w(c0, ncols):
            wb = wbufs.next()
            P.dma("pool", wb.t[:, :, 0:ncols], io["w_in"][:, c0:c0 + ncols].rearrange("(kc p) n -> p kc n", p=128),
                  writes=[wb.r])
            return wb

        otf = Ring([P.tile([128, TL], F32, "otf") for _ in range(2)])
        otb = Ring([P.tile([128, TL], BF16, "otb") for _ in range(2)])

        def feat_chunk(c0, nblk, out_ap, scale, is_bf):
            wb = load_w(c0, nblk * 128)
            for b in range(nblk):
                ot = (otb if is_bf else otf).next()
                for th in range(2):
                    ps = psA.next()
                    for kc in range(16):
                        P.emit("pe", lambda e, ps=ps, wb=wb, kc=kc, b=b, th=th: e.matmul(
                            ps.t[:], lhsT=wb.t[:, kc, b * 128:(b + 1) * 128], rhs=xT.t[:, kc, th * 512:(th + 1) * 512],
                            start=(kc == 0), stop=(kc == 15)), reads=[wb.r, xT.r], writes=[ps.r])
                    copy_op(P, evac_eng(), ot.t[:, th * 512:(th + 1) * 512], ps.t[:], [ps.r], [ot.r], scale)
                P.dma("sp", out_ap(b), ot.t[:], reads=[ot.r])

        def tok_chunk(c0, ncols, handler):
            wb = load_w(c0, ncols)
            for j in range(8):
                ps = psA.next()
                for kc in range(16):
                    P.emit("pe", lambda e, ps=ps, wb=wb, kc=kc, j=j: e.matmul(
                        ps.t[:, 0:ncols], lhsT=xT.t[:, kc, j * 128:(j + 1) * 128], rhs=wb.t[:, kc, 0:ncols],
                        start=(kc == 0), stop=(kc == 15)), reads=[wb.r, xT.r], writes=[ps.r])
                handler(j, ps)

        feat_chunk(0, 4, lambda b: io["qTf"][b * 128:(b + 1) * 128, :], 128 ** -0.5, True)
        feat_chunk(512, 2, lambda b: io["qTf"][(4 + b) * 128:(5 + b) * 128, :], 128 ** -0.5, True)
        feat_chunk(768, 4, lambda b: io["kTf"][b * 128:(b + 1) * 128, :], None, True)
        feat_chunk(1280, 2, lambda b: io["kTf"][(4 + b) * 128:(5 + b) * 128, :], None, True)
        feat_chunk(2310, 4, lambda b: io["xrT"][b * 128:(b + 1) * 128, :], None, False)
        feat_chunk(2310 + 512, 2, lambda b: io["xrT"][(4 + b) * 128:(5 + b) * 128, :], None, False)
        feat_chunk(3078, 4, lambda b: io["grT"][b * 128:(b + 1) * 128, :], None, False)
        feat_chunk(3078 + 512, 2, lambda b: io["grT"][(4 + b) * 128:(5 + b) * 128, :], None, False)

        vts = Ring([P.tile([128, 512], BF16, "vt") for _ in range(2)])

        def h_v(key, col0, ncols):
            def h(j, ps):
                vt = vts.next()
                copy_op(P, evac_eng(), vt.t[:, 0:ncols], ps.t[:, 0:ncols], [ps.r], [vt.r])
                P.dma("sp", io[key][j * 128:(j + 1) * 128, col0:col0 + ncols], vt.t[:, 0:ncols], reads=[vt.r])
            return h

        tok_chunk(1536, 512, h_v("vf", 0, 512))
        logf = P.tile([128, 8, 6], F32, "logf")
        zt = P.tile([128, 6], F32, "zt")
        h_v2 = h_v("vf", 512, 256)

        def h_vfa(j, ps):
            P.emit("dve", lambda e: e.tensor_tensor(out=zt.t[:], in0=ps.t[:, 256:262], in1=bfb.t[:], op=ALU.add),
                   [ps.r, bfb.r], [zt.r])
            P.emit("act", lambda e: e.activation(out=zt.t[:], in_=zt.t[:], func=AF.Exp, scale=-1.0), [zt.r], [zt.r])
            P.emit("act", lambda e: e.activation(out=zt.t[:], in_=zt.t[:], func=AF.Ln, bias=1.0), [zt.r], [zt.r])
            P.emit("dve", lambda e: e.tensor_scalar_mul(out=logf.t[:, j, :], in0=zt.t[:], scalar1=-1.0), [zt.r], [logf.r])
            h_v2(j, ps)

        tok_chunk(2048, 262, h_vfa)
        P.dma("sp", io["logf"].rearrange("(j p) h -> p j h", p=128), logf.t[:], reads=[logf.r])

        qk = P.tile([128, 512], F32, "qk")
        ta, tb, tc_, td = [P.tile([128, 8, 8], F32, "rt") for _ in range(4)]
        pstr = Ring([P.psum([128, 512], F32, "pstr") for _ in range(2)])

        def h_qk(key, scale):
            qTd = P.tile([128, 4, TL], BF16, "qTd")

            def h(j, ps):
                copy_op(P, "act", qk.t[:], ps.t[:], [ps.r], [qk.r], scale)
                v3 = qk.t[:].rearrange("p (s d) -> p s d", s=8)
                t1, t2 = v3[:, :, 0:8], v3[:, :, 8:16]
                cb = cost.t[:, j, :].unsqueeze(1).to_broadcast([128, 8, 8])
                sb = sint.t[:, j, :].unsqueeze(1).to_broadcast([128, 8, 8])
                rd = [qk.r, cost.r, sint.r]
                P.emit("dve", lambda e: e.tensor_tensor(out=ta.t[:], in0=t1, in1=cb, op=ALU.mult), rd, [ta.r])
                P.emit("dve", lambda e: e.tensor_tensor(out=tb.t[:], in0=t2, in1=sb, op=ALU.mult), rd, [tb.r])
                P.emit("dve", lambda e: e.tensor_tensor(out=tc_.t[:], in0=t2, in1=cb, op=ALU.mult), rd, [tc_.r])
                P.emit("dve", lambda e: e.tensor_tensor(out=td.t[:], in0=t1, in1=sb, op=ALU.mult), rd, [td.r])
                P.emit("dve", lambda e: e.tensor_tensor(out=t1, in0=ta.t[:], in1=tb.t[:], op=ALU.subtract), [ta.r, tb.r], [qk.r])
                P.emit("dve", lambda e: e.tensor_tensor(out=t2, in0=tc_.t[:], in1=td.t[:], op=ALU.add), [tc_.r, td.r], [qk.r])
                pt = pstr.next()
                for b in range(4):
                    P.emit("pe", lambda e, b=b, pt=pt: e.transpose(pt.t[:, b * 128:(b + 1) * 128], qk.t[:, b * 128:(b + 1) * 128], ident.t[:]),
                           [qk.r, ident.r], [pt.r])
                copy_op(P, evac_eng(), qTd.t[:, :, j * 128:(j + 1) * 128], pt.t[:].rearrange("p (a b) -> p a b", a=4), [pt.r], [qTd.r])
                if j == 7:
                    P.dma("sp", io[key].rearrange("(b p) t -> p b t", p=128), qTd.t[:], reads=[qTd.r])
            return h

        tok_chunk(3846, 512, h_qk("qTd", 64 ** -0.5))
        tok_chunk(4358, 512, h_qk("kTd", None))
        tok_chunk(4870, 512, h_v("vd", 0, 512))
        P.finalize()
    nc.all_engine_barrier()


def build_A():
    nc = bass.Bass("TRN2", target_bir_lowering=False)
    io = dict(
        hin=din(nc, "hin", [TL, D]), w_in=din(nc, "w_in", [D, N_IN]), b_f=din(nc, "b_f", [6]),
        pos=din(nc, "pos", [128, 8], I32), invf=din(nc, "invf", [128, 8]), ident=din(nc, "ident", [128, 128]),
        qTf=dout(nc, "qTf", [768, TL], BF16), kTf=dout(nc, "kTf", [768, TL], BF16), vf=dout(nc, "vf", [TL, 768], BF16),
        logf=dout(nc, "logf", [TL, 6]), xrT=dout(nc, "xrT", [768, TL]), grT=dout(nc, "grT", [768, TL]),
        qTd=dout(nc, "qTd", [512, TL], BF16), kTd=dout(nc, "kTd", [512, TL], BF16), vd=dout(nc, "vd", [TL, 512], BF16),
    )
    phase_A(nc, io)
    return nc


def own_rows(c):
    return np.concatenate([np.arange((8 * j + c) * 128, (8 * j + c + 1) * 128) for j in range(8)])


IDENT = np.eye(128, dtype=np.float32)
INVF = np.broadcast_to((np.float32(500000.0) ** (-np.arange(8, dtype=np.float32) * np.float32(2.0) / np.float32(16))).astype(np.float32), (128, 8)).copy()


def run(nc, in_maps):
    res = run_bass_kernel_spmd(nc, in_maps, core_ids=list(range(NC)))
    return res.results


def launch_A(h_shards, w_in_l, b_f_l, positions):
    nc = build_A()
    in_maps = []
    for c in range(NC):
        in_maps.append(dict(hin=h_shards[c], w_in=w_in_l, b_f=b_f_l,
                            pos=np.ascontiguousarray(positions[own_rows(c)].reshape(8, 128).T), invf=INVF, ident=IDENT))
    return run(nc, in_maps)


def split3(P, src, cur, back, n, npart, sign):
    outs = []
    P.emit("dve", lambda e: e.tensor_scalar_mul(out=cur.t[:], in0=src.t[:], scalar1=float(sign)), [src.r], [cur.r])
    for i in range(3):
        o = P.tile([npart, n], BF16, "s3o")
        P.emit("dve", lambda e, o=o: e.tensor_copy(out=o.t[:], in_=cur.t[:]), [cur.r], [o.r])
        if i < 2:
            P.emit("dve", lambda e, o=o: e.tensor_copy(out=back.t[:], in_=o.t[:]), [o.r], [back.r])
            P.emit("dve", lambda e: e.tensor_tensor(out=cur.t[:], in0=cur.t[:], in1=back.t[:], op=ALU.subtract), [cur.r, back.r], [cur.r])
        outs.append(o)
    return outs


def phase_B(nc, io, lam_init):
    with ExitStack() as st:
        P = Prog(nc, st)
        CH = 2048
        cw = P.tile([128, 4], F32, "cw"); cb = P.tile([128, 1], F32, "cb")
        wa32 = P.tile([128, 128], F32, "wa32"); wi32 = P.tile([128, 128], F32, "wi32")
        wa = P.tile([128, 128], BF16, "wa"); wi = P.tile([128, 128], BF16, "wi")
        ba = P.tile([128, 1], F32, "ba"); bi = P.tile([128, 1], F32, "bi"); lm = P.tile([128, 1], F32, "lm")
        one = P.tile([128, 1], F32, "one"); c8 = P.tile([128, 1], F32, "c8")
        for t_, k_ in ((cw, "conv_w"), (cb, "conv_b"), (wa32, "w_a"), (wi32, "w_i"), (ba, "b_a"), (bi, "b_i"), (lm, "lru_lam")):
            P.dma("sp", t_.t[:], io[k_], writes=[t_.r])
        P.emit("dve", lambda e: e.tensor_copy(out=wa.t[:], in_=wa32.t[:]), [wa32.r], [wa.r])
        P.emit("dve", lambda e: e.tensor_copy(out=wi.t[:], in_=wi32.t[:]), [wi32.r], [wi.r])
        P.emit("dve", lambda e: e.memset(one.t[:], 1.0), [], [one.r])
        P.emit("act", lambda e: e.activation(out=c8.t[:], in_=lm.t[:], func=AF.Exp, scale=-1.0), [lm.r], [c8.r])
        P.emit("act", lambda e: e.activation(out=c8.t[:], in_=c8.t[:], func=AF.Ln, bias=one.t[:, 0:1]), [c8.r, one.r], [c8.r])
        P.emit("dve", lambda e: e.tensor_scalar_mul(out=c8.t[:], in0=c8.t[:], scalar1=-8.0), [c8.r], [c8.r])
        xpad = P.tile([128, CH + 3], F32, "xpad"); xc = P.tile([128, CH], F32, "xc"); xcb = P.tile([128, CH], BF16, "xcb")
        rr = P.tile([128, CH], F32, "rr"); ii = P.tile([128, CH], F32, "ii"); tmp = P.tile([128, CH], F32, "tmp")
        hh = Ring([P.tile([128, CH], F32, "hh") for _ in range(2)]); gg = P.tile([128, CH], F32, "gg"); gt2 = P.tile([128, CH], F32, "gt2")
        psl = Ring([P.psum([128, 512], F32, "psl") for _ in range(2)])
        hprev = None
        for ck in range(SEQ // CH):
            t0 = ck * CH
            P.dma("sp", xpad.t[:], io["xr"][:, t0:t0 + CH + 3], writes=[xpad.r])
            P.dma("sp", gg.t[:], io["gr"][:, t0:t0 + CH], writes=[gg.r])
            P.emit("dve", lambda e: e.tensor_scalar(out=xc.t[:], in0=xpad.t[:, 0:CH], scalar1=cw.t[:, 0:1], scalar2=cb.t[:, 0:1],
                                                    op0=ALU.mult, op1=ALU.add), [xpad.r, cw.r, cb.r], [xc.r])
            for j in range(1, 4):
                P.emit("dve", lambda e, j=j: e.scalar_tensor_tensor(out=xc.t[:], in0=xpad.t[:, j:j + CH], scalar=cw.t[:, j:j + 1], in1=xc.t[:],
                                                                    op0=ALU.mult, op1=ALU.add), [xpad.r, cw.r, xc.r], [xc.r])
            P.emit("dve", lambda e: e.tensor_copy(out=xcb.t[:], in_=xc.t[:]), [xc.r], [xcb.r])
            for s in range(CH // 512):
                sl = slice(s * 512, (s + 1) * 512)
                for (wt, bt, dst) in ((wa, ba, rr), (wi, bi, ii)):
                    ps = psl.next()
                    P.emit("pe", lambda e, ps=ps, wt=wt, sl=sl: e.matmul(ps.t[:], lhsT=wt.t[:], rhs=xcb.t[:, sl], start=True, stop=True),
                           [wt.r, xcb.r], [ps.r])
                    P.emit("act", lambda e, ps=ps, bt=bt, dst=dst, sl=sl: e.activation(out=dst.t[:, sl], in_=ps.t[:], func=AF.Sigmoid, bias=bt.t[:, 0:1]),
                           [ps.r, bt.r], [dst.r])
            P.emit("act", lambda e: e.activation(out=rr.t[:], in_=rr.t[:], func=AF.Exp, scale=c8.t[:, 0:1]), [rr.r, c8.r], [rr.r])
            P.emit("dve", lambda e: e.tensor_tensor(out=tmp.t[:], in0=rr.t[:], in1=rr.t[:], op=ALU.mult), [rr.r], [tmp.r])
            P.emit("act", lambda e: e.activation(out=tmp.t[:], in_=tmp.t[:], func=AF.Sqrt, scale=-1.0, bias=one.t[:, 0:1]), [tmp.r, one.r], [tmp.r])
            P.emit("dve", lambda e: e.tensor_tensor(out=ii.t[:], in0=ii.t[:], in1=xc.t[:], op=ALU.mult), [ii.r, xc.r], [ii.r])
            P.emit("dve", lambda e: e.tensor_tensor(out=ii.t[:], in0=ii.t[:], in1=tmp.t[:], op=ALU.mult), [ii.r, tmp.r], [ii.r])
            hcur = hh.next()
            if hprev is None:
                P.emit("dve", lambda e, hcur=hcur: e.tensor_tensor_scan(out=hcur.t[:], data0=rr.t[:], data1=ii.t[:], initial=0.0,
                                                                        op0=ALU.mult, op1=ALU.add), [rr.r, ii.r], [hcur.r])
            else:
                P.emit("dve", lambda e, hcur=hcur, hp=hprev: e.tensor_tensor_scan(out=hcur.t[:], data0=rr.t[:], data1=ii.t[:],
                                                                                  initial=hp.t[:, CH - 1:CH], op0=ALU.mult, op1=ALU.add),
                       [rr.r, ii.r, hprev.r], [hcur.r])
            hprev = hcur
            P.emit("pool", lambda e: e.tensor_tensor(out=gt2.t[:], in0=gg.t[:], in1=gg.t[:], op=ALU.mult), [gg.r], [gt2.r])
            P.emit("pool", lambda e: e.tensor_scalar(out=gt2.t[:], in0=gt2.t[:], scalar1=0.044715, scalar2=1.0, op0=ALU.mult, op1=ALU.add), [gt2.r], [gt2.r])
            P.emit("pool", lambda e: e.tensor_tensor(out=gt2.t[:], in0=gt2.t[:], in1=gg.t[:], op=ALU.mult), [gt2.r, gg.r], [gt2.r])
            P.emit("act", lambda e: e.activation(out=gt2.t[:], in_=gt2.t[:], func=AF.Sigmoid, scale=2.0 * math.sqrt(2.0 / math.pi)), [gt2.r], [gt2.r])
            P.emit("pool", lambda e: e.tensor_tensor(out=gt2.t[:], in0=gt2.t[:], in1=gg.t[:], op=ALU.mult), [gt2.r, gg.r], [gt2.r])
            P.emit("dve", lambda e, hcur=hcur: e.tensor_tensor(out=tmp.t[:], in0=gt2.t[:], in1=hcur.t[:], op=ALU.mult), [gt2.r, hcur.r], [tmp.r])
            P.dma("sp", io["lru"][:, t0:t0 + CH], tmp.t[:], reads=[tmp.r])
        P.finalize()
    nc.all_engine_barrier()

    with ExitStack() as st:
        P = Prog(nc, st)
        kaug_d = nc.dram_tensor("kaug_d", [6, 6, SEQ], BF16).ap()
        qaug_d = nc.dram_tensor("qaug_d", [6, 6, TL], BF16).ap()
        lf = P.tile([6, SEQ], F32, "lf"); ones6 = P.tile([6, SEQ], BF16, "ones6"); cum = P.tile([6, SEQ], F32, "cum")
        om = P.tile([6, SEQ], F32, "om"); cown = P.tile([6, TL], F32, "cown")
        qcur = P.tile([6, TL], F32, "qcur"); qback = P.tile([6, TL], F32, "qback")
        P.dma("sp", lf.t[:], io["logfT"], writes=[lf.r])
        P.dma("sp", om.t[:], io["ownmask"], writes=[om.r])
        P.emit("dve", lambda e: e.memset(ones6.t[:], 1.0), [], [ones6.r])
        P.emit("dve", lambda e: e.tensor_tensor_scan(out=cum.t[:], data0=ones6.t[:], data1=lf.t[:], initial=0.0, op0=ALU.mult, op1=ALU.add),
               [ones6.r, lf.r], [cum.r])
        P.emit("dve", lambda e: e.tensor_tensor(out=om.t[:], in0=om.t[:], in1=cum.t[:], op=ALU.mult), [om.r, cum.r], [om.r])
        P.emit("dve", lambda e: e.tensor_reduce(out=cown.t[:].rearrange("p (j i) -> p j i", j=8),
                                                in_=om.t[:].rearrange("p (j r i) -> p j i r", j=8, r=8, i=128), axis=AX.X, op=ALU.add),
               [om.r], [cown.r])
        q3 = split3(P, cown, qcur, qback, TL, 6, 1.0)
        k3 = split3(P, cum, lf, om, SEQ, 6, -1.0)
        for i in range(3):
            P.dma("sp", kaug_d[:, i, :], ones6.t[:], reads=[ones6.r])
            P.dma("sp", kaug_d[:, 3 + i, :], k3[i].t[:], reads=[k3[i].r])
            P.dma("sp", qaug_d[:, i, :], q3[i].t[:], reads=[q3[i].r])
            P.dma("sp", qaug_d[:, 3 + i, :], ones6.t[:, 0:TL], reads=[ones6.r])
        P.finalize()
    nc.all_engine_barrier()

    with ExitStack() as st:
        P = Prog(nc, st)
        mF = P.tile([128, 8, 128], BF16, "mF"); mD = P.tile([128, 8, 128], BF16, "mD")
        P.dma("sp", mF.t[:], io["maskF"], writes=[mF.r])
        P.dma("sp", mD.t[:], io["maskD"], writes=[mD.r])
        lq = [P.tile([128, 64], F32, "lq") for _ in range(4)]
        for t_, k_ in zip(lq, ("lam_q1", "lam_k1", "lam_q2", "lam_k2")):
            P.dma("sp", t_.t[:], io[k_].partition_broadcast(128), writes=[t_.r])
        s1 = P.tile([128, 1], F32, "s1"); s2 = P.tile([128, 1], F32, "s2"); nlam = P.tile([128, 1], F32, "nlam")
        epst = P.tile([128, 1], F32, "eps")
        P.emit("dve", lambda e: e.memset(epst.t[:], LN_EPS), [], [epst.r])
        for (a_, b_, s_) in ((lq[0], lq[1], s1), (lq[2], lq[3], s2)):
            P.emit("dve", lambda e, a_=a_, b_=b_: e.tensor_tensor(out=a_.t[:], in0=a_.t[:], in1=b_.t[:], op=ALU.mult), [a_.r, b_.r], [a_.r])
            P.emit("dve", lambda e, a_=a_, s_=s_: e.reduce_sum(out=s_.t[:], in_=a_.t[:], axis=AX.X), [a_.r], [s_.r])
            P.emit("act", lambda e, s_=s_: e.activation(out=s_.t[:], in_=s_.t[:], func=AF.Exp), [s_.r], [s_.r])
        P.emit("dve", lambda e: e.tensor_tensor(out=nlam.t[:], in0=s2.t[:], in1=s1.t[:], op=ALU.subtract), [s1.r, s2.r], [nlam.r])
        P.emit("dve", lambda e: e.tensor_scalar_add(out=nlam.t[:], in0=nlam.t[:], scalar1=-float(lam_init)), [nlam.r], [nlam.r])
        gsb = P.tile([128, 128], F32, "gsb")
        P.dma("sp", gsb.t[:], io["subln_g"].partition_broadcast(128), writes=[gsb.r])
        P.emit("dve", lambda e: e.tensor_scalar_mul(out=gsb.t[:], in0=gsb.t[:], scalar1=float(1.0 - lam_init)), [gsb.r], [gsb.r])

        kTs = Ring([P.tile([128, SEQ], BF16, "kT") for _ in range(2)])
        kaugs = Ring([P.tile([128, SEQ], BF16, "kaug") for _ in range(2)])
        qaugs = Ring([P.tile([128, TL], BF16, "qaug") for _ in range(2)])
        for t_ in kaugs.tiles + qaugs.tiles:
            P.emit("pool", lambda e, t_=t_: e.memset(t_.t[:], 0.0), [], [t_.r])
        vs = Ring([P.tile([128, 64, 130], BF16, "v") for _ in range(2)])
        qTs = Ring([P.tile([128, TL], BF16, "qT") for _ in range(2)])
        for _ in range(2):
            v_ = vs.next()
            P.emit("dve", lambda e, v_=v_: e.memset(v_.t[:, :, 128:130], 1.0), [], [v_.r])
        pss = Ring([P.psum([128, 512], F32, "pss") for _ in range(LOOK + 1)])
        pTs = Ring([P.tile([128, 512], BF16, "pT") for _ in range(PVLAG + 3)])
        o1s = Ring([P.tile([128, 128], F32, "o1") for _ in range(3)])
        rcs = Ring([P.tile([128, 1], F32, "rc") for _ in range(6)])
        junk = P.tile([128, 128], F32, "junk")

        class Acc:
            pass
        accbanks = [P.psum([128, 512], F32, "accb") for _ in range(4)]
        accs = []
        for j in range(8):
            a_ = Acc()
            a_.ap = accbanks[j % 4].t[:, 0:130]
            a_.r = accbanks[j % 4].r
            accs.append(a_)
        o1all = P.tile([128, 8, 128], F32, "o1all")
        o1res = [Res() for _ in range(8)]

        tasks = []
        head_loads = []

        def sel_head(kall, vall, qown, hidx, aug=None):
            kT, v_, qT = kTs.next(), vs.next(), qTs.next()
            ka = qa = None
            if aug is not None:
                ka, qa = kaugs.next(), qaugs.next()

            def load():
                P.dma("sp", kT.t[:], kall[hidx * 128:(hidx + 1) * 128, :], writes=[kT.r])
                P.dma("sp", v_.t[:, :, 0:128], vall.rearrange("(t p) d -> p t d", p=128)[:, :, hidx * 128:(hidx + 1) * 128], writes=[v_.r])
                P.dma("sp", qT.t[:], qown[hidx * 128:(hidx + 1) * 128, :], writes=[qT.r])
                if aug is not None:
                    P.dma("sp", ka.t[0:6, :], kaug_d[aug], writes=[ka.r])
                    P.dma("sp", qa.t[0:6, :], qaug_d[aug], writes=[qa.r])
            head_loads.append(load)
            return kT, v_, qT, ka, qa

        def add_map(qk_mm, v_, mask, clamp, fin_fn, head_first):
            first = True
            for ps_ in range(2):
                for kt in range(32 * (ps_ + 1)):
                    G = kt // 8
                    jlo, jhi = max(G, 4 * ps_), 4 * (ps_ + 1)
                    if jlo >= jhi:
                        continue
                    tasks.append(dict(kt=kt, G=G, jlo=jlo, jhi=jhi, qk=qk_mm, v=v_, mask=mask, clamp=clamp, fin=fin_fn,
                                      head_first=head_first if first else None))
                    first = False

        def fox_fin(h):
            def fin(j):
                acc = accs[j]
                rc = rcs.next(); o1 = o1s.next()
                P.emit("dve", lambda e: e.reciprocal(out=rc.t[:], in_=acc.ap[:, 128:129]), [acc.r], [rc.r])
                P.emit("dve", lambda e: e.tensor_scalar_mul(out=o1.t[:], in0=acc.ap[:, 0:128], scalar1=rc.t[:, 0:1]), [acc.r, rc.r], [o1.r])
                P.dma("sp", io["attn"][j * 128:(j + 1) * 128, h * 128:(h + 1) * 128], o1.t[:], reads=[o1.r])
            return fin

        def diff_fin0():
            def fin(j):
                acc = accs[j]
                rc1 = rcs.next()
                P.emit("dve", lambda e: e.reciprocal(out=rc1.t[:], in_=acc.ap[:, 128:129]), [acc.r], [rc1.r])
                P.emit("dve", lambda e: e.tensor_scalar_mul(out=o1all.t[:, j, :], in0=acc.ap[:, 0:128], scalar1=rc1.t[:, 0:1]), [acc.r, rc1.r], [o1res[j]])
            return fin

        def diff_fin1(hd):
            def fin(j):
                acc = accs[j]
                rc2, ss = rcs.next(), rcs.next()
                oc = o1s.next()
                P.emit("dve", lambda e: e.reciprocal(out=rc2.t[:], in_=acc.ap[:, 128:129]), [acc.r], [rc2.r])
                P.emit("dve", lambda e: e.tensor_tensor(out=rc2.t[:], in0=rc2.t[:], in1=nlam.t[:], op=ALU.mult), [rc2.r, nlam.r], [rc2.r])
                P.emit("dve", lambda e: e.scalar_tensor_tensor(out=oc.t[:], in0=acc.ap[:, 0:128], scalar=rc2.t[:, 0:1], in1=o1all.t[:, j, :],
                                                               op0=ALU.mult, op1=ALU.add), [acc.r, rc2.r, o1res[j]], [oc.r])
                P.emit("act", lambda e: e.activation(out=junk.t[:], in_=oc.t[:], func=AF.Square, accum_out=ss.t[:, 0:1]), [oc.r], [junk.r, ss.r])
                P.emit("act", lambda e: e.activation(out=ss.t[:], in_=ss.t[:], func=AF.Sqrt, scale=1.0 / 128.0, bias=epst.t[:, 0:1]), [ss.r, epst.r], [ss.r])
                P.emit("dve", lambda e: e.reciprocal(out=ss.t[:], in_=ss.t[:]), [ss.r], [ss.r])
                P.emit("dve", lambda e: e.scalar_tensor_tensor(out=oc.t[:], in0=oc.t[:], scalar=ss.t[:, 0:1], in1=gsb.t[:], op0=ALU.mult, op1=ALU.mult),
                       [oc.r, ss.r, gsb.r], [oc.r])
                P.dma("sp", io["attn"][j * 128:(j + 1) * 128, 768 + hd * 128:768 + (hd + 1) * 128], oc.t[:], reads=[oc.r])
            return fin

        hcount = 0
        for h in range(6):
            kT, v_, qT, ka, qa = sel_head(io["kTf_all"], io["vf_all"], io["qTf"], h, aug=h)

            def qk_mm(ps, kt, jlo, jhi, kT=kT, qT=qT, ka=ka, qa=qa):
                n = (jhi - jlo) * 128
                P.emit("pe", lambda e: e.matmul(ps.t[:, 0:n], lhsT=kT.t[:, kt * 128:(kt + 1) * 128], rhs=qT.t[:, jlo * 128:jhi * 128],
                                                start=True, stop=False), [kT.r, qT.r], [ps.r])
                P.emit("pe", lambda e: e.matmul(ps.t[:, 0:n], lhsT=ka.t[:, kt * 128:(kt + 1) * 128], rhs=qa.t[:, jlo * 128:jhi * 128],
                                                start=False, stop=True), [ka.r, qa.r], [ps.r])
            add_map(qk_mm, v_, mF, True, fox_fin(h), hcount)
            hcount += 1
        for hd in range(4):
            kT, v_, qT, _, _ = sel_head(io["kTd_all"], io["vd_all"], io["qTd"], hd)
            for m in range(2):
                def qk_mm(ps, kt, jlo, jhi, kT=kT, qT=qT, m=m):
                    n = (jhi - jlo) * 128
                    P.emit("pe", lambda e: e.matmul(ps.t[:, 0:n], lhsT=kT.t[m * 64:(m + 1) * 64, kt * 128:(kt + 1) * 128],
                                                    rhs=qT.t[m * 64:(m + 1) * 64, jlo * 128:jhi * 128], start=True, stop=True), [kT.r, qT.r], [ps.r])
                add_map(qk_mm, v_, mD, False, diff_fin0() if m == 0 else diff_fin1(hd), hcount if m == 0 else None)
            hcount += 1

        def emit_qk(t):
            ps = pss.next()
            t["ps"] = ps
            t["qk"](ps, t["kt"], t["jlo"], t["jhi"])

        def emit_exp(t):
            ps, kt, G, jlo, jhi, mask = t["ps"], t["kt"], t["G"], t["jlo"], t["jhi"], t["mask"]
            n = (jhi - jlo) * 128
            pT = pTs.next()
            t["pT"] = pT
            diag = (jlo == G)
            if t["clamp"] and diag:
                P.emit("dve", lambda e: e.tensor_scalar_min(out=ps.t[:, 0:128], in0=ps.t[:, 0:128], scalar1=80.0), [ps.r], [ps.r])
            P.emit("act", lambda e: e.activation(out=pT.t[:, 0:n], in_=ps.t[:, 0:n], func=AF.Exp), [ps.r], [pT.r])
            if diag:
                r = kt - 8 * G
                P.emit("dve", lambda e: e.tensor_tensor(out=pT.t[:, 0:128], in0=pT.t[:, 0:128], in1=mask.t[:, r, :], op=ALU.mult), [pT.r, mask.r], [pT.r])

        def emit_pv(t):
            pT, kt, G, jlo, jhi, v_ = t["pT"], t["kt"], t["G"], t["jlo"], t["jhi"], t["v"]
            diag = (jlo == G)
            hf = t["head_first"]
            if hf is not None and hf + 1 < len(head_loads):
                head_loads[hf + 1]()
            while pending:
                pending.pop(0)()
            for jj in range(jlo, jhi):
                acc = accs[jj]
                c0 = (jj - jlo) * 128
                P.emit("pe", lambda e, acc=acc, c0=c0, jj=jj: e.matmul(acc.ap, lhsT=pT.t[:, c0:c0 + 128], rhs=v_.t[:, kt, :],
                                                                   start=(kt == 0), stop=(kt == 8 * jj + 7)), [pT.r, v_.r], [acc.r])
            if kt % 8 == 7 and diag:
                pending.append(lambda f=t["fin"], G=G: f(G))
            for _ in range(NDUMMY):
                P.emit("pe", lambda e: e.matmul(dummy.t[:], lhsT=warm_l.t[:, 0:128], rhs=warm_r.t[:, 0:512], start=True, stop=True),
                       [warm_l.r, warm_r.r], [dummy.r])

        if NDUMMY:
            dummy = P.psum([128, 512], F32, "dummy")
            warm_l = P.tile([128, 128], BF16, "warm_l")
            warm_r = P.tile([128, 512], BF16, "warm_r")
            P.emit("pool", lambda e: e.memset(warm_l.t[:], 0.0), [], [warm_l.r])
            P.emit("pool", lambda e: e.memset(warm_r.t[:], 0.0), [], [warm_r.r])
        pending = []
        pending_prev = []
        head_loads[0]()
        for i in range(min(LOOK, len(tasks))):
            emit_qk(tasks[i])
        for i in range(len(tasks) + PVLAG):
            if i + LOOK < len(tasks):
                emit_qk(tasks[i + LOOK])
            if i < len(tasks):
                emit_exp(tasks[i])
            if i - PVLAG >= 0:
                emit_pv(tasks[i - PVLAG])
        for f_ in pending_prev + pending:
            f_()
        P.finalize()
    nc.all_engine_barrier()


def build_B(lam_init):
    nc = bass.Bass("TRN2", target_bir_lowering=False)
    io = dict(
        qTf=din(nc, "qTf", [768, TL], BF16), kTf_all=din(nc, "kTf_all", [768, SEQ], BF16), vf_all=din(nc, "vf_all", [SEQ, 768], BF16),
        logfT=din(nc, "logfT", [6, SEQ]), ownmask=din(nc, "ownmask", [6, SEQ]),
        maskF=din(nc, "maskF", [128, 8, 128], BF16), maskD=din(nc, "maskD", [128, 8, 128], BF16),
        qTd=din(nc, "qTd", [512, TL], BF16), kTd_all=din(nc, "kTd_all", [512, SEQ], BF16), vd_all=din(nc, "vd_all", [SEQ, 512], BF16),
        lam_q1=din(nc, "lam_q1", [64]), lam_k1=din(nc, "lam_k1", [64]), lam_q2=din(nc, "lam_q2", [64]), lam_k2=din(nc, "lam_k2", [64]),
        subln_g=din(nc, "subln_g", [128]),
        xr=din(nc, "xr", [128, SEQ + 3]), gr=din(nc, "gr", [128, SEQ]), conv_w=din(nc, "conv_w", [128, 4]), conv_b=din(nc, "conv_b", [128, 1]),
        w_a=din(nc, "w_a", [128, 128]), w_i=din(nc, "w_i", [128, 128]), b_a=din(nc, "b_a", [128, 1]), b_i=din(nc, "b_i", [128, 1]),
        lru_lam=din(nc, "lru_lam", [128, 1]),
        attn=dout(nc, "attn", [TL, 1280]), lru=dout(nc, "lru", [128, SEQ]),
    )
    phase_B(nc, io, lam_init)
    return nc


def global_order(parts, axis):
    full = np.concatenate(parts, axis=axis)
    perm = np.concatenate([own_rows(c) for c in range(NC)])
    inv = np.empty_like(perm)
    inv[perm] = np.arange(perm.size)
    return np.take(full, inv, axis=axis)


def core_masks(c):
    i = np.arange(128)
    tri = (i[:, None] <= i[None, :])
    blk = ((i[:, None] // 64) <= (i[None, :] // 64))
    mF = np.zeros((128, 8, 128), np.float32)
    mD = np.zeros((128, 8, 128), np.float32)
    for r in range(8):
        if r < c:
            mF[:, r, :] = 1.0
            mD[:, r, :] = 1.0
        elif r == c:
            mF[:, r, :] = tri
            mD[:, r, :] = blk
    om = np.zeros((6, SEQ), np.float32)
    om[:, own_rows(c)] = 1.0
    return mF.astype(ml_dtypes.bfloat16), mD.astype(ml_dtypes.bfloat16), om


def launch_B(resA, prm, l):
    lam_init = 0.8 - 0.6 * math.exp(-0.3 * l)
    nc = build_B(lam_init)
    kTf_all = global_order([r["kTf"] for r in resA], 1)
    vf_all = global_order([r["vf"] for r in resA], 0)
    kTd_all = global_order([r["kTd"] for r in resA], 1)
    vd_all = global_order([r["vd"] for r in resA], 0)
    logfT = np.ascontiguousarray(global_order([r["logf"] for r in resA], 0).T)
    xr_all = global_order([r["xrT"] for r in resA], 1)
    gr_all = global_order([r["grT"] for r in resA], 1)
    in_maps = []
    for c in range(NC):
        mF, mD, om = core_masks(c)
        n = c % 6
        sl = slice(n * 128, (n + 1) * 128)
        xr = np.zeros((128, SEQ + 3), np.float32)
        xr[:, 3:] = xr_all[sl]
        in_maps.append(dict(
            qTf=resA[c]["qTf"], kTf_all=kTf_all, vf_all=vf_all, logfT=logfT, ownmask=om, maskF=mF, maskD=mD,
            qTd=resA[c]["qTd"], kTd_all=kTd_all, vd_all=vd_all,
            lam_q1=prm["lam_q1"][l], lam_k1=prm["lam_k1"][l], lam_q2=prm["lam_q2"][l], lam_k2=prm["lam_k2"][l],
            subln_g=prm["subln_g"][l], xr=xr, gr=np.ascontiguousarray(gr_all[sl]),
            conv_w=np.ascontiguousarray(prm["conv_w"][l][:, sl].T), conv_b=np.ascontiguousarray(prm["conv_b"][l][sl, None]),
            w_a=np.ascontiguousarray(prm["w_a"][l][n]), w_i=np.ascontiguousarray(prm["w_i"][l][n]),
            b_a=np.ascontiguousarray(prm["b_a"][l][sl, None]), b_i=np.ascontiguousarray(prm["b_i"][l][sl, None]),
            lru_lam=np.ascontiguousarray(prm["lru_lambda"][l][sl, None])))
    return run(nc, in_maps)


def layer_norm_tiles(P, acc, accr, gt, bt, epst, junk_ap, sm):
    for j in range(8):
        a = acc.t[:, j, :]
        ar = accr[j]
        mu, ss = sm.next(), sm.next()
        P.emit("dve", lambda e, a=a, mu=mu: e.reduce_sum(out=mu.t[:], in_=a, axis=AX.X), [ar], [mu.r])
        P.emit("dve", lambda e, mu=mu: e.tensor_scalar_mul(out=mu.t[:], in0=mu.t[:], scalar1=-1.0 / D), [mu.r], [mu.r])
        P.emit("dve", lambda e, a=a, mu=mu: e.tensor_scalar_add(out=a, in0=a, scalar1=mu.t[:, 0:1]), [ar, mu.r], [ar])
        P.emit("act", lambda e, a=a, ss=ss: e.activation(out=junk_ap[0], in_=a, func=AF.Square, accum_out=ss.t[:, 0:1]), [ar], [junk_ap[1], ss.r])
        P.emit("act", lambda e, ss=ss: e.activation(out=ss.t[:], in_=ss.t[:], func=AF.Sqrt, scale=1.0 / D, bias=epst.t[:, 0:1]), [ss.r, epst.r], [ss.r])
        P.emit("dve", lambda e, ss=ss: e.reciprocal(out=ss.t[:], in_=ss.t[:]), [ss.r], [ss.r])
        P.emit("dve", lambda e, a=a, ss=ss: e.scalar_tensor_tensor(out=a, in0=a, scalar=ss.t[:, 0:1], in1=gt.t[:], op0=ALU.mult, op1=ALU.mult),
               [ar, ss.r, gt.r], [ar])
        P.emit("dve", lambda e, a=a: e.tensor_tensor(out=a, in0=a, in1=bt.t[:], op=ALU.add), [ar, bt.r], [ar])


def phase_C(nc, io):
    with ExitStack() as st:
        P = Prog(nc, st)
        ident = P.tile([128, 128], F32, "ident")
        P.dma("sp", ident.t[:], io["ident"], writes=[ident.r])
        epst = P.tile([128, 1], F32, "eps")
        P.emit("dve", lambda e: e.memset(epst.t[:], LN_EPS), [], [epst.r])
        acc = P.tile([128, 8, D], F32, "acc")
        accr = [Res() for _ in range(8)]
        xT = P.tile([128, 16, TL], BF16, "xT")
        lng = P.tile([128, D], F32, "lng"); lnb = P.tile([128, D], F32, "lnb")
        hid = P.tile([128, 4, TL], BF16, "hid")
        junk_ap = (hid.t[:].rearrange("p a b -> p (a b)")[:, 0:D], hid.r)
        wgs = Ring([P.tile([128, 16, 256], BF16, "wg") for _ in range(2)])
        wus = Ring([P.tile([128, 16, 256], BF16, "wu") for _ in range(2)])
        wds = Ring([P.tile([128, 4, D], BF16, "wd") for _ in range(2)])
        psr = Ring([P.psum([128, 512], F32, "ps") for _ in range(7)])
        sm = Ring([P.tile([128, 1], F32, "sm") for _ in range(12)])
        gate = P.tile([128, 8, 32], F32, "gate")
        wgr = P.tile([128, 16, 36], F32, "wgr")
        bgr = P.tile([128, 36], F32, "bgr")
        sgs = Ring([P.tile([128, 512], F32, "sg") for _ in range(2)])
        P.dma("sp", wgr.t[:], io["w_gr"].rearrange("(kc p) n -> p kc n", p=128), writes=[wgr.r])
        P.dma("sp", bgr.t[:], io["b_gr"].partition_broadcast(128), writes=[bgr.r])
        P.dma("sp", lng.t[:], io["ln1_g"].partition_broadcast(128), writes=[lng.r])
        P.dma("sp", lnb.t[:], io["ln1_b"].partition_broadcast(128), writes=[lnb.r])

        att = Ring([P.tile([128, 512], F32, "att") for _ in range(2)])
        for j in range(8):
            P.dma("sp", acc.t[:, j, :], io["hres"][j * 128:(j + 1) * 128, :], writes=[accr[j]])
            for (c0, n, d0) in ((0, 4, 0), (512, 2, 4), (768, 4, 12)):
                at = att.next()
                P.dma("sp", at.t[:, 0:n * 128], io["attn"][j * 128:(j + 1) * 128, c0:c0 + n * 128], writes=[at.r])
                ps = psr.next()
                for q in range(n):
                    P.emit("pe", lambda e, ps=ps, at=at, q=q: e.transpose(ps.t[:, q * 128:(q + 1) * 128], at.t[:, q * 128:(q + 1) * 128], ident.t[:]),
                           [at.r, ident.r], [ps.r])
                copy_op(P, evac_eng(), xT.t[:, d0:d0 + n, j * 128:(j + 1) * 128], ps.t[:, 0:n * 128].rearrange("p (a b) -> p a b", a=n), [ps.r], [xT.r])
        for b in range(6):
            P.dma("pool", xT.t[:, 6 + b, :], io["lruT"][b * 128:(b + 1) * 128, :], writes=[xT.r])

        wos = wgs.tiles + wus.tiles
        wi_ = 0
        for n in range(8):
            wt = wos[wi_ % len(wos)]; wi_ += 1
            P.dma("pool", wt.t[:], io["w_o"][:, n * 256:(n + 1) * 256].rearrange("(kc p) n -> p kc n", p=128), writes=[wt.r])
            for j in range(8):
                ps = psr.next()
                for kc in range(16):
                    P.emit("pe", lambda e, ps=ps, wt=wt, kc=kc, j=j: e.matmul(ps.t[:, 0:256], lhsT=xT.t[:, kc, j * 128:(j + 1) * 128], rhs=wt.t[:, kc, :],
                                                                            start=(kc == 0), stop=(kc == 15)), [xT.r, wt.r], [ps.r])
                a = acc.t[:, j, n * 256:(n + 1) * 256]
                P.emit("dve", lambda e, a=a, ps=ps: e.scalar_tensor_tensor(out=a, in0=a, scalar=float(ALPHA), in1=ps.t[:, 0:256], op0=ALU.mult, op1=ALU.add),
                       [accr[j], ps.r], [accr[j]])
        if CSTOP >= 2:
            layer_norm_tiles(P, acc, accr, lng, lnb, epst, junk_ap, sm)

        h1f = lng
        h1f_v = h1f.t[:].rearrange("p (a b) -> p a b", a=16)
        hlo = P.tile([128, 16, 128], BF16, "hlo"); whi = P.tile([128, 16, 36], BF16, "whi"); wlo = P.tile([128, 16, 36], BF16, "wlo")
        P.emit("dve", lambda e: e.tensor_copy(out=whi.t[:], in_=wgr.t[:]), [wgr.r], [whi.r])
        P.emit("dve", lambda e: e.tensor_tensor(out=wlo.t[:], in0=wgr.t[:], in1=whi.t[:], op=ALU.subtract), [wgr.r, whi.r], [wlo.r])
        lg = P.tile([128, 36], F32, "lg"); goh = P.tile([128, 4], F32, "goh"); ge = P.tile([128, 4], F32, "ge")
        esel = P.tile([128, 8], F32, "esel"); oh1 = P.tile([128, 8], F32, "oh1"); oh2 = P.tile([128, 8], F32, "oh2")
        msk = P.tile([128, 8], F32, "msk"); eg = P.tile([128, 8], F32, "eg")
        for j in range(8 if CSTOP >= 3.1 else 0):
            for g in range(4):
                ps = psr.next()
                for q in range(4):
                    kc = g * 4 + q
                    P.emit("pe", lambda e, ps=ps, q=q, kc=kc, j=j: e.transpose(ps.t[:, q * 128:(q + 1) * 128], acc.t[:, j, kc * 128:(kc + 1) * 128], ident.t[:]),
                           [accr[j], ident.r], [ps.r])
                P.emit("act", lambda e, ps=ps, g=g, j=j: e.activation(out=xT.t[:, g * 4:(g + 1) * 4, j * 128:(j + 1) * 128],
                                                                    in_=ps.t[:].rearrange("p (a b) -> p a b", a=4), func=AF.Copy), [ps.r], [xT.r])
                ps2 = psr.next()
                for q in range(4):
                    kc = g * 4 + q
                    P.emit("pe", lambda e, ps2=ps2, q=q, kc=kc, j=j: e.transpose(ps2.t[:, q * 128:(q + 1) * 128], acc.t[:, j, kc * 128:(kc + 1) * 128], ident.t[:]),
                           [accr[j], ident.r], [ps2.r])
                P.emit("dve", lambda e, ps2=ps2, g=g: e.tensor_copy(out=h1f_v[:, g * 4:(g + 1) * 4, :], in_=ps2.t[:].rearrange("p (a b) -> p a b", a=4)),
                       [ps2.r], [h1f.r])
            if CSTOP < 3.2:
                continue
            P.emit("dve", lambda e, j=j: e.tensor_tensor(out=hlo.t[:], in0=h1f_v, in1=xT.t[:, :, j * 128:(j + 1) * 128], op=ALU.subtract), [h1f.r, xT.r], [hlo.r])
            ps = psr.next()
            for kc in range(16):
                for ti, (lt_, lr_, rt_, rr_) in enumerate(((xT.t[:, kc, j * 128:(j + 1) * 128], xT.r, whi.t[:, kc, :], whi.r),
                                                           (xT.t[:, kc, j * 128:(j + 1) * 128], xT.r, wlo.t[:, kc, :], wlo.r),
                                                           (hlo.t[:, kc, :], hlo.r, whi.t[:, kc, :], whi.r))):
                    P.emit("pe", lambda e, ps=ps, lt_=lt_, rt_=rt_, kc=kc, ti=ti: e.matmul(ps.t[:, 0:36], lhsT=lt_, rhs=rt_, start=(kc == 0 and ti == 0), stop=(kc == 15 and ti == 2)),
                           [lr_, rr_], [ps.r])
            P.emit("dve", lambda e, ps=ps: e.tensor_tensor(out=lg.t[:], in0=ps.t[:, 0:36], in1=bgr.t[:], op=ALU.add), [ps.r, bgr.r], [lg.r])
            if CSTOP < 3.3:
                continue
            gmax, ngmax, gsum, m1, m2, dd, w1, w2 = [sm.next() for _ in range(8)]
            P.emit("dve", lambda e, gmax=gmax: e.reduce_max(out=gmax.t[:], in_=lg.t[:, 0:4], axis=AX.X), [lg.r], [gmax.r])
            P.emit("dve", lambda e, gmax=gmax: e.tensor_scalar(out=goh.t[:], in0=lg.t[:, 0:4], scalar1=gmax.t[:, 0:1], scalar2=None, op0=ALU.is_equal), [lg.r, gmax.r], [goh.r])
            P.emit("dve", lambda e, gmax=gmax, ngmax=ngmax: e.tensor_scalar_mul(out=ngmax.t[:], in0=gmax.t[:], scalar1=-1.0), [gmax.r], [ngmax.r])
            P.emit("act", lambda e, ngmax=ngmax, gsum=gsum: e.activation(out=ge.t[:], in_=lg.t[:, 0:4], func=AF.Exp, bias=ngmax.t[:, 0:1], accum_out=gsum.t[:, 0:1]),
                   [lg.r, ngmax.r], [ge.r, gsum.r])
            P.emit("dve", lambda e, gsum=gsum: e.reciprocal(out=gsum.t[:], in_=gsum.t[:]), [gsum.r], [gsum.r])
            if CSTOP < 3.4:
                continue
            P.emit("dve", lambda e: e.tensor_scalar_mul(out=esel.t[:], in0=lg.t[:, 4:12], scalar1=goh.t[:, 0:1]), [lg.r, goh.r], [esel.r])
            for g in range(1, 4):
                P.emit("dve", lambda e, g=g: e.scalar_tensor_tensor(out=esel.t[:], in0=lg.t[:, 4 + 8 * g:12 + 8 * g], scalar=goh.t[:, g:g + 1], in1=esel.t[:],
                                                                    op0=ALU.mult, op1=ALU.add), [lg.r, goh.r, esel.r], [esel.r])
            P.emit("dve", lambda e, m1=m1: e.reduce_max(out=m1.t[:], in_=esel.t[:], axis=AX.X), [esel.r], [m1.r])
            P.emit("dve", lambda e, m1=m1: e.tensor_scalar(out=oh1.t[:], in0=esel.t[:], scalar1=m1.t[:, 0:1], scalar2=None, op0=ALU.is_equal), [esel.r, m1.r], [oh1.r])
            P.emit("dve", lambda e: e.scalar_tensor_tensor(out=msk.t[:], in0=oh1.t[:], scalar=-1e30, in1=esel.t[:], op0=ALU.mult, op1=ALU.add), [oh1.r, esel.r], [msk.r])
            P.emit("dve", lambda e, m2=m2: e.reduce_max(out=m2.t[:], in_=msk.t[:], axis=AX.X), [msk.r], [m2.r])
            P.emit("dve", lambda e, m2=m2: e.tensor_scalar(out=oh2.t[:], in0=msk.t[:], scalar1=m2.t[:, 0:1], scalar2=None, op0=ALU.is_equal), [msk.r, m2.r], [oh2.r])
            if CSTOP < 3.5:
                continue
            P.emit("dve", lambda e, m1=m1, m2=m2, dd=dd: e.tensor_tensor(out=dd.t[:], in0=m2.t[:], in1=m1.t[:], op=ALU.subtract), [m1.r, m2.r], [dd.r])
            P.emit("act", lambda e, dd=dd: e.activation(out=dd.t[:], in_=dd.t[:], func=AF.Exp), [dd.r], [dd.r])
            P.emit("dve", lambda e, dd=dd, w1=w1: e.tensor_scalar_add(out=w1.t[:], in0=dd.t[:], scalar1=1.0), [dd.r], [w1.r])
            P.emit("dve", lambda e, w1=w1: e.reciprocal(out=w1.t[:], in_=w1.t[:]), [w1.r], [w1.r])
            P.emit("dve", lambda e, w1=w1, w2=w2, dd=dd: e.tensor_tensor(out=w2.t[:], in0=dd.t[:], in1=w1.t[:], op=ALU.mult), [dd.r, w1.r], [w2.r])
            P.emit("dve", lambda e, w1=w1, gsum=gsum: e.tensor_tensor(out=w1.t[:], in0=w1.t[:], in1=gsum.t[:], op=ALU.mult), [w1.r, gsum.r], [w1.r])
            P.emit("dve", lambda e, w2=w2, gsum=gsum: e.tensor_tensor(out=w2.t[:], in0=w2.t[:], in1=gsum.t[:], op=ALU.mult), [w2.r, gsum.r], [w2.r])
            P.emit("dve", lambda e, w1=w1: e.tensor_scalar_mul(out=eg.t[:], in0=oh1.t[:], scalar1=w1.t[:, 0:1]), [oh1.r, w1.r], [eg.r])
            P.emit("dve", lambda e, w2=w2: e.scalar_tensor_tensor(out=eg.t[:], in0=oh2.t[:], scalar=w2.t[:, 0:1], in1=eg.t[:], op0=ALU.mult, op1=ALU.add),
                   [oh2.r, w2.r, eg.r], [eg.r])
            for g in range(4):
                P.emit("dve", lambda e, g=g, j=j: e.tensor_scalar_mul(out=gate.t[:, j, g * 8:(g + 1) * 8], in0=eg.t[:], scalar1=goh.t[:, g:g + 1]), [eg.r, goh.r], [gate.r])
        for j in range(8 if CSTOP >= 3 else 0):
            P.emit("dve", lambda e, j=j: e.tensor_scalar_mul(out=acc.t[:, j, :], in0=acc.t[:, j, :], scalar1=float(ALPHA)), [accr[j]], [accr[j]])
        if CSTOP >= 3.02:
            P.dma("sp", lng.t[:], io["ln2_g"].partition_broadcast(128), reads=[h1f.r], writes=[lng.r])
            P.dma("sp", lnb.t[:], io["ln2_b"].partition_broadcast(128), writes=[lnb.r])

        for ex in range(NEXP if CSTOP >= 4 else 0):
            wd = wds.next()
            P.dma("pool", wd.t[:], io["w_down"][ex].rearrange("(fc p) d -> p fc d", p=128), writes=[wd.r])
            for fh in range(2):
                wg, wu = wgs.next(), wus.next()
                P.dma("pool", wg.t[:], io["w_gate"][ex][:, fh * 256:(fh + 1) * 256].rearrange("(kc p) f -> p kc f", p=128), writes=[wg.r])
                P.dma("pool", wu.t[:], io["w_up"][ex][:, fh * 256:(fh + 1) * 256].rearrange("(kc p) f -> p kc f", p=128), writes=[wu.r])
                for fc in range(2):
                    for th in range(2):
                        pg, pu = psr.next(), psr.next()
                        for (wt, ps) in ((wg, pg), (wu, pu)):
                            for kc in range(16):
                                P.emit("pe", lambda e, ps=ps, wt=wt, kc=kc, fc=fc, th=th: e.matmul(
                                    ps.t[:], lhsT=wt.t[:, kc, fc * 128:(fc + 1) * 128], rhs=xT.t[:, kc, th * 512:(th + 1) * 512],
                                    start=(kc == 0), stop=(kc == 15)), [wt.r, xT.r], [ps.r])
                        sg = sgs.next()
                        P.emit("act", lambda e, pg=pg, sg=sg: e.activation(out=sg.t[:], in_=pg.t[:], func=AF.Silu), [pg.r], [sg.r])
                        P.emit("dve", lambda e, pu=pu, sg=sg, fh=fh, fc=fc, th=th: e.tensor_tensor(out=hid.t[:, fh * 2 + fc, th * 512:(th + 1) * 512], in0=sg.t[:], in1=pu.t[:], op=ALU.mult),
                               [sg.r, pu.r], [hid.r])
            for j in range(8):
                for n in range(4):
                    ps = psr.next()
                    for fc in range(4):
                        P.emit("pe", lambda e, ps=ps, fc=fc, j=j, n=n, wd=wd: e.matmul(ps.t[:], lhsT=hid.t[:, fc, j * 128:(j + 1) * 128], rhs=wd.t[:, fc, n * 512:(n + 1) * 512],
                                                                                   start=(fc == 0), stop=(fc == 3)), [hid.r, wd.r], [ps.r])
                    a = acc.t[:, j, n * 512:(n + 1) * 512]
                    P.emit("dve", lambda e, a=a, ps=ps, j=j, ex=ex: e.scalar_tensor_tensor(out=a, in0=ps.t[:], scalar=gate.t[:, j, ex:ex + 1], in1=a, op0=ALU.mult, op1=ALU.add),
                           [ps.r, gate.r, accr[j]], [accr[j]])
        if CSTOP >= 5:
            layer_norm_tiles(P, acc, accr, lng, lnb, epst, junk_ap, sm)
        for j in range(8):
            P.dma("sp", io["hout"][j * 128:(j + 1) * 128, :], acc.t[:, j, :], reads=[accr[j]])
        P.finalize()
    nc.all_engine_barrier()


def build_C():
    nc = bass.Bass("TRN2", target_bir_lowering=False)
    io = dict(
        hres=din(nc, "hres", [TL, D]), attn=din(nc, "attn", [TL, 1280]), lruT=din(nc, "lruT", [768, TL]),
        w_o=din(nc, "w_o", [D, D]), ln1_g=din(nc, "ln1_g", [D]), ln1_b=din(nc, "ln1_b", [D]),
        w_gr=din(nc, "w_gr", [D, 36]), b_gr=din(nc, "b_gr", [36]),
        w_gate=din(nc, "w_gate", [NEXP, D, 512]), w_up=din(nc, "w_up", [NEXP, D, 512]), w_down=din(nc, "w_down", [NEXP, 512, D]),
        ln2_g=din(nc, "ln2_g", [D]), ln2_b=din(nc, "ln2_b", [D]), ident=din(nc, "ident", [128, 128]),
        hout=dout(nc, "hout", [TL, D]),
    )
    phase_C(nc, io)
    return nc


def launch_C(h_shards, resB, prm, l):
    nc = build_C()
    lru_all = np.concatenate([resB[n]["lru"] for n in range(6)], axis=0)
    w_gr = np.ascontiguousarray(np.concatenate([prm["w_group"][l], prm["w_router"][l]], axis=1))
    b_gr = np.ascontiguousarray(np.concatenate([prm["b_group"][l], prm["b_router"][l]], axis=0))
    wg = prm["w_gate"][l].reshape(32, D, 512)[:NEXP]
    wu = prm["w_up"][l].reshape(32, D, 512)[:NEXP]
    wdn = prm["w_down"][l].reshape(32, 512, D)[:NEXP]
    in_maps = []
    for c in range(NC):
        in_maps.append(dict(
            hres=h_shards[c], attn=resB[c]["attn"], lruT=np.ascontiguousarray(lru_all[:, own_rows(c)]),
            w_o=prm["w_o"][l], ln1_g=prm["ln1_g"][l], ln1_b=prm["ln1_b"][l], w_gr=w_gr, b_gr=b_gr,
            w_gate=wg, w_up=wu, w_down=wdn, ln2_g=prm["ln2_g"][l], ln2_b=prm["ln2_b"][l], ident=IDENT))
    return run(nc, in_maps)


def kernel(**inputs):
    prm = {k: np.asarray(v) for k, v in inputs.items()}
    x = prm["x"][0]
    pos = prm["positions"][0]
    h_shards = [np.ascontiguousarray(x[own_rows(c)]) for c in range(NC)]
    for l in range(2):
        resA = launch_A(h_shards, prm["w_in"][l], prm["b_f"][l], pos)
        resB = launch_B(resA, prm, l)
        resC = launch_C(h_shards, resB, prm, l)
        h_shards = [np.ascontiguousarray(r["hout"]) for r in resC]
    out = np.empty((1, SEQ, D), np.float32)
    for c in range(NC):
        out[0, own_rows(c)] = h_shards[c]
    return out
```

```python
import math
from contextlib import ExitStack
import numpy as np
import ml_dtypes
import concourse.bass as bass
import concourse.mybir as mybir
from concourse.bass_utils import run_bass_kernel_spmd

F32 = mybir.dt.float32
BF16 = mybir.dt.bfloat16
I32 = mybir.dt.int32
AF = mybir.ActivationFunctionType
ALU = mybir.AluOpType
AX = mybir.AxisListType

NC = 8
SEQ = 8192
D = 2048
TL = 1024
N_IN = 5382
ALPHA = (2.0 * 2) ** 0.25
LN_EPS = 1e-5
PI = math.pi

SAME_ENGINE_SYNC = True
NEXP = 32
CSTOP = 9
LOOK = 3
PVLAG = 2
NDUMMY = 0


class Res:
    __slots__ = ("last_w", "readers")

    def __init__(self):
        self.last_w = None
        self.readers = []


class Tile:
    __slots__ = ("t", "r")

    def __init__(self, t):
        self.t = t
        self.r = Res()


class Op:
    __slots__ = ("eng", "fn", "deps", "sig", "is_dma", "dsem", "dtarget", "has_dep", "waits")

    def __init__(self, eng, fn, is_dma):
        self.eng = eng
        self.fn = fn
        self.deps = []
        self.sig = 0
        self.is_dma = is_dma
        self.dsem = None
        self.dtarget = 0
        self.has_dep = False
        self.waits = []


class Prog:
    ENGS = ("pe", "act", "dve", "pool", "sp")

    def __init__(self, nc, st, n_dma_sems=8):
        self.nc = nc
        self.st = st
        self.ops = []
        self.n_dma_sems = n_dma_sems
        self._n = 0

    def tile(self, shape, dtype, name=None):
        self._n += 1
        return Tile(self.st.enter_context(self.nc.sbuf_tensor("%s_%d" % (name or "t", self._n), shape, dtype)))

    def psum(self, shape, dtype=F32, name=None):
        self._n += 1
        return Tile(self.st.enter_context(self.nc.psum_tensor("%s_%d" % (name or "p", self._n), shape, dtype)))

    def emit(self, eng, fn, reads=(), writes=(), is_dma=False):
        op = Op(eng, fn, is_dma)
        deps = {}
        for r in reads:
            if r.last_w is not None:
                deps[id(r.last_w)] = r.last_w
        for w in writes:
            if w.last_w is not None:
                deps[id(w.last_w)] = w.last_w
            for rd in w.readers:
                deps[id(rd)] = rd
        op.deps = list(deps.values())
        for d in op.deps:
            d.has_dep = True
        for r in reads:
            r.readers.append(op)
        for w in writes:
            w.last_w = op
            w.readers = []
        self.ops.append(op)
        return op

    def dma(self, eng, out, in_, reads=(), writes=()):
        return self.emit(eng, lambda e: e.dma_start(out=out, in_=in_), reads, writes, is_dma=True)

    def finalize(self):
        nc, st = self.nc, self.st
        esem = {e: st.enter_context(nc.semaphore("s_" + e)) for e in self.ENGS}
        dsems = {q: [st.enter_context(nc.semaphore("d_%s%d" % (q, i))) for i in range(self.n_dma_sems)]
                 for q in ("sp", "pool")}
        dcount = {q: [0] * self.n_dma_sems for q in dsems}
        drr = {q: 0 for q in dsems}
        ecount = {e: 0 for e in self.ENGS}
        known = {e: {} for e in self.ENGS}
        per_eng = {e: [] for e in self.ENGS}
        for op in self.ops:
            waits = {}
            k = known[op.eng]

            def need(sem, val):
                key = id(sem)
                if k.get(key, 0) >= val:
                    return
                if key not in waits or waits[key][1] < val:
                    waits[key] = (sem, val)

            for d in op.deps:
                if d.is_dma:
                    need(d.dsem, d.dtarget)
                else:
                    if d.eng == op.eng and (d.eng == "pe" or not SAME_ENGINE_SYNC):
                        continue
                    need(esem[d.eng], d.sig)
            if op.is_dma:
                q = op.eng
                i = drr[q]
                drr[q] = (i + 1) % self.n_dma_sems
                sem = dsems[q][i]
                if dcount[q][i] > 0:
                    need(sem, dcount[q][i] * 16)
                dcount[q][i] += 1
                op.dsem = sem
                op.dtarget = dcount[q][i] * 16
            elif op.has_dep:
                ecount[op.eng] += 1
                op.sig = ecount[op.eng]
            op.waits = list(waits.values())
            for sem, val in op.waits:
                k[id(sem)] = val
            per_eng[op.eng].append(op)
        fin = []
        for q in dsems:
            for i, sem in enumerate(dsems[q]):
                if dcount[q][i] > 0:
                    fin.append((sem, dcount[q][i] * 16))
        for e in self.ENGS:
            if ecount[e] > 0:
                fin.append((esem[e], ecount[e]))
        block = st.enter_context(nc.Block())

        def replay(name, last=False):
            def body(e):
                for op in per_eng[name]:
                    for sem, val in op.waits:
                        e.wait_ge(sem, val)
                    inst = op.fn(e)
                    if op.is_dma:
                        inst.then_inc(op.dsem, 16)
                    elif op.sig:
                        inst.then_inc(esem[name], 1)
                if last:
                    for sem, val in fin:
                        e.wait_ge(sem, val)
            return body

        block.tensor(replay("pe"))
        block.scalar(replay("act"))
        block.vector(replay("dve"))
        block.gpsimd(replay("pool"))
        block.sync(replay("sp", True))


class Ring:
    def __init__(self, tiles):
        self.tiles = tiles
        self.i = 0

    def next(self):
        t = self.tiles[self.i % len(self.tiles)]
        self.i += 1
        return t


def din(nc, name, shape, dt=F32):
    return nc.dram_tensor(name, list(shape), dt, kind="ExternalInput").ap()


def dout(nc, name, shape, dt=F32):
    return nc.dram_tensor(name, list(shape), dt, kind="ExternalOutput").ap()


_ev = [0]


def evac_eng():
    _ev[0] += 1
    return "act" if _ev[0] % 2 else "dve"


def copy_op(P, eng, out, in_, reads, writes, scale=None):
    if eng == "act":
        if scale is None:
            P.emit("act", lambda e: e.activation(out=out, in_=in_, func=AF.Copy), reads, writes)
        else:
            P.emit("act", lambda e: e.activation(out=out, in_=in_, func=AF.Copy, scale=float(scale)), reads, writes)
    else:
        if scale is None:
            P.emit("dve", lambda e: e.tensor_copy(out=out, in_=in_), reads, writes)
        else:
            P.emit("dve", lambda e: e.tensor_scalar_mul(out=out, in0=in_, scalar1=float(scale)), reads, writes)


def transpose_to_xT(P, src_ap_fn, ident, xT, nchunks=16):
    hts = Ring([P.tile([128, nchunks * 128], F32, "ht") for _ in range(2)])
    pst = Ring([P.psum([128, 512], F32, "pst") for _ in range(2)])
    for j in range(8):
        ht = hts.next()
        P.dma("sp", ht.t[:], src_ap_fn(j), writes=[ht.r])
        for g in range(nchunks // 4):
            ps = pst.next()
            for q in range(4):
                kc = g * 4 + q
                P.emit("pe", lambda e, ps=ps, q=q, ht=ht, kc=kc: e.transpose(
                    ps.t[:, q * 128:(q + 1) * 128], ht.t[:, kc * 128:(kc + 1) * 128], ident.t[:]),
                    reads=[ht.r, ident.r], writes=[ps.r])
            copy_op(P, evac_eng(), xT.t[:, g * 4:(g + 1) * 4, j * 128:(j + 1) * 128],
                    ps.t[:].rearrange("p (a b) -> p a b", a=4), [ps.r], [xT.r])


def phase_A(nc, io):
    with ExitStack() as st:
        P = Prog(nc, st)
        ident = P.tile([128, 128], F32, "ident")
        P.dma("sp", ident.t[:], io["ident"], writes=[ident.r])
        xT = P.tile([128, 16, TL], BF16, "xT")
        transpose_to_xT(P, lambda j: io["hin"][j * 128:(j + 1) * 128, :], ident, xT)

        posi = P.tile([128, 8], I32, "posi")
        posf = P.tile([128, 8], F32, "posf")
        invf = P.tile([128, 8], F32, "invf")
        ang = P.tile([128, 8, 8], F32, "ang")
        red = P.tile([128, 8, 8], F32, "red")
        cost = P.tile([128, 8, 8], F32, "cos")
        sint = P.tile([128, 8, 8], F32, "sin")
        bfb = P.tile([128, 6], F32, "bfb")
        P.dma("sp", posi.t[:], io["pos"], writes=[posi.r])
        P.dma("sp", invf.t[:], io["invf"], writes=[invf.r])
        P.dma("sp", bfb.t[:], io["b_f"].partition_broadcast(128), writes=[bfb.r])
        P.emit("dve", lambda e: e.tensor_copy(out=posf.t[:], in_=posi.t[:]), [posi.r], [posf.r])
        P.emit("dve", lambda e: e.tensor_tensor(out=ang.t[:], in0=posf.t[:].unsqueeze(2).to_broadcast([128, 8, 8]),
                                                in1=invf.t[:].unsqueeze(1).to_broadcast([128, 8, 8]), op=ALU.mult),
               [posf.r, invf.r], [ang.r])
        angi = P.tile([128, 8, 8], I32, "angi")
        angf = P.tile([128, 8, 8], F32, "angf")
        ang2 = P.tile([128, 8, 8], F32, "ang2")
        for (shift, dst) in ((0.0, sint), (0.5 * PI, cost)):
            P.emit("dve", lambda e, shift=shift: e.tensor_scalar_add(out=ang2.t[:], in0=ang.t[:], scalar1=float(shift)), [ang.r], [ang2.r])
            P.emit("dve", lambda e: e.tensor_scalar_mul(out=red.t[:], in0=ang2.t[:], scalar1=float(1.0 / (2 * PI))), [ang2.r], [red.r])
            P.emit("dve", lambda e: e.tensor_copy(out=angi.t[:], in_=red.t[:]), [red.r], [angi.r])
            P.emit("dve", lambda e: e.tensor_copy(out=angf.t[:], in_=angi.t[:]), [angi.r], [angf.r])
            P.emit("dve", lambda e: e.scalar_tensor_tensor(out=red.t[:], in0=angf.t[:], scalar=float(-2 * PI), in1=ang2.t[:], op0=ALU.mult, op1=ALU.add),
                   [angf.r, ang2.r], [red.r])
            P.emit("dve", lambda e: e.tensor_single_scalar(out=angf.t[:], in_=red.t[:], scalar=float(PI), op=ALU.is_gt), [red.r], [angf.r])
            P.emit("dve", lambda e: e.scalar_tensor_tensor(out=red.t[:], in0=angf.t[:], scalar=float(-2 * PI), in1=red.t[:], op0=ALU.mult, op1=ALU.add),
                   [angf.r, red.r], [red.r])
            P.emit("dve", lambda e: e.tensor_single_scalar(out=angf.t[:], in_=red.t[:], scalar=float(-PI), op=ALU.is_lt), [red.r], [angf.r])
            P.emit("dve", lambda e: e.scalar_tensor_tensor(out=red.t[:], in0=angf.t[:], scalar=float(2 * PI), in1=red.t[:], op0=ALU.mult, op1=ALU.add),
                   [angf.r, red.r], [red.r])
            P.emit("act", lambda e, dst=dst: e.activation(out=dst.t[:], in_=red.t[:], func=AF.Sin), [red.r], [dst.r])

        wbufs = Ring([P.tile([128, 16, 512], BF16, "w") for _ in range(3)])
        psA = Ring([P.psum([128, 512], F32, "psA") for _ in range(3)])

        def load_w(c0, ncols):
            wb = wbufs.next()
            P.dma("pool", wb.t[:, :, 0:ncols], io["w_in"][:, c0:c0 + ncols].rearrange("(kc p) n -> p kc n", p=128),
                  writes=[wb.r])
            return wb

        otf = Ring([P.tile([128, TL], F32, "otf") for _ in range(2)])
        otb = Ring([P.tile([128, TL], BF16, "otb") for _ in range(2)])

        def feat_chunk(c0, nblk, out_ap, scale, is_bf):
            wb = load_w(c0, nblk * 128)
            for b in range(nblk):
                ot = (otb if is_bf else otf).next()
                for th in range(2):
                    ps = psA.next()
                    for kc in range(16):
                        P.emit("pe", lambda e, ps=ps, wb=wb, kc=kc, b=b, th=th: e.matmul(
                            ps.t[:], lhsT=wb.t[:, kc, b * 128:(b + 1) * 128], rhs=xT.t[:, kc, th * 512:(th + 1) * 512],
                            start=(kc == 0), stop=(kc == 15)), reads=[wb.r, xT.r], writes=[ps.r])
                    copy_op(P, evac_eng(), ot.t[:, th * 512:(th + 1) * 512], ps.t[:], [ps.r], [ot.r], scale)
                P.dma("sp", out_ap(b), ot.t[:], reads=[ot.r])

        def tok_chunk(c0, ncols, handler):
            wb = load_w(c0, ncols)
            for j in range(8):
                ps = psA.next()
                for kc in range(16):
                    P.emit("pe", lambda e, ps=ps, wb=wb, kc=kc, j=j: e.matmul(
                        ps.t[:, 0:ncols], lhsT=xT.t[:, kc, j * 128:(j + 1) * 128], rhs=wb.t[:, kc, 0:ncols],
                        start=(kc == 0), stop=(kc == 15)), reads=[wb.r, xT.r], writes=[ps.r])
                handler(j, ps)

        feat_chunk(0, 4, lambda b: io["qTf"][b * 128:(b + 1) * 128, :], 128 ** -0.5, True)
        feat_chunk(512, 2, lambda b: io["qTf"][(4 + b) * 128:(5 + b) * 128, :], 128 ** -0.5, True)
        feat_chunk(768, 4, lambda b: io["kTf"][b * 128:(b + 1) * 128, :], None, True)
        feat_chunk(1280, 2, lambda b: io["kTf"][(4 + b) * 128:(5 + b) * 128, :], None, True)
        feat_chunk(2310, 4, lambda b: io["xrT"][b * 128:(b + 1) * 128, :], None, False)
        feat_chunk(2310 + 512, 2, lambda b: io["xrT"][(4 + b) * 128:(5 + b) * 128, :], None, False)
        feat_chunk(3078, 4, lambda b: io["grT"][b * 128:(b + 1) * 128, :], None, False)
        feat_chunk(3078 + 512, 2, lambda b: io["grT"][(4 + b) * 128:(5 + b) * 128, :], None, False)

        vts = Ring([P.tile([128, 512], BF16, "vt") for _ in range(2)])

        def h_v(key, col0, ncols):
            def h(j, ps):
                vt = vts.next()
                copy_op(P, evac_eng(), vt.t[:, 0:ncols], ps.t[:, 0:ncols], [ps.r], [vt.r])
                P.dma("sp", io[key][j * 128:(j + 1) * 128, col0:col0 + ncols], vt.t[:, 0:ncols], reads=[vt.r])
            return h

        tok_chunk(1536, 512, h_v("vf", 0, 512))
        logf = P.tile([128, 8, 6], F32, "logf")
        zt = P.tile([128, 6], F32, "zt")
        h_v2 = h_v("vf", 512, 256)

        def h_vfa(j, ps):
            P.emit("dve", lambda e: e.tensor_tensor(out=zt.t[:], in0=ps.t[:, 256:262], in1=bfb.t[:], op=ALU.add),
                   [ps.r, bfb.r], [zt.r])
            P.emit("act", lambda e: e.activation(out=zt.t[:], in_=zt.t[:], func=AF.Exp, scale=-1.0), [zt.r], [zt.r])
            P.emit("act", lambda e: e.activation(out=zt.t[:], in_=zt.t[:], func=AF.Ln, bias=1.0), [zt.r], [zt.r])
            P.emit("dve", lambda e: e.tensor_scalar_mul(out=logf.t[:, j, :], in0=zt.t[:], scalar1=-1.0), [zt.r], [logf.r])
            h_v2(j, ps)

        tok_chunk(2048, 262, h_vfa)
        P.dma("sp", io["logf"].rearrange("(j p) h -> p j h", p=128), logf.t[:], reads=[logf.r])

        qk = P.tile([128, 512], F32, "qk")
        ta, tb, tc_, td = [P.tile([128, 8, 8], F32, "rt") for _ in range(4)]
        pstr = Ring([P.psum([128, 512], F32, "pstr") for _ in range(2)])

        def h_qk(key, scale):
            qTd = P.tile([128, 4, TL], BF16, "qTd")

            def h(j, ps):
                copy_op(P, "act", qk.t[:], ps.t[:], [ps.r], [qk.r], scale)
                v3 = qk.t[:].rearrange("p (s d) -> p s d", s=8)
                t1, t2 = v3[:, :, 0:8], v3[:, :, 8:16]
                cb = cost.t[:, j, :].unsqueeze(1).to_broadcast([128, 8, 8])
                sb = sint.t[:, j, :].unsqueeze(1).to_broadcast([128, 8, 8])
                rd = [qk.r, cost.r, sint.r]
                P.emit("dve", lambda e: e.tensor_tensor(out=ta.t[:], in0=t1, in1=cb, op=ALU.mult), rd, [ta.r])
                P.emit("dve", lambda e: e.tensor_tensor(out=tb.t[:], in0=t2, in1=sb, op=ALU.mult), rd, [tb.r])
                P.emit("dve", lambda e: e.tensor_tensor(out=tc_.t[:], in0=t2, in1=cb, op=ALU.mult), rd, [tc_.r])
                P.emit("dve", lambda e: e.tensor_tensor(out=td.t[:], in0=t1, in1=sb, op=ALU.mult), rd, [td.r])
                P.emit("dve", lambda e: e.tensor_tensor(out=t1, in0=ta.t[:], in1=tb.t[:], op=ALU.subtract), [ta.r, tb.r], [qk.r])
                P.emit("dve", lambda e: e.tensor_tensor(out=t2, in0=tc_.t[:], in1=td.t[:], op=ALU.add), [tc_.r, td.r], [qk.r])
                pt = pstr.next()
                for b in range(4):
                    P.emit("pe", lambda e, b=b, pt=pt: e.transpose(pt.t[:, b * 128:(b + 1) * 128], qk.t[:, b * 128:(b + 1) * 128], ident.t[:]),
                           [qk.r, ident.r], [pt.r])
                copy_op(P, evac_eng(), qTd.t[:, :, j * 128:(j + 1) * 128], pt.t[:].rearrange("p (a b) -> p a b", a=4), [pt.r], [qTd.r])
                if j == 7:
                    P.dma("sp", io[key].rearrange("(b p) t -> p b t", p=128), qTd.t[:], reads=[qTd.r])
            return h

        tok_chunk(3846, 512, h_qk("qTd", 64 ** -0.5))
        tok_chunk(4358, 512, h_qk("kTd", None))
        tok_chunk(4870, 512, h_v("vd", 0, 512))
        P.finalize()
    nc.all_engine_barrier()


def build_A():
    nc = bass.Bass("TRN2", target_bir_lowering=False)
    io = dict(
        hin=din(nc, "hin", [TL, D]), w_in=din(nc, "w_in", [D, N_IN]), b_f=din(nc, "b_f", [6]),
        pos=din(nc, "pos", [128, 8], I32), invf=din(nc, "invf", [128, 8]), ident=din(nc, "ident", [128, 128]),
        qTf=dout(nc, "qTf", [768, TL], BF16), kTf=dout(nc, "kTf", [768, TL], BF16), vf=dout(nc, "vf", [TL, 768], BF16),
        logf=dout(nc, "logf", [TL, 6]), xrT=dout(nc, "xrT", [768, TL]), grT=dout(nc, "grT", [768, TL]),
        qTd=dout(nc, "qTd", [512, TL], BF16), kTd=dout(nc, "kTd", [512, TL], BF16), vd=dout(nc, "vd", [TL, 512], BF16),
    )
    phase_A(nc, io)
    return nc


def own_rows(c):
    return np.concatenate([np.arange((8 * j + c) * 128, (8 * j + c + 1) * 128) for j in range(8)])


IDENT = np.eye(128, dtype=np.float32)
INVF = np.broadcast_to((np.float32(500000.0) ** (-np.arange(8, dtype=np.float32) * np.float32(2.0) / np.float32(16))).astype(np.float32), (128, 8)).copy()


def run(nc, in_maps):
    res = run_bass_kernel_spmd(nc, in_maps, core_ids=list(range(NC)))
    return res.results


def launch_A(h_shards, w_in_l, b_f_l, positions):
    nc = build_A()
    in_maps = []
    for c in range(NC):
        in_maps.append(dict(hin=h_shards[c], w_in=w_in_l, b_f=b_f_l,
                            pos=np.ascontiguousarray(positions[own_rows(c)].reshape(8, 128).T), invf=INVF, ident=IDENT))
    return run(nc, in_maps)


def split3(P, src, cur, back, n, npart, sign):
    outs = []
    P.emit("dve", lambda e: e.tensor_scalar_mul(out=cur.t[:], in0=src.t[:], scalar1=float(sign)), [src.r], [cur.r])
    for i in range(3):
        o = P.tile([npart, n], BF16, "s3o")
        P.emit("dve", lambda e, o=o: e.tensor_copy(out=o.t[:], in_=cur.t[:]), [cur.r], [o.r])
        if i < 2:
            P.emit("dve", lambda e, o=o: e.tensor_copy(out=back.t[:], in_=o.t[:]), [o.r], [back.r])
            P.emit("dve", lambda e: e.tensor_tensor(out=cur.t[:], in0=cur.t[:], in1=back.t[:], op=ALU.subtract), [cur.r, back.r], [cur.r])
        outs.append(o)
    return outs


def phase_B(nc, io, lam_init):
    with ExitStack() as st:
        P = Prog(nc, st)
        CH = 2048
        cw = P.tile([128, 4], F32, "cw"); cb = P.tile([128, 1], F32, "cb")
        wa32 = P.tile([128, 128], F32, "wa32"); wi32 = P.tile([128, 128], F32, "wi32")
        wa = P.tile([128, 128], BF16, "wa"); wi = P.tile([128, 128], BF16, "wi")
        ba = P.tile([128, 1], F32, "ba"); bi = P.tile([128, 1], F32, "bi"); lm = P.tile([128, 1], F32, "lm")
        one = P.tile([128, 1], F32, "one"); c8 = P.tile([128, 1], F32, "c8")
        for t_, k_ in ((cw, "conv_w"), (cb, "conv_b"), (wa32, "w_a"), (wi32, "w_i"), (ba, "b_a"), (bi, "b_i"), (lm, "lru_lam")):
            P.dma("sp", t_.t[:], io[k_], writes=[t_.r])
        P.emit("dve", lambda e: e.tensor_copy(out=wa.t[:], in_=wa32.t[:]), [wa32.r], [wa.r])
        P.emit("dve", lambda e: e.tensor_copy(out=wi.t[:], in_=wi32.t[:]), [wi32.r], [wi.r])
        P.emit("dve", lambda e: e.memset(one.t[:], 1.0), [], [one.r])
        P.emit("act", lambda e: e.activation(out=c8.t[:], in_=lm.t[:], func=AF.Exp, scale=-1.0), [lm.r], [c8.r])
        P.emit("act", lambda e: e.activation(out=c8.t[:], in_=c8.t[:], func=AF.Ln, bias=one.t[:, 0:1]), [c8.r, one.r], [c8.r])
        P.emit("dve", lambda e: e.tensor_scalar_mul(out=c8.t[:], in0=c8.t[:], scalar1=-8.0), [c8.r], [c8.r])
        xpad = P.tile([128, CH + 3], F32, "xpad"); xc = P.tile([128, CH], F32, "xc"); xcb = P.tile([128, CH], BF16, "xcb")
        rr = P.tile([128, CH], F32, "rr"); ii = P.tile([128, CH], F32, "ii"); tmp = P.tile([128, CH], F32, "tmp")
        hh = Ring([P.tile([128, CH], F32, "hh") for _ in range(2)]); gg = P.tile([128, CH], F32, "gg"); gt2 = P.tile([128, CH], F32, "gt2")
        psl = Ring([P.psum([128, 512], F32, "psl") for _ in range(2)])
        hprev = None
        for ck in range(SEQ // CH):
            t0 = ck * CH
            P.dma("sp", xpad.t[:], io["xr"][:, t0:t0 + CH + 3], writes=[xpad.r])
            P.dma("sp", gg.t[:], io["gr"][:, t0:t0 + CH], writes=[gg.r])
            P.emit("dve", lambda e: e.tensor_scalar(out=xc.t[:], in0=xpad.t[:, 0:CH], scalar1=cw.t[:, 0:1], scalar2=cb.t[:, 0:1],
                                                    op0=ALU.mult, op1=ALU.add), [xpad.r, cw.r, cb.r], [xc.r])
            for j in range(1, 4):
                P.emit("dve", lambda e, j=j: e.scalar_tensor_tensor(out=xc.t[:], in0=xpad.t[:, j:j + CH], scalar=cw.t[:, j:j + 1], in1=xc.t[:],
                                                                    op0=ALU.mult, op1=ALU.add), [xpad.r, cw.r, xc.r], [xc.r])
            P.emit("dve", lambda e: e.tensor_copy(out=xcb.t[:], in_=xc.t[:]), [xc.r], [xcb.r])
            for s in range(CH // 512):
                sl = slice(s * 512, (s + 1) * 512)
                for (wt, bt, dst) in ((wa, ba, rr), (wi, bi, ii)):
                    ps = psl.next()
                    P.emit("pe", lambda e, ps=ps, wt=wt, sl=sl: e.matmul(ps.t[:], lhsT=wt.t[:], rhs=xcb.t[:, sl], start=True, stop=True),
                           [wt.r, xcb.r], [ps.r])
                    P.emit("act", lambda e, ps=ps, bt=bt, dst=dst, sl=sl: e.activation(out=dst.t[:, sl], in_=ps.t[:], func=AF.Sigmoid, bias=bt.t[:, 0:1]),
                           [ps.r, bt.r], [dst.r])
            P.emit("act", lambda e: e.activation(out=rr.t[:], in_=rr.t[:], func=AF.Exp, scale=c8.t[:, 0:1]), [rr.r, c8.r], [rr.r])
            P.emit("dve", lambda e: e.tensor_tensor(out=tmp.t[:], in0=rr.t[:], in1=rr.t[:], op=ALU.mult), [rr.r], [tmp.r])
            P.emit("act", lambda e: e.activation(out=tmp.t[:], in_=tmp.t[:], func=AF.Sqrt, scale=-1.0, bias=one.t[:, 0:1]), [tmp.r, one.r], [tmp.r])
            P.emit("dve", lambda e: e.tensor_tensor(out=ii.t[:], in0=ii.t[:], in1=xc.t[:], op=ALU.mult), [ii.r, xc.r], [ii.r])
            P.emit("dve", lambda e: e.tensor_tensor(out=ii.t[:], in0=ii.t[:], in1=tmp.t[:], op=ALU.mult), [ii.r, tmp.r], [ii.r])
            hcur = hh.next()
            if hprev is None:
                P.emit("dve", lambda e, hcur=hcur: e.tensor_tensor_scan(out=hcur.t[:], data0=rr.t[:], data1=ii.t[:], initial=0.0,
                                                                        op0=ALU.mult, op1=ALU.add), [rr.r, ii.r], [hcur.r])
            else:
                P.emit("dve", lambda e, hcur=hcur, hp=hprev: e.tensor_tensor_scan(out=hcur.t[:], data0=rr.t[:], data1=ii.t[:],
                                                                                  initial=hp.t[:, CH - 1:CH], op0=ALU.mult, op1=ALU.add),
                       [rr.r, ii.r, hprev.r], [hcur.r])
            hprev = hcur
            P.emit("pool", lambda e: e.tensor_tensor(out=gt2.t[:], in0=gg.t[:], in1=gg.t[:], op=ALU.mult), [gg.r], [gt2.r])
            P.emit("pool", lambda e: e.tensor_scalar(out=gt2.t[:], in0=gt2.t[:], scalar1=0.044715, scalar2=1.0, op0=ALU.mult, op1=ALU.add), [gt2.r], [gt2.r])
            P.emit("pool", lambda e: e.tensor_tensor(out=gt2.t[:], in0=gt2.t[:], in1=gg.t[:], op=ALU.mult), [gt2.r, gg.r], [gt2.r])
            P.emit("act", lambda e: e.activation(out=gt2.t[:], in_=gt2.t[:], func=AF.Sigmoid, scale=2.0 * math.sqrt(2.0 / math.pi)), [gt2.r], [gt2.r])
            P.emit("pool", lambda e: e.tensor_tensor(out=gt2.t[:], in0=gt2.t[:], in1=gg.t[:], op=ALU.mult), [gt2.r, gg.r], [gt2.r])
            P.emit("dve", lambda e, hcur=hcur: e.tensor_tensor(out=tmp.t[:], in0=gt2.t[:], in1=hcur.t[:], op=ALU.mult), [gt2.r, hcur.r], [tmp.r])
            P.dma("sp", io["lru"][:, t0:t0 + CH], tmp.t[:], reads=[tmp.r])
        P.finalize()
    nc.all_engine_barrier()

    with ExitStack() as st:
        P = Prog(nc, st)
        kaug_d = nc.dram_tensor("kaug_d", [6, 6, SEQ], BF16).ap()
        qaug_d = nc.dram_tensor("qaug_d", [6, 6, TL], BF16).ap()
        lf = P.tile([6, SEQ], F32, "lf"); ones6 = P.tile([6, SEQ], BF16, "ones6"); cum = P.tile([6, SEQ], F32, "cum")
        om = P.tile([6, SEQ], F32, "om"); cown = P.tile([6, TL], F32, "cown")
        qcur = P.tile([6, TL], F32, "qcur"); qback = P.tile([6, TL], F32, "qback")
        P.dma("sp", lf.t[:], io["logfT"], writes=[lf.r])
        P.dma("sp", om.t[:], io["ownmask"], writes=[om.r])
        P.emit("dve", lambda e: e.memset(ones6.t[:], 1.0), [], [ones6.r])
        P.emit("dve", lambda e: e.tensor_tensor_scan(out=cum.t[:], data0=ones6.t[:], data1=lf.t[:], initial=0.0, op0=ALU.mult, op1=ALU.add),
               [ones6.r, lf.r], [cum.r])
        P.emit("dve", lambda e: e.tensor_tensor(out=om.t[:], in0=om.t[:], in1=cum.t[:], op=ALU.mult), [om.r, cum.r], [om.r])
        P.emit("dve", lambda e: e.tensor_reduce(out=cown.t[:].rearrange("p (j i) -> p j i", j=8),
                                                in_=om.t[:].rearrange("p (j r i) -> p j i r", j=8, r=8, i=128), axis=AX.X, op=ALU.add),
               [om.r], [cown.r])
        q3 = split3(P, cown, qcur, qback, TL, 6, 1.0)
        k3 = split3(P, cum, lf, om, SEQ, 6, -1.0)
        for i in range(3):
            P.dma("sp", kaug_d[:, i, :], ones6.t[:], reads=[ones6.r])
            P.dma("sp", kaug_d[:, 3 + i, :], k3[i].t[:], reads=[k3[i].r])
            P.dma("sp", qaug_d[:, i, :], q3[i].t[:], reads=[q3[i].r])
            P.dma("sp", qaug_d[:, 3 + i, :], ones6.t[:, 0:TL], reads=[ones6.r])
        P.finalize()
    nc.all_engine_barrier()

    with ExitStack() as st:
        P = Prog(nc, st)
        mF = P.tile([128, 8, 128], BF16, "mF"); mD = P.tile([128, 8, 128], BF16, "mD")
        P.dma("sp", mF.t[:], io["maskF"], writes=[mF.r])
        P.dma("sp", mD.t[:], io["maskD"], writes=[mD.r])
        lq = [P.tile([128, 64], F32, "lq") for _ in range(4)]
        for t_, k_ in zip(lq, ("lam_q1", "lam_k1", "lam_q2", "lam_k2")):
            P.dma("sp", t_.t[:], io[k_].partition_broadcast(128), writes=[t_.r])
        s1 = P.tile([128, 1], F32, "s1"); s2 = P.tile([128, 1], F32, "s2"); nlam = P.tile([128, 1], F32, "nlam")
        epst = P.tile([128, 1], F32, "eps")
        P.emit("dve", lambda e: e.memset(epst.t[:], LN_EPS), [], [epst.r])
        for (a_, b_, s_) in ((lq[0], lq[1], s1), (lq[2], lq[3], s2)):
            P.emit("dve", lambda e, a_=a_, b_=b_: e.tensor_tensor(out=a_.t[:], in0=a_.t[:], in1=b_.t[:], op=ALU.mult), [a_.r, b_.r], [a_.r])
            P.emit("dve", lambda e, a_=a_, s_=s_: e.reduce_sum(out=s_.t[:], in_=a_.t[:], axis=AX.X), [a_.r], [s_.r])
            P.emit("act", lambda e, s_=s_: e.activation(out=s_.t[:], in_=s_.t[:], func=AF.Exp), [s_.r], [s_.r])
        P.emit("dve", lambda e: e.tensor_tensor(out=nlam.t[:], in0=s2.t[:], in1=s1.t[:], op=ALU.subtract), [s1.r, s2.r], [nlam.r])
        P.emit("dve", lambda e: e.tensor_scalar_add(out=nlam.t[:], in0=nlam.t[:], scalar1=-float(lam_init)), [nlam.r], [nlam.r])
        gsb = P.tile([128, 128], F32, "gsb")
        P.dma("sp", gsb.t[:], io["subln_g"].partition_broadcast(128), writes=[gsb.r])
        P.emit("dve", lambda e: e.tensor_scalar_mul(out=gsb.t[:], in0=gsb.t[:], scalar1=float(1.0 - lam_init)), [gsb.r], [gsb.r])

        kTs = Ring([P.tile([128, SEQ], BF16, "kT") for _ in range(2)])
        kaugs = Ring([P.tile([128, SEQ], BF16, "kaug") for _ in range(2)])
        qaugs = Ring([P.tile([128, TL], BF16, "qaug") for _ in range(2)])
        for t_ in kaugs.tiles + qaugs.tiles:
            P.emit("pool", lambda e, t_=t_: e.memset(t_.t[:], 0.0), [], [t_.r])
        vs = Ring([P.tile([128, 64, 130], BF16, "v") for _ in range(2)])
        qTs = Ring([P.tile([128, TL], BF16, "qT") for _ in range(2)])
        for _ in range(2):
            v_ = vs.next()
            P.emit("dve", lambda e, v_=v_: e.memset(v_.t[:, :, 128:130], 1.0), [], [v_.r])
        pss = Ring([P.psum([128, 512], F32, "pss") for _ in range(LOOK + 1)])
        pTs = Ring([P.tile([128, 512], BF16, "pT") for _ in range(PVLAG + 3)])
        o1s = Ring([P.tile([128, 128], F32, "o1") for _ in range(3)])
        rcs = Ring([P.tile([128, 1], F32, "rc") for _ in range(6)])
        junk = P.tile([128, 128], F32, "junk")

        class Acc:
            pass
        accbanks = [P.psum([128, 512], F32, "accb") for _ in range(4)]
        accs = []
        for j in range(8):
            a_ = Acc()
            a_.ap = accbanks[j % 4].t[:, 0:130]
            a_.r = accbanks[j % 4].r
            accs.append(a_)
        o1all = P.tile([128, 8, 128], F32, "o1all")
        o1res = [Res() for _ in range(8)]

        tasks = []
        head_loads = []

        def sel_head(kall, vall, qown, hidx, aug=None):
            kT, v_, qT = kTs.next(), vs.next(), qTs.next()
            ka = qa = None
            if aug is not None:
                ka, qa = kaugs.next(), qaugs.next()

            def load():
                P.dma("sp", kT.t[:], kall[hidx * 128:(hidx + 1) * 128, :], writes=[kT.r])
                P.dma("sp", v_.t[:, :, 0:128], vall.rearrange("(t p) d -> p t d", p=128)[:, :, hidx * 128:(hidx + 1) * 128], writes=[v_.r])
                P.dma("sp", qT.t[:], qown[hidx * 128:(hidx + 1) * 128, :], writes=[qT.r])
                if aug is not None:
                    P.dma("sp", ka.t[0:6, :], kaug_d[aug], writes=[ka.r])
                    P.dma("sp", qa.t[0:6, :], qaug_d[aug], writes=[qa.r])
            head_loads.append(load)
            return kT, v_, qT, ka, qa

        def add_map(qk_mm, v_, mask, clamp, fin_fn, head_first):
            first = True
            for ps_ in range(2):
                for kt in range(32 * (ps_ + 1)):
                    G = kt // 8
                    jlo, jhi = max(G, 4 * ps_), 4 * (ps_ + 1)
                    if jlo >= jhi:
                        continue
                    tasks.append(dict(kt=kt, G=G, jlo=jlo, jhi=jhi, qk=qk_mm, v=v_, mask=mask, clamp=clamp, fin=fin_fn,
                                      head_first=head_first if first else None))
                    first = False

        def fox_fin(h):
            def fin(j):
                acc = accs[j]
                rc = rcs.next(); o1 = o1s.next()
                P.emit("dve", lambda e: e.reciprocal(out=rc.t[:], in_=acc.ap[:, 128:129]), [acc.r], [rc.r])
                P.emit("dve", lambda e: e.tensor_scalar_mul(out=o1.t[:], in0=acc.ap[:, 0:128], scalar1=rc.t[:, 0:1]), [acc.r, rc.r], [o1.r])
                P.dma("sp", io["attn"][j * 128:(j + 1) * 128, h * 128:(h + 1) * 128], o1.t[:], reads=[o1.r])
            return fin

        def diff_fin0():
            def fin(j):
                acc = accs[j]
                rc1 = rcs.next()
                P.emit("dve", lambda e: e.reciprocal(out=rc1.t[:], in_=acc.ap[:, 128:129]), [acc.r], [rc1.r])
                P.emit("dve", lambda e: e.tensor_scalar_mul(out=o1all.t[:, j, :], in0=acc.ap[:, 0:128], scalar1=rc1.t[:, 0:1]), [acc.r, rc1.r], [o1res[j]])
            return fin

        def diff_fin1(hd):
            def fin(j):
                acc = accs[j]
                rc2, ss = rcs.next(), rcs.next()
                oc = o1s.next()
                P.emit("dve", lambda e: e.reciprocal(out=rc2.t[:], in_=acc.ap[:, 128:129]), [acc.r], [rc2.r])
                P.emit("dve", lambda e: e.tensor_tensor(out=rc2.t[:], in0=rc2.t[:], in1=nlam.t[:], op=ALU.mult), [rc2.r, nlam.r], [rc2.r])
                P.emit("dve", lambda e: e.scalar_tensor_tensor(out=oc.t[:], in0=acc.ap[:, 0:128], scalar=rc2.t[:, 0:1], in1=o1all.t[:, j, :],
                                                               op0=ALU.mult, op1=ALU.add), [acc.r, rc2.r, o1res[j]], [oc.r])
                P.emit("act", lambda e: e.activation(out=junk.t[:], in_=oc.t[:], func=AF.Square, accum_out=ss.t[:, 0:1]), [oc.r], [junk.r, ss.r])
                P.emit("act", lambda e: e.activation(out=ss.t[:], in_=ss.t[:], func=AF.Sqrt, scale=1.0 / 128.0, bias=epst.t[:, 0:1]), [ss.r, epst.r], [ss.r])
                P.emit("dve", lambda e: e.reciprocal(out=ss.t[:], in_=ss.t[:]), [ss.r], [ss.r])
                P.emit("dve", lambda e: e.scalar_tensor_tensor(out=oc.t[:], in0=oc.t[:], scalar=ss.t[:, 0:1], in1=gsb.t[:], op0=ALU.mult, op1=ALU.mult),
                       [oc.r, ss.r, gsb.r], [oc.r])
                P.dma("sp", io["attn"][j * 128:(j + 1) * 128, 768 + hd * 128:768 + (hd + 1) * 128], oc.t[:], reads=[oc.r])
            return fin

        hcount = 0
        for h in range(6):
            kT, v_, qT, ka, qa = sel_head(io["kTf_all"], io["vf_all"], io["qTf"], h, aug=h)

            def qk_mm(ps, kt, jlo, jhi, kT=kT, qT=qT, ka=ka, qa=qa):
                n = (jhi - jlo) * 128
                P.emit("pe", lambda e: e.matmul(ps.t[:, 0:n], lhsT=kT.t[:, kt * 128:(kt + 1) * 128], rhs=qT.t[:, jlo * 128:jhi * 128],
                                                start=True, stop=False), [kT.r, qT.r], [ps.r])
                P.emit("pe", lambda e: e.matmul(ps.t[:, 0:n], lhsT=ka.t[:, kt * 128:(kt + 1) * 128], rhs=qa.t[:, jlo * 128:jhi * 128],
                                                start=False, stop=True), [ka.r, qa.r], [ps.r])
            add_map(qk_mm, v_, mF, True, fox_fin(h), hcount)
            hcount += 1
        for hd in range(4):
            kT, v_, qT, _, _ = sel_head(io["kTd_all"], io["vd_all"], io["qTd"], hd)
            for m in range(2):
                def qk_mm(ps, kt, jlo, jhi, kT=kT, qT=qT, m=m):
                    n = (jhi - jlo) * 128
                    P.emit("pe", lambda e: e.matmul(ps.t[:, 0:n], lhsT=kT.t[m * 64:(m + 1) * 64, kt * 128:(kt + 1) * 128],
                                                    rhs=qT.t[m * 64:(m + 1) * 64, jlo * 128:jhi * 128], start=True, stop=True), [kT.r, qT.r], [ps.r])
                add_map(qk_mm, v_, mD, False, diff_fin0() if m == 0 else diff_fin1(hd), hcount if m == 0 else None)
            hcount += 1

        def emit_qk(t):
            ps = pss.next()
            t["ps"] = ps
            t["qk"](ps, t["kt"], t["jlo"], t["jhi"])

        def emit_clamp(t):
            ps = t["ps"]
            if t["clamp"] and t["jlo"] == t["G"]:
                P.emit("dve", lambda e: e.tensor_scalar_min(out=ps.t[:, 0:128], in0=ps.t[:, 0:128], scalar1=80.0), [ps.r], [ps.r])

        def emit_exp(t):
            ps, kt, G, jlo, jhi, mask = t["ps"], t["kt"], t["G"], t["jlo"], t["jhi"], t["mask"]
            n = (jhi - jlo) * 128
            pT = pTs.next()
            t["pT"] = pT
            diag = (jlo == G)
            P.emit("act", lambda e: e.activation(out=pT.t[:, 0:n], in_=ps.t[:, 0:n], func=AF.Exp), [ps.r], [pT.r])
            if diag:
                r = kt - 8 * G
                P.emit("dve", lambda e: e.tensor_tensor(out=pT.t[:, 0:128], in0=pT.t[:, 0:128], in1=mask.t[:, r, :], op=ALU.mult), [pT.r, mask.r], [pT.r])

        def emit_pv(t):
            pT, kt, G, jlo, jhi, v_ = t["pT"], t["kt"], t["G"], t["jlo"], t["jhi"], t["v"]
            diag = (jlo == G)
            hf = t["head_first"]
            if hf is not None and hf + 1 < len(head_loads):
                head_loads[hf + 1]()
            while pending:
                pending.pop(0)()
            for jj in range(jlo, jhi):
                acc = accs[jj]
                c0 = (jj - jlo) * 128
                P.emit("pe", lambda e, acc=acc, c0=c0, jj=jj: e.matmul(acc.ap, lhsT=pT.t[:, c0:c0 + 128], rhs=v_.t[:, kt, :],
                                                                   start=(kt == 0), stop=(kt == 8 * jj + 7)), [pT.r, v_.r], [acc.r])
            if kt % 8 == 7 and diag:
                pending.append(lambda f=t["fin"], G=G: f(G))
            for _ in range(NDUMMY):
                P.emit("pe", lambda e: e.matmul(dummy.t[:], lhsT=warm_l.t[:, 0:128], rhs=warm_r.t[:, 0:512], start=True, stop=True),
                       [warm_l.r, warm_r.r], [dummy.r])

        if NDUMMY:
            dummy = P.psum([128, 512], F32, "dummy")
            warm_l = P.tile([128, 128], BF16, "warm_l")
            warm_r = P.tile([128, 512], BF16, "warm_r")
            P.emit("pool", lambda e: e.memset(warm_l.t[:], 0.0), [], [warm_l.r])
            P.emit("pool", lambda e: e.memset(warm_r.t[:], 0.0), [], [warm_r.r])
        pending = []
        pending_prev = []
        head_loads[0]()
        for i in range(min(LOOK, len(tasks))):
            emit_qk(tasks[i])
        emit_clamp(tasks[0])
        for i in range(len(tasks) + PVLAG):
            if i + LOOK < len(tasks):
                emit_qk(tasks[i + LOOK])
            if i + 1 < len(tasks):
                emit_clamp(tasks[i + 1])
            if i < len(tasks):
                emit_exp(tasks[i])
            if i - PVLAG >= 0:
                emit_pv(tasks[i - PVLAG])
        for f_ in pending_prev + pending:
            f_()
        P.finalize()
    nc.all_engine_barrier()


def build_B(lam_init):
    nc = bass.Bass("TRN2", target_bir_lowering=False)
    io = dict(
        qTf=din(nc, "qTf", [768, TL], BF16), kTf_all=din(nc, "kTf_all", [768, SEQ], BF16), vf_all=din(nc, "vf_all", [SEQ, 768], BF16),
        logfT=din(nc, "logfT", [6, SEQ]), ownmask=din(nc, "ownmask", [6, SEQ]),
        maskF=din(nc, "maskF", [128, 8, 128], BF16), maskD=din(nc, "maskD", [128, 8, 128], BF16),
        qTd=din(nc, "qTd", [512, TL], BF16), kTd_all=din(nc, "kTd_all", [512, SEQ], BF16), vd_all=din(nc, "vd_all", [SEQ, 512], BF16),
        lam_q1=din(nc, "lam_q1", [64]), lam_k1=din(nc, "lam_k1", [64]), lam_q2=din(nc, "lam_q2", [64]), lam_k2=din(nc, "lam_k2", [64]),
        subln_g=din(nc, "subln_g", [128]),
        xr=din(nc, "xr", [128, SEQ + 3]), gr=din(nc, "gr", [128, SEQ]), conv_w=din(nc, "conv_w", [128, 4]), conv_b=din(nc, "conv_b", [128, 1]),
        w_a=din(nc, "w_a", [128, 128]), w_i=din(nc, "w_i", [128, 128]), b_a=din(nc, "b_a", [128, 1]), b_i=din(nc, "b_i", [128, 1]),
        lru_lam=din(nc, "lru_lam", [128, 1]),
        attn=dout(nc, "attn", [TL, 1280]), lru=dout(nc, "lru", [128, SEQ]),
    )
    phase_B(nc, io, lam_init)
    return nc


def global_order(parts, axis):
    full = np.concatenate(parts, axis=axis)
    perm = np.concatenate([own_rows(c) for c in range(NC)])
    inv = np.empty_like(perm)
    inv[perm] = np.arange(perm.size)
    return np.take(full, inv, axis=axis)


def core_masks(c):
    i = np.arange(128)
    tri = (i[:, None] <= i[None, :])
    blk = ((i[:, None] // 64) <= (i[None, :] // 64))
    mF = np.zeros((128, 8, 128), np.float32)
    mD = np.zeros((128, 8, 128), np.float32)
    for r in range(8):
        if r < c:
            mF[:, r, :] = 1.0
            mD[:, r, :] = 1.0
        elif r == c:
            mF[:, r, :] = tri
            mD[:, r, :] = blk
    om = np.zeros((6, SEQ), np.float32)
    om[:, own_rows(c)] = 1.0
    return mF.astype(ml_dtypes.bfloat16), mD.astype(ml_dtypes.bfloat16), om


def launch_B(resA, prm, l):
    lam_init = 0.8 - 0.6 * math.exp(-0.3 * l)
    nc = build_B(lam_init)
    kTf_all = global_order([r["kTf"] for r in resA], 1)
    vf_all = global_order([r["vf"] for r in resA], 0)
    kTd_all = global_order([r["kTd"] for r in resA], 1)
    vd_all = global_order([r["vd"] for r in resA], 0)
    logfT = np.ascontiguousarray(global_order([r["logf"] for r in resA], 0).T)
    xr_all = global_order([r["xrT"] for r in resA], 1)
    gr_all = global_order([r["grT"] for r in resA], 1)
    in_maps = []
    for c in range(NC):
        mF, mD, om = core_masks(c)
        n = c % 6
        sl = slice(n * 128, (n + 1) * 128)
        xr = np.zeros((128, SEQ + 3), np.float32)
        xr[:, 3:] = xr_all[sl]
        in_maps.append(dict(
            qTf=resA[c]["qTf"], kTf_all=kTf_all, vf_all=vf_all, logfT=logfT, ownmask=om, maskF=mF, maskD=mD,
            qTd=resA[c]["qTd"], kTd_all=kTd_all, vd_all=vd_all,
            lam_q1=prm["lam_q1"][l], lam_k1=prm["lam_k1"][l], lam_q2=prm["lam_q2"][l], lam_k2=prm["lam_k2"][l],
            subln_g=prm["subln_g"][l], xr=xr, gr=np.ascontiguousarray(gr_all[sl]),
            conv_w=np.ascontiguousarray(prm["conv_w"][l][:, sl].T), conv_b=np.ascontiguousarray(prm["conv_b"][l][sl, None]),
            w_a=np.ascontiguousarray(prm["w_a"][l][n]), w_i=np.ascontiguousarray(prm["w_i"][l][n]),
            b_a=np.ascontiguousarray(prm["b_a"][l][sl, None]), b_i=np.ascontiguousarray(prm["b_i"][l][sl, None]),
            lru_lam=np.ascontiguousarray(prm["lru_lambda"][l][sl, None])))
    return run(nc, in_maps)


def layer_norm_tiles(P, acc, accr, gt, bt, epst, junk_ap, sm):
    for j in range(8):
        a = acc.t[:, j, :]
        ar = accr[j]
        mu, ss = sm.next(), sm.next()
        P.emit("dve", lambda e, a=a, mu=mu: e.reduce_sum(out=mu.t[:], in_=a, axis=AX.X), [ar], [mu.r])
        P.emit("dve", lambda e, mu=mu: e.tensor_scalar_mul(out=mu.t[:], in0=mu.t[:], scalar1=-1.0 / D), [mu.r], [mu.r])
        P.emit("dve", lambda e, a=a, mu=mu: e.tensor_scalar_add(out=a, in0=a, scalar1=mu.t[:, 0:1]), [ar, mu.r], [ar])
        P.emit("act", lambda e, a=a, ss=ss: e.activation(out=junk_ap[0], in_=a, func=AF.Square, accum_out=ss.t[:, 0:1]), [ar], [junk_ap[1], ss.r])
        P.emit("act", lambda e, ss=ss: e.activation(out=ss.t[:], in_=ss.t[:], func=AF.Sqrt, scale=1.0 / D, bias=epst.t[:, 0:1]), [ss.r, epst.r], [ss.r])
        P.emit("dve", lambda e, ss=ss: e.reciprocal(out=ss.t[:], in_=ss.t[:]), [ss.r], [ss.r])
        P.emit("dve", lambda e, a=a, ss=ss: e.scalar_tensor_tensor(out=a, in0=a, scalar=ss.t[:, 0:1], in1=gt.t[:], op0=ALU.mult, op1=ALU.mult),
               [ar, ss.r, gt.r], [ar])
        P.emit("dve", lambda e, a=a: e.tensor_tensor(out=a, in0=a, in1=bt.t[:], op=ALU.add), [ar, bt.r], [ar])


def phase_C(nc, io):
    with ExitStack() as st:
        P = Prog(nc, st)
        ident = P.tile([128, 128], F32, "ident")
        P.dma("sp", ident.t[:], io["ident"], writes=[ident.r])
        epst = P.tile([128, 1], F32, "eps")
        P.emit("dve", lambda e: e.memset(epst.t[:], LN_EPS), [], [epst.r])
        acc = P.tile([128, 8, D], F32, "acc")
        accr = [Res() for _ in range(8)]
        xT = P.tile([128, 16, TL], BF16, "xT")
        lng = P.tile([128, D], F32, "lng"); lnb = P.tile([128, D], F32, "lnb")
        hid = P.tile([128, 4, TL], BF16, "hid")
        junk_ap = (hid.t[:].rearrange("p a b -> p (a b)")[:, 0:D], hid.r)
        wgs = Ring([P.tile([128, 16, 256], BF16, "wg") for _ in range(2)])
        wus = Ring([P.tile([128, 16, 256], BF16, "wu") for _ in range(2)])
        wds = Ring([P.tile([128, 4, D], BF16, "wd") for _ in range(2)])
        psr = Ring([P.psum([128, 512], F32, "ps") for _ in range(7)])
        sm = Ring([P.tile([128, 1], F32, "sm") for _ in range(12)])
        gate = P.tile([128, 8, 32], F32, "gate")
        wgr = P.tile([128, 16, 36], F32, "wgr")
        bgr = P.tile([128, 36], F32, "bgr")
        sgs = Ring([P.tile([128, 512], F32, "sg") for _ in range(2)])
        P.dma("sp", wgr.t[:], io["w_gr"].rearrange("(kc p) n -> p kc n", p=128), writes=[wgr.r])
        P.dma("sp", bgr.t[:], io["b_gr"].partition_broadcast(128), writes=[bgr.r])
        P.dma("sp", lng.t[:], io["ln1_g"].partition_broadcast(128), writes=[lng.r])
        P.dma("sp", lnb.t[:], io["ln1_b"].partition_broadcast(128), writes=[lnb.r])

        att = Ring([P.tile([128, 512], F32, "att") for _ in range(2)])
        for j in range(8):
            P.dma("sp", acc.t[:, j, :], io["hres"][j * 128:(j + 1) * 128, :], writes=[accr[j]])
            for (c0, n, d0) in ((0, 4, 0), (512, 2, 4), (768, 4, 12)):
                at = att.next()
                P.dma("sp", at.t[:, 0:n * 128], io["attn"][j * 128:(j + 1) * 128, c0:c0 + n * 128], writes=[at.r])
                ps = psr.next()
                for q in range(n):
                    P.emit("pe", lambda e, ps=ps, at=at, q=q: e.transpose(ps.t[:, q * 128:(q + 1) * 128], at.t[:, q * 128:(q + 1) * 128], ident.t[:]),
                           [at.r, ident.r], [ps.r])
                copy_op(P, evac_eng(), xT.t[:, d0:d0 + n, j * 128:(j + 1) * 128], ps.t[:, 0:n * 128].rearrange("p (a b) -> p a b", a=n), [ps.r], [xT.r])
        for b in range(6):
            P.dma("pool", xT.t[:, 6 + b, :], io["lruT"][b * 128:(b + 1) * 128, :], writes=[xT.r])

        wos = wgs.tiles + wus.tiles
        wi_ = 0
        for n in range(8):
            wt = wos[wi_ % len(wos)]; wi_ += 1
            P.dma("pool", wt.t[:], io["w_o"][:, n * 256:(n + 1) * 256].rearrange("(kc p) n -> p kc n", p=128), writes=[wt.r])
            for j in range(8):
                ps = psr.next()
                for kc in range(16):
                    P.emit("pe", lambda e, ps=ps, wt=wt, kc=kc, j=j: e.matmul(ps.t[:, 0:256], lhsT=xT.t[:, kc, j * 128:(j + 1) * 128], rhs=wt.t[:, kc, :],
                                                                            start=(kc == 0), stop=(kc == 15)), [xT.r, wt.r], [ps.r])
                a = acc.t[:, j, n * 256:(n + 1) * 256]
                P.emit("dve", lambda e, a=a, ps=ps: e.scalar_tensor_tensor(out=a, in0=a, scalar=float(ALPHA), in1=ps.t[:, 0:256], op0=ALU.mult, op1=ALU.add),
                       [accr[j], ps.r], [accr[j]])
        if CSTOP >= 2:
            layer_norm_tiles(P, acc, accr, lng, lnb, epst, junk_ap, sm)

        h1f = lng
        h1f_v = h1f.t[:].rearrange("p (a b) -> p a b", a=16)
        hlo = P.tile([128, 16, 128], BF16, "hlo"); whi = P.tile([128, 16, 36], BF16, "whi"); wlo = P.tile([128, 16, 36], BF16, "wlo")
        P.emit("dve", lambda e: e.tensor_copy(out=whi.t[:], in_=wgr.t[:]), [wgr.r], [whi.r])
        P.emit("dve", lambda e: e.tensor_tensor(out=wlo.t[:], in0=wgr.t[:], in1=whi.t[:], op=ALU.subtract), [wgr.r, whi.r], [wlo.r])
        lg = P.tile([128, 36], F32, "lg"); goh = P.tile([128, 4], F32, "goh"); ge = P.tile([128, 4], F32, "ge")
        esel = P.tile([128, 8], F32, "esel"); oh1 = P.tile([128, 8], F32, "oh1"); oh2 = P.tile([128, 8], F32, "oh2")
        msk = P.tile([128, 8], F32, "msk"); eg = P.tile([128, 8], F32, "eg")
        for j in range(8 if CSTOP >= 3.1 else 0):
            for g in range(4):
                ps = psr.next()
                for q in range(4):
                    kc = g * 4 + q
                    P.emit("pe", lambda e, ps=ps, q=q, kc=kc, j=j: e.transpose(ps.t[:, q * 128:(q + 1) * 128], acc.t[:, j, kc * 128:(kc + 1) * 128], ident.t[:]),
                           [accr[j], ident.r], [ps.r])
                P.emit("act", lambda e, ps=ps, g=g, j=j: e.activation(out=xT.t[:, g * 4:(g + 1) * 4, j * 128:(j + 1) * 128],
                                                                    in_=ps.t[:].rearrange("p (a b) -> p a b", a=4), func=AF.Copy), [ps.r], [xT.r])
                ps2 = psr.next()
                for q in range(4):
                    kc = g * 4 + q
                    P.emit("pe", lambda e, ps2=ps2, q=q, kc=kc, j=j: e.transpose(ps2.t[:, q * 128:(q + 1) * 128], acc.t[:, j, kc * 128:(kc + 1) * 128], ident.t[:]),
                           [accr[j], ident.r], [ps2.r])
                P.emit("dve", lambda e, ps2=ps2, g=g: e.tensor_copy(out=h1f_v[:, g * 4:(g + 1) * 4, :], in_=ps2.t[:].rearrange("p (a b) -> p a b", a=4)),
                       [ps2.r], [h1f.r])
            if CSTOP < 3.2:
                continue
            P.emit("dve", lambda e, j=j: e.tensor_tensor(out=hlo.t[:], in0=h1f_v, in1=xT.t[:, :, j * 128:(j + 1) * 128], op=ALU.subtract), [h1f.r, xT.r], [hlo.r])
            ps = psr.next()
            for kc in range(16):
                for ti, (lt_, lr_, rt_, rr_) in enumerate(((xT.t[:, kc, j * 128:(j + 1) * 128], xT.r, whi.t[:, kc, :], whi.r),
                                                           (xT.t[:, kc, j * 128:(j + 1) * 128], xT.r, wlo.t[:, kc, :], wlo.r),
                                                           (hlo.t[:, kc, :], hlo.r, whi.t[:, kc, :], whi.r))):
                    P.emit("pe", lambda e, ps=ps, lt_=lt_, rt_=rt_, kc=kc, ti=ti: e.matmul(ps.t[:, 0:36], lhsT=lt_, rhs=rt_, start=(kc == 0 and ti == 0), stop=(kc == 15 and ti == 2)),
                           [lr_, rr_], [ps.r])
            P.emit("dve", lambda e, ps=ps: e.tensor_tensor(out=lg.t[:], in0=ps.t[:, 0:36], in1=bgr.t[:], op=ALU.add), [ps.r, bgr.r], [lg.r])
            if CSTOP < 3.3:
                continue
            gmax, ngmax, gsum, m1, m2, dd, w1, w2 = [sm.next() for _ in range(8)]
            P.emit("dve", lambda e, gmax=gmax: e.reduce_max(out=gmax.t[:], in_=lg.t[:, 0:4], axis=AX.X), [lg.r], [gmax.r])
            P.emit("dve", lambda e, gmax=gmax: e.tensor_scalar(out=goh.t[:], in0=lg.t[:, 0:4], scalar1=gmax.t[:, 0:1], scalar2=None, op0=ALU.is_equal), [lg.r, gmax.r], [goh.r])
            P.emit("dve", lambda e, gmax=gmax, ngmax=ngmax: e.tensor_scalar_mul(out=ngmax.t[:], in0=gmax.t[:], scalar1=-1.0), [gmax.r], [ngmax.r])
            P.emit("act", lambda e, ngmax=ngmax, gsum=gsum: e.activation(out=ge.t[:], in_=lg.t[:, 0:4], func=AF.Exp, bias=ngmax.t[:, 0:1], accum_out=gsum.t[:, 0:1]),
                   [lg.r, ngmax.r], [ge.r, gsum.r])
            P.emit("dve", lambda e, gsum=gsum: e.reciprocal(out=gsum.t[:], in_=gsum.t[:]), [gsum.r], [gsum.r])
            if CSTOP < 3.4:
                continue
            P.emit("dve", lambda e: e.tensor_scalar_mul(out=esel.t[:], in0=lg.t[:, 4:12], scalar1=goh.t[:, 0:1]), [lg.r, goh.r], [esel.r])
            for g in range(1, 4):
                P.emit("dve", lambda e, g=g: e.scalar_tensor_tensor(out=esel.t[:], in0=lg.t[:, 4 + 8 * g:12 + 8 * g], scalar=goh.t[:, g:g + 1], in1=esel.t[:],
                                                                    op0=ALU.mult, op1=ALU.add), [lg.r, goh.r, esel.r], [esel.r])
            P.emit("dve", lambda e, m1=m1: e.reduce_max(out=m1.t[:], in_=esel.t[:], axis=AX.X), [esel.r], [m1.r])
            P.emit("dve", lambda e, m1=m1: e.tensor_scalar(out=oh1.t[:], in0=esel.t[:], scalar1=m1.t[:, 0:1], scalar2=None, op0=ALU.is_equal), [esel.r, m1.r], [oh1.r])
            P.emit("dve", lambda e: e.scalar_tensor_tensor(out=msk.t[:], in0=oh1.t[:], scalar=-1e30, in1=esel.t[:], op0=ALU.mult, op1=ALU.add), [oh1.r, esel.r], [msk.r])
            P.emit("dve", lambda e, m2=m2: e.reduce_max(out=m2.t[:], in_=msk.t[:], axis=AX.X), [msk.r], [m2.r])
            P.emit("dve", lambda e, m2=m2: e.tensor_scalar(out=oh2.t[:], in0=msk.t[:], scalar1=m2.t[:, 0:1], scalar2=None, op0=ALU.is_equal), [msk.r, m2.r], [oh2.r])
            if CSTOP < 3.5:
                continue
            P.emit("dve", lambda e, m1=m1, m2=m2, dd=dd: e.tensor_tensor(out=dd.t[:], in0=m2.t[:], in1=m1.t[:], op=ALU.subtract), [m1.r, m2.r], [dd.r])
            P.emit("act", lambda e, dd=dd: e.activation(out=dd.t[:], in_=dd.t[:], func=AF.Exp), [dd.r], [dd.r])
            P.emit("dve", lambda e, dd=dd, w1=w1: e.tensor_scalar_add(out=w1.t[:], in0=dd.t[:], scalar1=1.0), [dd.r], [w1.r])
            P.emit("dve", lambda e, w1=w1: e.reciprocal(out=w1.t[:], in_=w1.t[:]), [w1.r], [w1.r])
            P.emit("dve", lambda e, w1=w1, w2=w2, dd=dd: e.tensor_tensor(out=w2.t[:], in0=dd.t[:], in1=w1.t[:], op=ALU.mult), [dd.r, w1.r], [w2.r])
            P.emit("dve", lambda e, w1=w1, gsum=gsum: e.tensor_tensor(out=w1.t[:], in0=w1.t[:], in1=gsum.t[:], op=ALU.mult), [w1.r, gsum.r], [w1.r])
            P.emit("dve", lambda e, w2=w2, gsum=gsum: e.tensor_tensor(out=w2.t[:], in0=w2.t[:], in1=gsum.t[:], op=ALU.mult), [w2.r, gsum.r], [w2.r])
            P.emit("dve", lambda e, w1=w1: e.tensor_scalar_mul(out=eg.t[:], in0=oh1.t[:], scalar1=w1.t[:, 0:1]), [oh1.r, w1.r], [eg.r])
            P.emit("dve", lambda e, w2=w2: e.scalar_tensor_tensor(out=eg.t[:], in0=oh2.t[:], scalar=w2.t[:, 0:1], in1=eg.t[:], op0=ALU.mult, op1=ALU.add),
                   [oh2.r, w2.r, eg.r], [eg.r])
            for g in range(4):
                P.emit("dve", lambda e, g=g, j=j: e.tensor_scalar_mul(out=gate.t[:, j, g * 8:(g + 1) * 8], in0=eg.t[:], scalar1=goh.t[:, g:g + 1]), [eg.r, goh.r], [gate.r])
        for j in range(8 if CSTOP >= 3 else 0):
            P.emit("dve", lambda e, j=j: e.tensor_scalar_mul(out=acc.t[:, j, :], in0=acc.t[:, j, :], scalar1=float(ALPHA)), [accr[j]], [accr[j]])
        if CSTOP >= 3.02:
            P.dma("sp", lng.t[:], io["ln2_g"].partition_broadcast(128), reads=[h1f.r], writes=[lng.r])
            P.dma("sp", lnb.t[:], io["ln2_b"].partition_broadcast(128), writes=[lnb.r])

        for ex in range(NEXP if CSTOP >= 4 else 0):
            wd = wds.next()
            P.dma("pool", wd.t[:], io["w_down"][ex].rearrange("(fc p) d -> p fc d", p=128), writes=[wd.r])
            for fh in range(2):
                wg, wu = wgs.next(), wus.next()
                P.dma("pool", wg.t[:], io["w_gate"][ex][:, fh * 256:(fh + 1) * 256].rearrange("(kc p) f -> p kc f", p=128), writes=[wg.r])
                P.dma("pool", wu.t[:], io["w_up"][ex][:, fh * 256:(fh + 1) * 256].rearrange("(kc p) f -> p kc f", p=128), writes=[wu.r])
                for fc in range(2):
                    for th in range(2):
                        pg, pu = psr.next(), psr.next()
                        for (wt, ps) in ((wg, pg), (wu, pu)):
                            for kc in range(16):
                                P.emit("pe", lambda e, ps=ps, wt=wt, kc=kc, fc=fc, th=th: e.matmul(
                                    ps.t[:], lhsT=wt.t[:, kc, fc * 128:(fc + 1) * 128], rhs=xT.t[:, kc, th * 512:(th + 1) * 512],
                                    start=(kc == 0), stop=(kc == 15)), [wt.r, xT.r], [ps.r])
                        sg = sgs.next()
                        P.emit("act", lambda e, pg=pg, sg=sg: e.activation(out=sg.t[:], in_=pg.t[:], func=AF.Silu), [pg.r], [sg.r])
                        P.emit("dve", lambda e, pu=pu, sg=sg, fh=fh, fc=fc, th=th: e.tensor_tensor(out=hid.t[:, fh * 2 + fc, th * 512:(th + 1) * 512], in0=sg.t[:], in1=pu.t[:], op=ALU.mult),
                               [sg.r, pu.r], [hid.r])
            for j in range(8):
                for n in range(4):
                    ps = psr.next()
                    for fc in range(4):
                        P.emit("pe", lambda e, ps=ps, fc=fc, j=j, n=n, wd=wd: e.matmul(ps.t[:], lhsT=hid.t[:, fc, j * 128:(j + 1) * 128], rhs=wd.t[:, fc, n * 512:(n + 1) * 512],
                                                                                   start=(fc == 0), stop=(fc == 3)), [hid.r, wd.r], [ps.r])
                    a = acc.t[:, j, n * 512:(n + 1) * 512]
                    P.emit("dve", lambda e, a=a, ps=ps, j=j, ex=ex: e.scalar_tensor_tensor(out=a, in0=ps.t[:], scalar=gate.t[:, j, ex:ex + 1], in1=a, op0=ALU.mult, op1=ALU.add),
                           [ps.r, gate.r, accr[j]], [accr[j]])
        if CSTOP >= 5:
            layer_norm_tiles(P, acc, accr, lng, lnb, epst, junk_ap, sm)
        for j in range(8):
            P.dma("sp", io["hout"][j * 128:(j + 1) * 128, :], acc.t[:, j, :], reads=[accr[j]])
        P.finalize()
    nc.all_engine_barrier()


def build_C():
    nc = bass.Bass("TRN2", target_bir_lowering=False)
    io = dict(
        hres=din(nc, "hres", [TL, D]), attn=din(nc, "attn", [TL, 1280]), lruT=din(nc, "lruT", [768, TL]),
        w_o=din(nc, "w_o", [D, D]), ln1_g=din(nc, "ln1_g", [D]), ln1_b=din(nc, "ln1_b", [D]),
        w_gr=din(nc, "w_gr", [D, 36]), b_gr=din(nc, "b_gr", [36]),
        w_gate=din(nc, "w_gate", [NEXP, D, 512]), w_up=din(nc, "w_up", [NEXP, D, 512]), w_down=din(nc, "w_down", [NEXP, 512, D]),
        ln2_g=din(nc, "ln2_g", [D]), ln2_b=din(nc, "ln2_b", [D]), ident=din(nc, "ident", [128, 128]),
        hout=dout(nc, "hout", [TL, D]),
    )
    phase_C(nc, io)
    return nc


def launch_C(h_shards, resB, prm, l):
    nc = build_C()
    lru_all = np.concatenate([resB[n]["lru"] for n in range(6)], axis=0)
    w_gr = np.ascontiguousarray(np.concatenate([prm["w_group"][l], prm["w_router"][l]], axis=1))
    b_gr = np.ascontiguousarray(np.concatenate([prm["b_group"][l], prm["b_router"][l]], axis=0))
    wg = prm["w_gate"][l].reshape(32, D, 512)[:NEXP]
    wu = prm["w_up"][l].reshape(32, D, 512)[:NEXP]
    wdn = prm["w_down"][l].reshape(32, 512, D)[:NEXP]
    in_maps = []
    for c in range(NC):
        in_maps.append(dict(
            hres=h_shards[c], attn=resB[c]["attn"], lruT=np.ascontiguousarray(lru_all[:, own_rows(c)]),
            w_o=prm["w_o"][l], ln1_g=prm["ln1_g"][l], ln1_b=prm["ln1_b"][l], w_gr=w_gr, b_gr=b_gr,
            w_gate=wg, w_up=wu, w_down=wdn, ln2_g=prm["ln2_g"][l], ln2_b=prm["ln2_b"][l], ident=IDENT))
    return run(nc, in_maps)


def kernel(**inputs):
    prm = {k: np.asarray(v) for k, v in inputs.items()}
    x = prm["x"][0]
    pos = prm["positions"][0]
    h_shards = [np.ascontiguousarray(x[own_rows(c)]) for c in range(NC)]
    for l in range(2):
        resA = launch_A(h_shards, prm["w_in"][l], prm["b_f"][l], pos)
        resB = launch_B(resA, prm, l)
        resC = launch_C(h_shards, resB, prm, l)
        h_shards = [np.ascontiguousarray(r["hout"]) for r in resC]
    out = np.empty((1, SEQ, D), np.float32)
    for c in range(NC):
        out[0, own_rows(c)] = h_shards[c]
    return out
```

```python
import math
from contextlib import ExitStack
import numpy as np
import ml_dtypes
import concourse.bass as bass
import concourse.mybir as mybir
from concourse.bass_utils import run_bass_kernel_spmd

F32 = mybir.dt.float32
BF16 = mybir.dt.bfloat16
I32 = mybir.dt.int32
AF = mybir.ActivationFunctionType
ALU = mybir.AluOpType
AX = mybir.AxisListType

NC = 8
SEQ = 8192
D = 2048
TL = 1024
N_IN = 5382
ALPHA = (2.0 * 2) ** 0.25
LN_EPS = 1e-5
PI = math.pi

SAME_ENGINE_SYNC = True
NEXP = 32
CSTOP = 9
LOOK = 3
PVLAG = 2
NDUMMY = 0


class Res:
    __slots__ = ("last_w", "readers")

    def __init__(self):
        self.last_w = None
        self.readers = []


class Tile:
    __slots__ = ("t", "r")

    def __init__(self, t):
        self.t = t
        self.r = Res()


class Op:
    __slots__ = ("eng", "fn", "deps", "sig", "is_dma", "dsem", "dtarget", "has_dep", "waits")

    def __init__(self, eng, fn, is_dma):
        self.eng = eng
        self.fn = fn
        self.deps = []
        self.sig = 0
        self.is_dma = is_dma
        self.dsem = None
        self.dtarget = 0
        self.has_dep = False
        self.waits = []


class Prog:
    ENGS = ("pe", "act", "dve", "pool", "sp")

    def __init__(self, nc, st, n_dma_sems=8):
        self.nc = nc
        self.st = st
        self.ops = []
        self.n_dma_sems = n_dma_sems
        self._n = 0

    _uid = [0]

    def tile(self, shape, dtype, name=None):
        Prog._uid[0] += 1
        return Tile(self.st.enter_context(self.nc.sbuf_tensor("%s_%d" % (name or "t", Prog._uid[0]), shape, dtype)))

    def psum(self, shape, dtype=F32, name=None):
        Prog._uid[0] += 1
        return Tile(self.st.enter_context(self.nc.psum_tensor("%s_%d" % (name or "p", Prog._uid[0]), shape, dtype)))

    def emit(self, eng, fn, reads=(), writes=(), is_dma=False):
        op = Op(eng, fn, is_dma)
        deps = {}
        for r in reads:
            if r.last_w is not None:
                deps[id(r.last_w)] = r.last_w
        for w in writes:
            if w.last_w is not None:
                deps[id(w.last_w)] = w.last_w
            for rd in w.readers:
                deps[id(rd)] = rd
        op.deps = list(deps.values())
        for d in op.deps:
            d.has_dep = True
        for r in reads:
            r.readers.append(op)
        for w in writes:
            w.last_w = op
            w.readers = []
        self.ops.append(op)
        return op

    def dma(self, eng, out, in_, reads=(), writes=()):
        return self.emit(eng, lambda e: e.dma_start(out=out, in_=in_), reads, writes, is_dma=True)

    def finalize(self):
        nc, st = self.nc, self.st
        esem = {e: st.enter_context(nc.semaphore("s_" + e)) for e in self.ENGS}
        dsems = {q: [st.enter_context(nc.semaphore("d_%s%d" % (q, i))) for i in range(self.n_dma_sems)]
                 for q in ("sp", "pool")}
        dcount = {q: [0] * self.n_dma_sems for q in dsems}
        drr = {q: 0 for q in dsems}
        ecount = {e: 0 for e in self.ENGS}
        known = {e: {} for e in self.ENGS}
        per_eng = {e: [] for e in self.ENGS}
        for op in self.ops:
            waits = {}
            k = known[op.eng]

            def need(sem, val):
                key = id(sem)
                if k.get(key, 0) >= val:
                    return
                if key not in waits or waits[key][1] < val:
                    waits[key] = (sem, val)

            for d in op.deps:
                if d.is_dma:
                    need(d.dsem, d.dtarget)
                else:
                    if d.eng == op.eng and (d.eng == "pe" or not SAME_ENGINE_SYNC):
                        continue
                    need(esem[d.eng], d.sig)
            if op.is_dma:
                q = op.eng
                i = drr[q]
                drr[q] = (i + 1) % self.n_dma_sems
                sem = dsems[q][i]
                if dcount[q][i] > 0:
                    need(sem, dcount[q][i] * 16)
                dcount[q][i] += 1
                op.dsem = sem
                op.dtarget = dcount[q][i] * 16
            elif op.has_dep:
                ecount[op.eng] += 1
                op.sig = ecount[op.eng]
            op.waits = list(waits.values())
            for sem, val in op.waits:
                k[id(sem)] = val
            per_eng[op.eng].append(op)
        fin = []
        for q in dsems:
            for i, sem in enumerate(dsems[q]):
                if dcount[q][i] > 0:
                    fin.append((sem, dcount[q][i] * 16))
        for e in self.ENGS:
            if ecount[e] > 0:
                fin.append((esem[e], ecount[e]))
        block = st.enter_context(nc.Block())

        def replay(name, last=False):
            def body(e):
                for op in per_eng[name]:
                    for sem, val in op.waits:
                        e.wait_ge(sem, val)
                    inst = op.fn(e)
                    if op.is_dma:
                        inst.then_inc(op.dsem, 16)
                    elif op.sig:
                        inst.then_inc(esem[name], 1)
                if last:
                    for sem, val in fin:
                        e.wait_ge(sem, val)
            return body

        block.tensor(replay("pe"))
        block.scalar(replay("act"))
        block.vector(replay("dve"))
        block.gpsimd(replay("pool"))
        block.sync(replay("sp", True))


class Ring:
    def __init__(self, tiles):
        self.tiles = tiles
        self.i = 0

    def next(self):
        t = self.tiles[self.i % len(self.tiles)]
        self.i += 1
        return t


def din(nc, name, shape, dt=F32):
    return nc.dram_tensor(name, list(shape), dt, kind="ExternalInput").ap()


def dout(nc, name, shape, dt=F32):
    return nc.dram_tensor(name, list(shape), dt, kind="ExternalOutput").ap()


_ev = [0]


def evac_eng():
    _ev[0] += 1
    return "act" if _ev[0] % 2 else "dve"


def copy_op(P, eng, out, in_, reads, writes, scale=None):
    if eng == "act":
        if scale is None:
            P.emit("act", lambda e: e.activation(out=out, in_=in_, func=AF.Copy), reads, writes)
        else:
            P.emit("act", lambda e: e.activation(out=out, in_=in_, func=AF.Copy, scale=float(scale)), reads, writes)
    else:
        if scale is None:
            P.emit("dve", lambda e: e.tensor_copy(out=out, in_=in_), reads, writes)
        else:
            P.emit("dve", lambda e: e.tensor_scalar_mul(out=out, in0=in_, scalar1=float(scale)), reads, writes)


def transpose_to_xT(P, src_ap_fn, ident, xT, nchunks=16):
    hts = Ring([P.tile([128, nchunks * 128], F32, "ht") for _ in range(2)])
    pst = Ring([P.psum([128, 512], F32, "pst") for _ in range(2)])
    for j in range(8):
        ht = hts.next()
        P.dma("sp", ht.t[:], src_ap_fn(j), writes=[ht.r])
        for g in range(nchunks // 4):
            ps = pst.next()
            for q in range(4):
                kc = g * 4 + q
                P.emit("pe", lambda e, ps=ps, q=q, ht=ht, kc=kc: e.transpose(
                    ps.t[:, q * 128:(q + 1) * 128], ht.t[:, kc * 128:(kc + 1) * 128], ident.t[:]),
                    reads=[ht.r, ident.r], writes=[ps.r])
            copy_op(P, evac_eng(), xT.t[:, g * 4:(g + 1) * 4, j * 128:(j + 1) * 128],
                    ps.t[:].rearrange("p (a b) -> p a b", a=4), [ps.r], [xT.r])


def phase_A(nc, io):
    with ExitStack() as st:
        P = Prog(nc, st)
        ident = P.tile([128, 128], F32, "ident")
        P.dma("sp", ident.t[:], io["ident"], writes=[ident.r])
        xT = P.tile([128, 16, TL], BF16, "xT")
        transpose_to_xT(P, lambda j: io["hin"][j * 128:(j + 1) * 128, :], ident, xT)

        posi = P.tile([128, 8], I32, "posi")
        posf = P.tile([128, 8], F32, "posf")
        invf = P.tile([128, 8], F32, "invf")
        ang = P.tile([128, 8, 8], F32, "ang")
        red = P.tile([128, 8, 8], F32, "red")
        cost = P.tile([128, 8, 8], F32, "cos")
        sint = P.tile([128, 8, 8], F32, "sin")
        bfb = P.tile([128, 6], F32, "bfb")
        P.dma("sp", posi.t[:], io["pos"], writes=[posi.r])
        P.dma("sp", invf.t[:], io["invf"], writes=[invf.r])
        P.dma("sp", bfb.t[:], io["b_f"].partition_broadcast(128), writes=[bfb.r])
        P.emit("dve", lambda e: e.tensor_copy(out=posf.t[:], in_=posi.t[:]), [posi.r], [posf.r])
        P.emit("dve", lambda e: e.tensor_tensor(out=ang.t[:], in0=posf.t[:].unsqueeze(2).to_broadcast([128, 8, 8]),
                                                in1=invf.t[:].unsqueeze(1).to_broadcast([128, 8, 8]), op=ALU.mult),
               [posf.r, invf.r], [ang.r])
        angi = P.tile([128, 8, 8], I32, "angi")
        angf = P.tile([128, 8, 8], F32, "angf")
        ang2 = P.tile([128, 8, 8], F32, "ang2")
        for (shift, dst) in ((0.0, sint), (0.5 * PI, cost)):
            P.emit("dve", lambda e, shift=shift: e.tensor_scalar_add(out=ang2.t[:], in0=ang.t[:], scalar1=float(shift)), [ang.r], [ang2.r])
            P.emit("dve", lambda e: e.tensor_scalar_mul(out=red.t[:], in0=ang2.t[:], scalar1=float(1.0 / (2 * PI))), [ang2.r], [red.r])
            P.emit("dve", lambda e: e.tensor_copy(out=angi.t[:], in_=red.t[:]), [red.r], [angi.r])
            P.emit("dve", lambda e: e.tensor_copy(out=angf.t[:], in_=angi.t[:]), [angi.r], [angf.r])
            P.emit("dve", lambda e: e.scalar_tensor_tensor(out=red.t[:], in0=angf.t[:], scalar=float(-2 * PI), in1=ang2.t[:], op0=ALU.mult, op1=ALU.add),
                   [angf.r, ang2.r], [red.r])
            P.emit("dve", lambda e: e.tensor_single_scalar(out=angf.t[:], in_=red.t[:], scalar=float(PI), op=ALU.is_gt), [red.r], [angf.r])
            P.emit("dve", lambda e: e.scalar_tensor_tensor(out=red.t[:], in0=angf.t[:], scalar=float(-2 * PI), in1=red.t[:], op0=ALU.mult, op1=ALU.add),
                   [angf.r, red.r], [red.r])
            P.emit("dve", lambda e: e.tensor_single_scalar(out=angf.t[:], in_=red.t[:], scalar=float(-PI), op=ALU.is_lt), [red.r], [angf.r])
            P.emit("dve", lambda e: e.scalar_tensor_tensor(out=red.t[:], in0=angf.t[:], scalar=float(2 * PI), in1=red.t[:], op0=ALU.mult, op1=ALU.add),
                   [angf.r, red.r], [red.r])
            P.emit("act", lambda e, dst=dst: e.activation(out=dst.t[:], in_=red.t[:], func=AF.Sin), [red.r], [dst.r])

        wbufs = Ring([P.tile([128, 16, 512], BF16, "w") for _ in range(3)])
        psA = Ring([P.psum([128, 512], F32, "psA") for _ in range(3)])

        def load_w(c0, ncols):
            wb = wbufs.next()
            P.dma("pool", wb.t[:, :, 0:ncols], io["w_in"][:, c0:c0 + ncols].rearrange("(kc p) n -> p kc n", p=128),
                  writes=[wb.r])
            return wb

        otf = Ring([P.tile([128, TL], F32, "otf") for _ in range(2)])
        otb = Ring([P.tile([128, TL], BF16, "otb") for _ in range(2)])

        def feat_chunk(c0, nblk, out_ap, scale, is_bf):
            wb = load_w(c0, nblk * 128)
            for b in range(nblk):
                ot = (otb if is_bf else otf).next()
                for th in range(2):
                    ps = psA.next()
                    for kc in range(16):
                        P.emit("pe", lambda e, ps=ps, wb=wb, kc=kc, b=b, th=th: e.matmul(
                            ps.t[:], lhsT=wb.t[:, kc, b * 128:(b + 1) * 128], rhs=xT.t[:, kc, th * 512:(th + 1) * 512],
                            start=(kc == 0), stop=(kc == 15)), reads=[wb.r, xT.r], writes=[ps.r])
                    copy_op(P, evac_eng(), ot.t[:, th * 512:(th + 1) * 512], ps.t[:], [ps.r], [ot.r], scale)
                P.dma("sp", out_ap(b), ot.t[:], reads=[ot.r])

        def tok_chunk(c0, ncols, handler):
            wb = load_w(c0, ncols)
            for j in range(8):
                ps = psA.next()
                for kc in range(16):
                    P.emit("pe", lambda e, ps=ps, wb=wb, kc=kc, j=j: e.matmul(
                        ps.t[:, 0:ncols], lhsT=xT.t[:, kc, j * 128:(j + 1) * 128], rhs=wb.t[:, kc, 0:ncols],
                        start=(kc == 0), stop=(kc == 15)), reads=[wb.r, xT.r], writes=[ps.r])
                handler(j, ps)

        feat_chunk(0, 4, lambda b: io["qTf"][b * 128:(b + 1) * 128, :], 128 ** -0.5, True)
        feat_chunk(512, 2, lambda b: io["qTf"][(4 + b) * 128:(5 + b) * 128, :], 128 ** -0.5, True)
        feat_chunk(768, 4, lambda b: io["kTf"][b * 128:(b + 1) * 128, :], None, True)
        feat_chunk(1280, 2, lambda b: io["kTf"][(4 + b) * 128:(5 + b) * 128, :], None, True)
        feat_chunk(2310, 4, lambda b: io["xrT"][b * 128:(b + 1) * 128, :], None, False)
        feat_chunk(2310 + 512, 2, lambda b: io["xrT"][(4 + b) * 128:(5 + b) * 128, :], None, False)
        feat_chunk(3078, 4, lambda b: io["grT"][b * 128:(b + 1) * 128, :], None, False)
        feat_chunk(3078 + 512, 2, lambda b: io["grT"][(4 + b) * 128:(5 + b) * 128, :], None, False)

        vts = Ring([P.tile([128, 512], BF16, "vt") for _ in range(2)])

        def h_v(key, col0, ncols):
            def h(j, ps):
                vt = vts.next()
                copy_op(P, evac_eng(), vt.t[:, 0:ncols], ps.t[:, 0:ncols], [ps.r], [vt.r])
                P.dma("sp", io[key][j * 128:(j + 1) * 128, col0:col0 + ncols], vt.t[:, 0:ncols], reads=[vt.r])
            return h

        tok_chunk(1536, 512, h_v("vf", 0, 512))
        logf = P.tile([128, 8, 6], F32, "logf")
        zt = P.tile([128, 6], F32, "zt")
        h_v2 = h_v("vf", 512, 256)

        def h_vfa(j, ps):
            P.emit("dve", lambda e: e.tensor_tensor(out=zt.t[:], in0=ps.t[:, 256:262], in1=bfb.t[:], op=ALU.add),
                   [ps.r, bfb.r], [zt.r])
            P.emit("act", lambda e: e.activation(out=zt.t[:], in_=zt.t[:], func=AF.Exp, scale=-1.0), [zt.r], [zt.r])
            P.emit("act", lambda e: e.activation(out=zt.t[:], in_=zt.t[:], func=AF.Ln, bias=1.0), [zt.r], [zt.r])
            P.emit("dve", lambda e: e.tensor_scalar_mul(out=logf.t[:, j, :], in0=zt.t[:], scalar1=-1.0), [zt.r], [logf.r])
            h_v2(j, ps)

        tok_chunk(2048, 262, h_vfa)
        P.dma("sp", io["logf"].rearrange("(j p) h -> p j h", p=128), logf.t[:], reads=[logf.r])

        qk = P.tile([128, 512], F32, "qk")
        ta, tb, tc_, td = [P.tile([128, 8, 8], F32, "rt") for _ in range(4)]
        pstr = Ring([P.psum([128, 512], F32, "pstr") for _ in range(2)])

        def h_qk(key, scale):
            qTd = P.tile([128, 4, TL], BF16, "qTd")

            def h(j, ps):
                copy_op(P, "act", qk.t[:], ps.t[:], [ps.r], [qk.r], scale)
                v3 = qk.t[:].rearrange("p (s d) -> p s d", s=8)
                t1, t2 = v3[:, :, 0:8], v3[:, :, 8:16]
                cb = cost.t[:, j, :].unsqueeze(1).to_broadcast([128, 8, 8])
                sb = sint.t[:, j, :].unsqueeze(1).to_broadcast([128, 8, 8])
                rd = [qk.r, cost.r, sint.r]
                P.emit("dve", lambda e: e.tensor_tensor(out=ta.t[:], in0=t1, in1=cb, op=ALU.mult), rd, [ta.r])
                P.emit("dve", lambda e: e.tensor_tensor(out=tb.t[:], in0=t2, in1=sb, op=ALU.mult), rd, [tb.r])
                P.emit("dve", lambda e: e.tensor_tensor(out=tc_.t[:], in0=t2, in1=cb, op=ALU.mult), rd, [tc_.r])
                P.emit("dve", lambda e: e.tensor_tensor(out=td.t[:], in0=t1, in1=sb, op=ALU.mult), rd, [td.r])
                P.emit("dve", lambda e: e.tensor_tensor(out=t1, in0=ta.t[:], in1=tb.t[:], op=ALU.subtract), [ta.r, tb.r], [qk.r])
                P.emit("dve", lambda e: e.tensor_tensor(out=t2, in0=tc_.t[:], in1=td.t[:], op=ALU.add), [tc_.r, td.r], [qk.r])
                pt = pstr.next()
                for b in range(4):
                    P.emit("pe", lambda e, b=b, pt=pt: e.transpose(pt.t[:, b * 128:(b + 1) * 128], qk.t[:, b * 128:(b + 1) * 128], ident.t[:]),
                           [qk.r, ident.r], [pt.r])
                copy_op(P, evac_eng(), qTd.t[:, :, j * 128:(j + 1) * 128], pt.t[:].rearrange("p (a b) -> p a b", a=4), [pt.r], [qTd.r])
                if j == 7:
                    P.dma("sp", io[key].rearrange("(b p) t -> p b t", p=128), qTd.t[:], reads=[qTd.r])
            return h

        tok_chunk(3846, 512, h_qk("qTd", 64 ** -0.5))
        tok_chunk(4358, 512, h_qk("kTd", None))
        tok_chunk(4870, 512, h_v("vd", 0, 512))
        P.finalize()
    nc.all_engine_barrier()


def build_A():
    nc = bass.Bass("TRN2", target_bir_lowering=False)
    io = dict(
        hin=din(nc, "hin", [TL, D]), w_in=din(nc, "w_in", [D, N_IN]), b_f=din(nc, "b_f", [6]),
        pos=din(nc, "pos", [128, 8], I32), invf=din(nc, "invf", [128, 8]), ident=din(nc, "ident", [128, 128]),
        qTf=dout(nc, "qTf", [768, TL], BF16), kTf=dout(nc, "kTf", [768, TL], BF16), vf=dout(nc, "vf", [TL, 768], BF16),
        logf=dout(nc, "logf", [TL, 6]), xrT=dout(nc, "xrT", [768, TL]), grT=dout(nc, "grT", [768, TL]),
        qTd=dout(nc, "qTd", [512, TL], BF16), kTd=dout(nc, "kTd", [512, TL], BF16), vd=dout(nc, "vd", [TL, 512], BF16),
    )
    phase_A(nc, io)
    return nc


def own_rows(c):
    return np.concatenate([np.arange((8 * j + c) * 128, (8 * j + c + 1) * 128) for j in range(8)])


IDENT = np.eye(128, dtype=np.float32)
INVF = np.broadcast_to((np.float32(500000.0) ** (-np.arange(8, dtype=np.float32) * np.float32(2.0) / np.float32(16))).astype(np.float32), (128, 8)).copy()


def run(nc, in_maps):
    res = run_bass_kernel_spmd(nc, in_maps, core_ids=list(range(NC)))
    return res.results


def launch_A(h_shards, w_in_l, b_f_l, positions):
    nc = build_A()
    in_maps = []
    for c in range(NC):
        in_maps.append(dict(hin=h_shards[c], w_in=w_in_l, b_f=b_f_l,
                            pos=np.ascontiguousarray(positions[own_rows(c)].reshape(8, 128).T), invf=INVF, ident=IDENT))
    return run(nc, in_maps)


def split3(P, src, cur, back, n, npart, sign):
    outs = []
    P.emit("dve", lambda e: e.tensor_scalar_mul(out=cur.t[:], in0=src.t[:], scalar1=float(sign)), [src.r], [cur.r])
    for i in range(3):
        o = P.tile([npart, n], BF16, "s3o")
        P.emit("dve", lambda e, o=o: e.tensor_copy(out=o.t[:], in_=cur.t[:]), [cur.r], [o.r])
        if i < 2:
            P.emit("dve", lambda e, o=o: e.tensor_copy(out=back.t[:], in_=o.t[:]), [o.r], [back.r])
            P.emit("dve", lambda e: e.tensor_tensor(out=cur.t[:], in0=cur.t[:], in1=back.t[:], op=ALU.subtract), [cur.r, back.r], [cur.r])
        outs.append(o)
    return outs


def phase_B(nc, io, lam_init):
    with ExitStack() as st:
        P = Prog(nc, st)
        CH = 2048
        cw = P.tile([128, 4], F32, "cw"); cb = P.tile([128, 1], F32, "cb")
        wa32 = P.tile([128, 128], F32, "wa32"); wi32 = P.tile([128, 128], F32, "wi32")
        wa = P.tile([128, 128], BF16, "wa"); wi = P.tile([128, 128], BF16, "wi")
        ba = P.tile([128, 1], F32, "ba"); bi = P.tile([128, 1], F32, "bi"); lm = P.tile([128, 1], F32, "lm")
        one = P.tile([128, 1], F32, "one"); c8 = P.tile([128, 1], F32, "c8")
        for t_, k_ in ((cw, "conv_w"), (cb, "conv_b"), (wa32, "w_a"), (wi32, "w_i"), (ba, "b_a"), (bi, "b_i"), (lm, "lru_lam")):
            P.dma("sp", t_.t[:], io[k_], writes=[t_.r])
        P.emit("dve", lambda e: e.tensor_copy(out=wa.t[:], in_=wa32.t[:]), [wa32.r], [wa.r])
        P.emit("dve", lambda e: e.tensor_copy(out=wi.t[:], in_=wi32.t[:]), [wi32.r], [wi.r])
        P.emit("dve", lambda e: e.memset(one.t[:], 1.0), [], [one.r])
        P.emit("act", lambda e: e.activation(out=c8.t[:], in_=lm.t[:], func=AF.Exp, scale=-1.0), [lm.r], [c8.r])
        P.emit("act", lambda e: e.activation(out=c8.t[:], in_=c8.t[:], func=AF.Ln, bias=one.t[:, 0:1]), [c8.r, one.r], [c8.r])
        P.emit("dve", lambda e: e.tensor_scalar_mul(out=c8.t[:], in0=c8.t[:], scalar1=-8.0), [c8.r], [c8.r])
        xpad = P.tile([128, CH + 3], F32, "xpad"); xc = P.tile([128, CH], F32, "xc"); xcb = P.tile([128, CH], BF16, "xcb")
        rr = P.tile([128, CH], F32, "rr"); ii = P.tile([128, CH], F32, "ii"); tmp = P.tile([128, CH], F32, "tmp")
        hh = Ring([P.tile([128, CH], F32, "hh") for _ in range(2)]); gg = P.tile([128, CH], F32, "gg"); gt2 = P.tile([128, CH], F32, "gt2")
        psl = Ring([P.psum([128, 512], F32, "psl") for _ in range(2)])
        hprev = None
        for ck in range(SEQ // CH):
            t0 = ck * CH
            P.dma("sp", xpad.t[:], io["xr"][:, t0:t0 + CH + 3], writes=[xpad.r])
            P.dma("sp", gg.t[:], io["gr"][:, t0:t0 + CH], writes=[gg.r])
            P.emit("dve", lambda e: e.tensor_scalar(out=xc.t[:], in0=xpad.t[:, 0:CH], scalar1=cw.t[:, 0:1], scalar2=cb.t[:, 0:1],
                                                    op0=ALU.mult, op1=ALU.add), [xpad.r, cw.r, cb.r], [xc.r])
            for j in range(1, 4):
                P.emit("dve", lambda e, j=j: e.scalar_tensor_tensor(out=xc.t[:], in0=xpad.t[:, j:j + CH], scalar=cw.t[:, j:j + 1], in1=xc.t[:],
                                                                    op0=ALU.mult, op1=ALU.add), [xpad.r, cw.r, xc.r], [xc.r])
            P.emit("dve", lambda e: e.tensor_copy(out=xcb.t[:], in_=xc.t[:]), [xc.r], [xcb.r])
            for s in range(CH // 512):
                sl = slice(s * 512, (s + 1) * 512)
                for (wt, bt, dst) in ((wa, ba, rr), (wi, bi, ii)):
                    ps = psl.next()
                    P.emit("pe", lambda e, ps=ps, wt=wt, sl=sl: e.matmul(ps.t[:], lhsT=wt.t[:], rhs=xcb.t[:, sl], start=True, stop=True),
                           [wt.r, xcb.r], [ps.r])
                    P.emit("act", lambda e, ps=ps, bt=bt, dst=dst, sl=sl: e.activation(out=dst.t[:, sl], in_=ps.t[:], func=AF.Sigmoid, bias=bt.t[:, 0:1]),
                           [ps.r, bt.r], [dst.r])
            P.emit("act", lambda e: e.activation(out=rr.t[:], in_=rr.t[:], func=AF.Exp, scale=c8.t[:, 0:1]), [rr.r, c8.r], [rr.r])
            P.emit("dve", lambda e: e.tensor_tensor(out=tmp.t[:], in0=rr.t[:], in1=rr.t[:], op=ALU.mult), [rr.r], [tmp.r])
            P.emit("act", lambda e: e.activation(out=tmp.t[:], in_=tmp.t[:], func=AF.Sqrt, scale=-1.0, bias=one.t[:, 0:1]), [tmp.r, one.r], [tmp.r])
            P.emit("dve", lambda e: e.tensor_tensor(out=ii.t[:], in0=ii.t[:], in1=xc.t[:], op=ALU.mult), [ii.r, xc.r], [ii.r])
            P.emit("dve", lambda e: e.tensor_tensor(out=ii.t[:], in0=ii.t[:], in1=tmp.t[:], op=ALU.mult), [ii.r, tmp.r], [ii.r])
            hcur = hh.next()
            if hprev is None:
                P.emit("dve", lambda e, hcur=hcur: e.tensor_tensor_scan(out=hcur.t[:], data0=rr.t[:], data1=ii.t[:], initial=0.0,
                                                                        op0=ALU.mult, op1=ALU.add), [rr.r, ii.r], [hcur.r])
            else:
                P.emit("dve", lambda e, hcur=hcur, hp=hprev: e.tensor_tensor_scan(out=hcur.t[:], data0=rr.t[:], data1=ii.t[:],
                                                                                  initial=hp.t[:, CH - 1:CH], op0=ALU.mult, op1=ALU.add),
                       [rr.r, ii.r, hprev.r], [hcur.r])
            hprev = hcur
            P.emit("pool", lambda e: e.tensor_tensor(out=gt2.t[:], in0=gg.t[:], in1=gg.t[:], op=ALU.mult), [gg.r], [gt2.r])
            P.emit("pool", lambda e: e.tensor_scalar(out=gt2.t[:], in0=gt2.t[:], scalar1=0.044715, scalar2=1.0, op0=ALU.mult, op1=ALU.add), [gt2.r], [gt2.r])
            P.emit("pool", lambda e: e.tensor_tensor(out=gt2.t[:], in0=gt2.t[:], in1=gg.t[:], op=ALU.mult), [gt2.r, gg.r], [gt2.r])
            P.emit("act", lambda e: e.activation(out=gt2.t[:], in_=gt2.t[:], func=AF.Sigmoid, scale=2.0 * math.sqrt(2.0 / math.pi)), [gt2.r], [gt2.r])
            P.emit("pool", lambda e: e.tensor_tensor(out=gt2.t[:], in0=gt2.t[:], in1=gg.t[:], op=ALU.mult), [gt2.r, gg.r], [gt2.r])
            P.emit("dve", lambda e, hcur=hcur: e.tensor_tensor(out=tmp.t[:], in0=gt2.t[:], in1=hcur.t[:], op=ALU.mult), [gt2.r, hcur.r], [tmp.r])
            P.dma("sp", io["lru"][:, t0:t0 + CH], tmp.t[:], reads=[tmp.r])
        P.finalize()
    nc.all_engine_barrier()

    with ExitStack() as st:
        P = Prog(nc, st)
        kaug_d = nc.dram_tensor("kaug_d", [6, 6, SEQ], BF16).ap()
        qaug_d = nc.dram_tensor("qaug_d", [6, 6, TL], BF16).ap()
        lf = P.tile([6, SEQ], F32, "lf"); ones6 = P.tile([6, SEQ], BF16, "ones6"); cum = P.tile([6, SEQ], F32, "cum")
        om = P.tile([6, SEQ], F32, "om"); cown = P.tile([6, TL], F32, "cown")
        qcur = P.tile([6, TL], F32, "qcur"); qback = P.tile([6, TL], F32, "qback")
        P.dma("sp", lf.t[:], io["logfT"], writes=[lf.r])
        P.dma("sp", om.t[:], io["ownmask"], writes=[om.r])
        P.emit("dve", lambda e: e.memset(ones6.t[:], 1.0), [], [ones6.r])
        P.emit("dve", lambda e: e.tensor_tensor_scan(out=cum.t[:], data0=ones6.t[:], data1=lf.t[:], initial=0.0, op0=ALU.mult, op1=ALU.add),
               [ones6.r, lf.r], [cum.r])
        P.emit("dve", lambda e: e.tensor_tensor(out=om.t[:], in0=om.t[:], in1=cum.t[:], op=ALU.mult), [om.r, cum.r], [om.r])
        P.emit("dve", lambda e: e.tensor_reduce(out=cown.t[:].rearrange("p (j i) -> p j i", j=8),
                                                in_=om.t[:].rearrange("p (j r i) -> p j i r", j=8, r=8, i=128), axis=AX.X, op=ALU.add),
               [om.r], [cown.r])
        q3 = split3(P, cown, qcur, qback, TL, 6, 1.0)
        k3 = split3(P, cum, lf, om, SEQ, 6, -1.0)
        for i in range(3):
            P.dma("sp", kaug_d[:, i, :], ones6.t[:], reads=[ones6.r])
            P.dma("sp", kaug_d[:, 3 + i, :], k3[i].t[:], reads=[k3[i].r])
            P.dma("sp", qaug_d[:, i, :], q3[i].t[:], reads=[q3[i].r])
            P.dma("sp", qaug_d[:, 3 + i, :], ones6.t[:, 0:TL], reads=[ones6.r])
        P.finalize()
    nc.all_engine_barrier()

    with ExitStack() as st:
        P = Prog(nc, st)
        mF = P.tile([128, 8, 128], BF16, "mF"); mD = P.tile([128, 8, 128], BF16, "mD")
        P.dma("sp", mF.t[:], io["maskF"], writes=[mF.r])
        P.dma("sp", mD.t[:], io["maskD"], writes=[mD.r])
        lq = [P.tile([128, 64], F32, "lq") for _ in range(4)]
        for t_, k_ in zip(lq, ("lam_q1", "lam_k1", "lam_q2", "lam_k2")):
            P.dma("sp", t_.t[:], io[k_].partition_broadcast(128), writes=[t_.r])
        s1 = P.tile([128, 1], F32, "s1"); s2 = P.tile([128, 1], F32, "s2"); nlam = P.tile([128, 1], F32, "nlam")
        epst = P.tile([128, 1], F32, "eps")
        P.emit("dve", lambda e: e.memset(epst.t[:], LN_EPS), [], [epst.r])
        for (a_, b_, s_) in ((lq[0], lq[1], s1), (lq[2], lq[3], s2)):
            P.emit("dve", lambda e, a_=a_, b_=b_: e.tensor_tensor(out=a_.t[:], in0=a_.t[:], in1=b_.t[:], op=ALU.mult), [a_.r, b_.r], [a_.r])
            P.emit("dve", lambda e, a_=a_, s_=s_: e.reduce_sum(out=s_.t[:], in_=a_.t[:], axis=AX.X), [a_.r], [s_.r])
            P.emit("act", lambda e, s_=s_: e.activation(out=s_.t[:], in_=s_.t[:], func=AF.Exp), [s_.r], [s_.r])
        P.emit("dve", lambda e: e.tensor_tensor(out=nlam.t[:], in0=s2.t[:], in1=s1.t[:], op=ALU.subtract), [s1.r, s2.r], [nlam.r])
        P.emit("dve", lambda e: e.tensor_scalar_add(out=nlam.t[:], in0=nlam.t[:], scalar1=-float(lam_init)), [nlam.r], [nlam.r])
        gsb = P.tile([128, 128], F32, "gsb")
        P.dma("sp", gsb.t[:], io["subln_g"].partition_broadcast(128), writes=[gsb.r])
        P.emit("dve", lambda e: e.tensor_scalar_mul(out=gsb.t[:], in0=gsb.t[:], scalar1=float(1.0 - lam_init)), [gsb.r], [gsb.r])

        kTs = Ring([P.tile([128, SEQ], BF16, "kT") for _ in range(2)])
        kaugs = Ring([P.tile([128, SEQ], BF16, "kaug") for _ in range(2)])
        qaugs = Ring([P.tile([128, TL], BF16, "qaug") for _ in range(2)])
        for t_ in kaugs.tiles + qaugs.tiles:
            P.emit("pool", lambda e, t_=t_: e.memset(t_.t[:], 0.0), [], [t_.r])
        vs = Ring([P.tile([128, 64, 130], BF16, "v") for _ in range(2)])
        qTs = Ring([P.tile([128, TL], BF16, "qT") for _ in range(2)])
        for _ in range(2):
            v_ = vs.next()
            P.emit("dve", lambda e, v_=v_: e.memset(v_.t[:, :, 128:130], 1.0), [], [v_.r])
        pss = Ring([P.psum([128, 512], F32, "pss") for _ in range(LOOK + 1)])
        pTs = Ring([P.tile([128, 512], BF16, "pT") for _ in range(PVLAG + 3)])
        o1s = Ring([P.tile([128, 128], F32, "o1") for _ in range(3)])
        rcs = Ring([P.tile([128, 1], F32, "rc") for _ in range(6)])
        junk = P.tile([128, 128], F32, "junk")

        class Acc:
            pass
        accbanks = [P.psum([128, 512], F32, "accb") for _ in range(4)]
        accs = []
        for j in range(8):
            a_ = Acc()
            a_.ap = accbanks[j % 4].t[:, 0:130]
            a_.r = accbanks[j % 4].r
            accs.append(a_)
        o1all = P.tile([128, 8, 128], F32, "o1all")
        o1res = [Res() for _ in range(8)]

        tasks = []
        head_loads = []

        def sel_head(kall, vall, qown, hidx, aug=None):
            kT, v_, qT = kTs.next(), vs.next(), qTs.next()
            ka = qa = None
            if aug is not None:
                ka, qa = kaugs.next(), qaugs.next()

            def load():
                P.dma("sp", kT.t[:], kall[hidx * 128:(hidx + 1) * 128, :], writes=[kT.r])
                P.dma("sp", v_.t[:, :, 0:128], vall.rearrange("(t p) d -> p t d", p=128)[:, :, hidx * 128:(hidx + 1) * 128], writes=[v_.r])
                P.dma("sp", qT.t[:], qown[hidx * 128:(hidx + 1) * 128, :], writes=[qT.r])
                if aug is not None:
                    P.dma("sp", ka.t[0:6, :], kaug_d[aug], writes=[ka.r])
                    P.dma("sp", qa.t[0:6, :], qaug_d[aug], writes=[qa.r])
            head_loads.append(load)
            return kT, v_, qT, ka, qa

        def add_map(qk_mm, v_, mask, clamp, fin_fn, head_first):
            first = True
            for ps_ in range(2):
                for kt in range(32 * (ps_ + 1)):
                    G = kt // 8
                    jlo, jhi = max(G, 4 * ps_), 4 * (ps_ + 1)
                    if jlo >= jhi:
                        continue
                    tasks.append(dict(kt=kt, G=G, jlo=jlo, jhi=jhi, qk=qk_mm, v=v_, mask=mask, clamp=clamp, fin=fin_fn,
                                      head_first=head_first if first else None))
                    first = False

        def fox_fin(h):
            def fin(j):
                acc = accs[j]
                rc = rcs.next(); o1 = o1s.next()
                P.emit("dve", lambda e: e.reciprocal(out=rc.t[:], in_=acc.ap[:, 128:129]), [acc.r], [rc.r])
                P.emit("dve", lambda e: e.tensor_scalar_mul(out=o1.t[:], in0=acc.ap[:, 0:128], scalar1=rc.t[:, 0:1]), [acc.r, rc.r], [o1.r])
                P.dma("sp", io["attn"][j * 128:(j + 1) * 128, h * 128:(h + 1) * 128], o1.t[:], reads=[o1.r])
            return fin

        def diff_fin0():
            def fin(j):
                acc = accs[j]
                rc1 = rcs.next()
                P.emit("dve", lambda e: e.reciprocal(out=rc1.t[:], in_=acc.ap[:, 128:129]), [acc.r], [rc1.r])
                P.emit("dve", lambda e: e.tensor_scalar_mul(out=o1all.t[:, j, :], in0=acc.ap[:, 0:128], scalar1=rc1.t[:, 0:1]), [acc.r, rc1.r], [o1res[j]])
            return fin

        def diff_fin1(hd):
            def fin(j):
                acc = accs[j]
                rc2, ss = rcs.next(), rcs.next()
                oc = o1s.next()
                P.emit("dve", lambda e: e.reciprocal(out=rc2.t[:], in_=acc.ap[:, 128:129]), [acc.r], [rc2.r])
                P.emit("dve", lambda e: e.tensor_tensor(out=rc2.t[:], in0=rc2.t[:], in1=nlam.t[:], op=ALU.mult), [rc2.r, nlam.r], [rc2.r])
                P.emit("dve", lambda e: e.scalar_tensor_tensor(out=oc.t[:], in0=acc.ap[:, 0:128], scalar=rc2.t[:, 0:1], in1=o1all.t[:, j, :],
                                                               op0=ALU.mult, op1=ALU.add), [acc.r, rc2.r, o1res[j]], [oc.r])
                P.emit("act", lambda e: e.activation(out=junk.t[:], in_=oc.t[:], func=AF.Square, accum_out=ss.t[:, 0:1]), [oc.r], [junk.r, ss.r])
                P.emit("act", lambda e: e.activation(out=ss.t[:], in_=ss.t[:], func=AF.Sqrt, scale=1.0 / 128.0, bias=epst.t[:, 0:1]), [ss.r, epst.r], [ss.r])
                P.emit("dve", lambda e: e.reciprocal(out=ss.t[:], in_=ss.t[:]), [ss.r], [ss.r])
                P.emit("dve", lambda e: e.scalar_tensor_tensor(out=oc.t[:], in0=oc.t[:], scalar=ss.t[:, 0:1], in1=gsb.t[:], op0=ALU.mult, op1=ALU.mult),
                       [oc.r, ss.r, gsb.r], [oc.r])
                P.dma("sp", io["attn"][j * 128:(j + 1) * 128, 768 + hd * 128:768 + (hd + 1) * 128], oc.t[:], reads=[oc.r])
            return fin

        hcount = 0
        for h in range(6):
            kT, v_, qT, ka, qa = sel_head(io["kTf_all"], io["vf_all"], io["qTf"], h, aug=h)

            def qk_mm(ps, kt, jlo, jhi, kT=kT, qT=qT, ka=ka, qa=qa):
                n = (jhi - jlo) * 128
                P.emit("pe", lambda e: e.matmul(ps.t[:, 0:n], lhsT=kT.t[:, kt * 128:(kt + 1) * 128], rhs=qT.t[:, jlo * 128:jhi * 128],
                                                start=True, stop=False), [kT.r, qT.r], [ps.r])
                P.emit("pe", lambda e: e.matmul(ps.t[:, 0:n], lhsT=ka.t[:, kt * 128:(kt + 1) * 128], rhs=qa.t[:, jlo * 128:jhi * 128],
                                                start=False, stop=True), [ka.r, qa.r], [ps.r])
            add_map(qk_mm, v_, mF, True, fox_fin(h), hcount)
            hcount += 1
        for hd in range(4):
            kT, v_, qT, _, _ = sel_head(io["kTd_all"], io["vd_all"], io["qTd"], hd)
            for m in range(2):
                def qk_mm(ps, kt, jlo, jhi, kT=kT, qT=qT, m=m):
                    n = (jhi - jlo) * 128
                    P.emit("pe", lambda e: e.matmul(ps.t[:, 0:n], lhsT=kT.t[m * 64:(m + 1) * 64, kt * 128:(kt + 1) * 128],
                                                    rhs=qT.t[m * 64:(m + 1) * 64, jlo * 128:jhi * 128], start=True, stop=True), [kT.r, qT.r], [ps.r])
                add_map(qk_mm, v_, mD, False, diff_fin0() if m == 0 else diff_fin1(hd), hcount if m == 0 else None)
            hcount += 1

        def emit_qk(t):
            ps = pss.next()
            t["ps"] = ps
            t["qk"](ps, t["kt"], t["jlo"], t["jhi"])

        def emit_clamp(t):
            ps = t["ps"]
            if t["clamp"] and t["jlo"] == t["G"]:
                P.emit("dve", lambda e: e.tensor_scalar_min(out=ps.t[:, 0:128], in0=ps.t[:, 0:128], scalar1=80.0), [ps.r], [ps.r])

        def emit_exp(t):
            ps, kt, G, jlo, jhi, mask = t["ps"], t["kt"], t["G"], t["jlo"], t["jhi"], t["mask"]
            n = (jhi - jlo) * 128
            pT = pTs.next()
            t["pT"] = pT
            diag = (jlo == G)
            P.emit("act", lambda e: e.activation(out=pT.t[:, 0:n], in_=ps.t[:, 0:n], func=AF.Exp), [ps.r], [pT.r])
            if diag:
                r = kt - 8 * G
                P.emit("dve", lambda e: e.tensor_tensor(out=pT.t[:, 0:128], in0=pT.t[:, 0:128], in1=mask.t[:, r, :], op=ALU.mult), [pT.r, mask.r], [pT.r])

        def emit_pv(t):
            pT, kt, G, jlo, jhi, v_ = t["pT"], t["kt"], t["G"], t["jlo"], t["jhi"], t["v"]
            diag = (jlo == G)
            hf = t["head_first"]
            if hf is not None and hf + 1 < len(head_loads):
                head_loads[hf + 1]()
            while pending:
                pending.pop(0)()
            for jj in range(jlo, jhi):
                acc = accs[jj]
                c0 = (jj - jlo) * 128
                P.emit("pe", lambda e, acc=acc, c0=c0, jj=jj: e.matmul(acc.ap, lhsT=pT.t[:, c0:c0 + 128], rhs=v_.t[:, kt, :],
                                                                   start=(kt == 0), stop=(kt == 8 * jj + 7)), [pT.r, v_.r], [acc.r])
            if kt % 8 == 7 and diag:
                pending.append(lambda f=t["fin"], G=G: f(G))
            for _ in range(NDUMMY):
                P.emit("pe", lambda e: e.matmul(dummy.t[:], lhsT=warm_l.t[:, 0:128], rhs=warm_r.t[:, 0:512], start=True, stop=True),
                       [warm_l.r, warm_r.r], [dummy.r])

        if NDUMMY:
            dummy = P.psum([128, 512], F32, "dummy")
            warm_l = P.tile([128, 128], BF16, "warm_l")
            warm_r = P.tile([128, 512], BF16, "warm_r")
            P.emit("pool", lambda e: e.memset(warm_l.t[:], 0.0), [], [warm_l.r])
            P.emit("pool", lambda e: e.memset(warm_r.t[:], 0.0), [], [warm_r.r])
        pending = []
        pending_prev = []
        head_loads[0]()
        for i in range(min(LOOK, len(tasks))):
            emit_qk(tasks[i])
        emit_clamp(tasks[0])
        for i in range(len(tasks) + PVLAG):
            if i + LOOK < len(tasks):
                emit_qk(tasks[i + LOOK])
            if i + 1 < len(tasks):
                emit_clamp(tasks[i + 1])
            if i < len(tasks):
                emit_exp(tasks[i])
            if i - PVLAG >= 0:
                emit_pv(tasks[i - PVLAG])
        for f_ in pending_prev + pending:
            f_()
        P.finalize()
    nc.all_engine_barrier()


def build_B(lam_init):
    nc = bass.Bass("TRN2", target_bir_lowering=False)
    io = dict(
        qTf=din(nc, "qTf", [768, TL], BF16), kTf_all=din(nc, "kTf_all", [768, SEQ], BF16), vf_all=din(nc, "vf_all", [SEQ, 768], BF16),
        logfT=din(nc, "logfT", [6, SEQ]), ownmask=din(nc, "ownmask", [6, SEQ]),
        maskF=din(nc, "maskF", [128, 8, 128], BF16), maskD=din(nc, "maskD", [128, 8, 128], BF16),
        qTd=din(nc, "qTd", [512, TL], BF16), kTd_all=din(nc, "kTd_all", [512, SEQ], BF16), vd_all=din(nc, "vd_all", [SEQ, 512], BF16),
        lam_q1=din(nc, "lam_q1", [64]), lam_k1=din(nc, "lam_k1", [64]), lam_q2=din(nc, "lam_q2", [64]), lam_k2=din(nc, "lam_k2", [64]),
        subln_g=din(nc, "subln_g", [128]),
        xr=din(nc, "xr", [128, SEQ + 3]), gr=din(nc, "gr", [128, SEQ]), conv_w=din(nc, "conv_w", [128, 4]), conv_b=din(nc, "conv_b", [128, 1]),
        w_a=din(nc, "w_a", [128, 128]), w_i=din(nc, "w_i", [128, 128]), b_a=din(nc, "b_a", [128, 1]), b_i=din(nc, "b_i", [128, 1]),
        lru_lam=din(nc, "lru_lam", [128, 1]),
        attn=dout(nc, "attn", [TL, 1280]), lru=dout(nc, "lru", [128, SEQ]),
    )
    phase_B(nc, io, lam_init)
    return nc


def global_order(parts, axis):
    full = np.concatenate(parts, axis=axis)
    perm = np.concatenate([own_rows(c) for c in range(NC)])
    inv = np.empty_like(perm)
    inv[perm] = np.arange(perm.size)
    return np.take(full, inv, axis=axis)


def core_masks(c):
    i = np.arange(128)
    tri = (i[:, None] <= i[None, :])
    blk = ((i[:, None] // 64) <= (i[None, :] // 64))
    mF = np.zeros((128, 8, 128), np.float32)
    mD = np.zeros((128, 8, 128), np.float32)
    for r in range(8):
        if r < c:
            mF[:, r, :] = 1.0
            mD[:, r, :] = 1.0
        elif r == c:
            mF[:, r, :] = tri
            mD[:, r, :] = blk
    om = np.zeros((6, SEQ), np.float32)
    om[:, own_rows(c)] = 1.0
    return mF.astype(ml_dtypes.bfloat16), mD.astype(ml_dtypes.bfloat16), om


def launch_B(resA, prm, l):
    lam_init = 0.8 - 0.6 * math.exp(-0.3 * l)
    nc = build_B(lam_init)
    kTf_all = global_order([r["kTf"] for r in resA], 1)
    vf_all = global_order([r["vf"] for r in resA], 0)
    kTd_all = global_order([r["kTd"] for r in resA], 1)
    vd_all = global_order([r["vd"] for r in resA], 0)
    logfT = np.ascontiguousarray(global_order([r["logf"] for r in resA], 0).T)
    xr_all = global_order([r["xrT"] for r in resA], 1)
    gr_all = global_order([r["grT"] for r in resA], 1)
    in_maps = []
    for c in range(NC):
        mF, mD, om = core_masks(c)
        n = c % 6
        sl = slice(n * 128, (n + 1) * 128)
        xr = np.zeros((128, SEQ + 3), np.float32)
        xr[:, 3:] = xr_all[sl]
        in_maps.append(dict(
            qTf=resA[c]["qTf"], kTf_all=kTf_all, vf_all=vf_all, logfT=logfT, ownmask=om, maskF=mF, maskD=mD,
            qTd=resA[c]["qTd"], kTd_all=kTd_all, vd_all=vd_all,
            lam_q1=prm["lam_q1"][l], lam_k1=prm["lam_k1"][l], lam_q2=prm["lam_q2"][l], lam_k2=prm["lam_k2"][l],
            subln_g=prm["subln_g"][l], xr=xr, gr=np.ascontiguousarray(gr_all[sl]),
            conv_w=np.ascontiguousarray(prm["conv_w"][l][:, sl].T), conv_b=np.ascontiguousarray(prm["conv_b"][l][sl, None]),
            w_a=np.ascontiguousarray(prm["w_a"][l][n]), w_i=np.ascontiguousarray(prm["w_i"][l][n]),
            b_a=np.ascontiguousarray(prm["b_a"][l][sl, None]), b_i=np.ascontiguousarray(prm["b_i"][l][sl, None]),
            lru_lam=np.ascontiguousarray(prm["lru_lambda"][l][sl, None])))
    return run(nc, in_maps)


def layer_norm_tiles(P, acc, accr, gt, bt, epst, junk_ap, sm):
    for j in range(8):
        a = acc.t[:, j, :]
        ar = accr[j]
        mu, ss = sm.next(), sm.next()
        P.emit("dve", lambda e, a=a, mu=mu: e.reduce_sum(out=mu.t[:], in_=a, axis=AX.X), [ar], [mu.r])
        P.emit("dve", lambda e, mu=mu: e.tensor_scalar_mul(out=mu.t[:], in0=mu.t[:], scalar1=-1.0 / D), [mu.r], [mu.r])
        P.emit("dve", lambda e, a=a, mu=mu: e.tensor_scalar_add(out=a, in0=a, scalar1=mu.t[:, 0:1]), [ar, mu.r], [ar])
        P.emit("act", lambda e, a=a, ss=ss: e.activation(out=junk_ap[0], in_=a, func=AF.Square, accum_out=ss.t[:, 0:1]), [ar], [junk_ap[1], ss.r])
        P.emit("act", lambda e, ss=ss: e.activation(out=ss.t[:], in_=ss.t[:], func=AF.Sqrt, scale=1.0 / D, bias=epst.t[:, 0:1]), [ss.r, epst.r], [ss.r])
        P.emit("dve", lambda e, ss=ss: e.reciprocal(out=ss.t[:], in_=ss.t[:]), [ss.r], [ss.r])
        P.emit("dve", lambda e, a=a, ss=ss: e.scalar_tensor_tensor(out=a, in0=a, scalar=ss.t[:, 0:1], in1=gt.t[:], op0=ALU.mult, op1=ALU.mult),
               [ar, ss.r, gt.r], [ar])
        P.emit("dve", lambda e, a=a: e.tensor_tensor(out=a, in0=a, in1=bt.t[:], op=ALU.add), [ar, bt.r], [ar])


def phase_C(nc, io):
    with ExitStack() as st:
        P = Prog(nc, st)
        ident = P.tile([128, 128], F32, "ident")
        P.dma("sp", ident.t[:], io["ident"], writes=[ident.r])
        epst = P.tile([128, 1], F32, "eps")
        P.emit("dve", lambda e: e.memset(epst.t[:], LN_EPS), [], [epst.r])
        acc = P.tile([128, 8, D], F32, "acc")
        accr = [Res() for _ in range(8)]
        xT = P.tile([128, 16, TL], BF16, "xT")
        lng = P.tile([128, D], F32, "lng"); lnb = P.tile([128, D], F32, "lnb")
        hid = P.tile([128, 4, TL], BF16, "hid")
        junk_ap = (hid.t[:].rearrange("p a b -> p (a b)")[:, 0:D], hid.r)
        wgs = Ring([P.tile([128, 16, 256], BF16, "wg") for _ in range(2)])
        wus = Ring([P.tile([128, 16, 256], BF16, "wu") for _ in range(2)])
        wds = Ring([P.tile([128, 4, D], BF16, "wd") for _ in range(2)])
        psr = Ring([P.psum([128, 512], F32, "ps") for _ in range(7)])
        sm = Ring([P.tile([128, 1], F32, "sm") for _ in range(12)])
        gate = P.tile([128, 8, 32], F32, "gate")
        wgr = P.tile([128, 16, 36], F32, "wgr")
        bgr = P.tile([128, 36], F32, "bgr")
        sgs = Ring([P.tile([128, 512], F32, "sg") for _ in range(2)])
        P.dma("sp", wgr.t[:], io["w_gr"].rearrange("(kc p) n -> p kc n", p=128), writes=[wgr.r])
        P.dma("sp", bgr.t[:], io["b_gr"].partition_broadcast(128), writes=[bgr.r])
        P.dma("sp", lng.t[:], io["ln1_g"].partition_broadcast(128), writes=[lng.r])
        P.dma("sp", lnb.t[:], io["ln1_b"].partition_broadcast(128), writes=[lnb.r])

        att = Ring([P.tile([128, 512], F32, "att") for _ in range(2)])
        for j in range(8):
            P.dma("sp", acc.t[:, j, :], io["hres"][j * 128:(j + 1) * 128, :], writes=[accr[j]])
            for (c0, n, d0) in ((0, 4, 0), (512, 2, 4), (768, 4, 12)):
                at = att.next()
                P.dma("sp", at.t[:, 0:n * 128], io["attn"][j * 128:(j + 1) * 128, c0:c0 + n * 128], writes=[at.r])
                ps = psr.next()
                for q in range(n):
                    P.emit("pe", lambda e, ps=ps, at=at, q=q: e.transpose(ps.t[:, q * 128:(q + 1) * 128], at.t[:, q * 128:(q + 1) * 128], ident.t[:]),
                           [at.r, ident.r], [ps.r])
                copy_op(P, evac_eng(), xT.t[:, d0:d0 + n, j * 128:(j + 1) * 128], ps.t[:, 0:n * 128].rearrange("p (a b) -> p a b", a=n), [ps.r], [xT.r])
        for b in range(6):
            P.dma("pool", xT.t[:, 6 + b, :], io["lruT"][b * 128:(b + 1) * 128, :], writes=[xT.r])

        wos = wgs.tiles + wus.tiles
        wi_ = 0
        for n in range(8):
            wt = wos[wi_ % len(wos)]; wi_ += 1
            P.dma("pool", wt.t[:], io["w_o"][:, n * 256:(n + 1) * 256].rearrange("(kc p) n -> p kc n", p=128), writes=[wt.r])
            for j in range(8):
                ps = psr.next()
                for kc in range(16):
                    P.emit("pe", lambda e, ps=ps, wt=wt, kc=kc, j=j: e.matmul(ps.t[:, 0:256], lhsT=xT.t[:, kc, j * 128:(j + 1) * 128], rhs=wt.t[:, kc, :],
                                                                            start=(kc == 0), stop=(kc == 15)), [xT.r, wt.r], [ps.r])
                a = acc.t[:, j, n * 256:(n + 1) * 256]
                P.emit("dve", lambda e, a=a, ps=ps: e.scalar_tensor_tensor(out=a, in0=a, scalar=float(ALPHA), in1=ps.t[:, 0:256], op0=ALU.mult, op1=ALU.add),
                       [accr[j], ps.r], [accr[j]])
        if CSTOP >= 2:
            layer_norm_tiles(P, acc, accr, lng, lnb, epst, junk_ap, sm)

        h1f = lng
        h1f_v = h1f.t[:].rearrange("p (a b) -> p a b", a=16)
        hlo = P.tile([128, 16, 128], BF16, "hlo"); whi = P.tile([128, 16, 36], BF16, "whi"); wlo = P.tile([128, 16, 36], BF16, "wlo")
        P.emit("dve", lambda e: e.tensor_copy(out=whi.t[:], in_=wgr.t[:]), [wgr.r], [whi.r])
        P.emit("dve", lambda e: e.tensor_tensor(out=wlo.t[:], in0=wgr.t[:], in1=whi.t[:], op=ALU.subtract), [wgr.r, whi.r], [wlo.r])
        lg = P.tile([128, 36], F32, "lg"); goh = P.tile([128, 4], F32, "goh"); ge = P.tile([128, 4], F32, "ge")
        esel = P.tile([128, 8], F32, "esel"); oh1 = P.tile([128, 8], F32, "oh1"); oh2 = P.tile([128, 8], F32, "oh2")
        msk = P.tile([128, 8], F32, "msk"); eg = P.tile([128, 8], F32, "eg")
        for j in range(8 if CSTOP >= 3.1 else 0):
            for g in range(4):
                ps = psr.next()
                for q in range(4):
                    kc = g * 4 + q
                    P.emit("pe", lambda e, ps=ps, q=q, kc=kc, j=j: e.transpose(ps.t[:, q * 128:(q + 1) * 128], acc.t[:, j, kc * 128:(kc + 1) * 128], ident.t[:]),
                           [accr[j], ident.r], [ps.r])
                P.emit("act", lambda e, ps=ps, g=g, j=j: e.activation(out=xT.t[:, g * 4:(g + 1) * 4, j * 128:(j + 1) * 128],
                                                                    in_=ps.t[:].rearrange("p (a b) -> p a b", a=4), func=AF.Copy), [ps.r], [xT.r])
                ps2 = psr.next()
                for q in range(4):
                    kc = g * 4 + q
                    P.emit("pe", lambda e, ps2=ps2, q=q, kc=kc, j=j: e.transpose(ps2.t[:, q * 128:(q + 1) * 128], acc.t[:, j, kc * 128:(kc + 1) * 128], ident.t[:]),
                           [accr[j], ident.r], [ps2.r])
                P.emit("dve", lambda e, ps2=ps2, g=g: e.tensor_copy(out=h1f_v[:, g * 4:(g + 1) * 4, :], in_=ps2.t[:].rearrange("p (a b) -> p a b", a=4)),
                       [ps2.r], [h1f.r])
            if CSTOP < 3.2:
                continue
            P.emit("dve", lambda e, j=j: e.tensor_tensor(out=hlo.t[:], in0=h1f_v, in1=xT.t[:, :, j * 128:(j + 1) * 128], op=ALU.subtract), [h1f.r, xT.r], [hlo.r])
            ps = psr.next()
            for kc in range(16):
                for ti, (lt_, lr_, rt_, rr_) in enumerate(((xT.t[:, kc, j * 128:(j + 1) * 128], xT.r, whi.t[:, kc, :], whi.r),
                                                           (xT.t[:, kc, j * 128:(j + 1) * 128], xT.r, wlo.t[:, kc, :], wlo.r),
                                                           (hlo.t[:, kc, :], hlo.r, whi.t[:, kc, :], whi.r))):
                    P.emit("pe", lambda e, ps=ps, lt_=lt_, rt_=rt_, kc=kc, ti=ti: e.matmul(ps.t[:, 0:36], lhsT=lt_, rhs=rt_, start=(kc == 0 and ti == 0), stop=(kc == 15 and ti == 2)),
                           [lr_, rr_], [ps.r])
            P.emit("dve", lambda e, ps=ps: e.tensor_tensor(out=lg.t[:], in0=ps.t[:, 0:36], in1=bgr.t[:], op=ALU.add), [ps.r, bgr.r], [lg.r])
            if CSTOP < 3.3:
                continue
            gmax, ngmax, gsum, m1, m2, dd, w1, w2 = [sm.next() for _ in range(8)]
            P.emit("dve", lambda e, gmax=gmax: e.reduce_max(out=gmax.t[:], in_=lg.t[:, 0:4], axis=AX.X), [lg.r], [gmax.r])
            P.emit("dve", lambda e, gmax=gmax: e.tensor_scalar(out=goh.t[:], in0=lg.t[:, 0:4], scalar1=gmax.t[:, 0:1], scalar2=None, op0=ALU.is_equal), [lg.r, gmax.r], [goh.r])
            P.emit("dve", lambda e, gmax=gmax, ngmax=ngmax: e.tensor_scalar_mul(out=ngmax.t[:], in0=gmax.t[:], scalar1=-1.0), [gmax.r], [ngmax.r])
            P.emit("act", lambda e, ngmax=ngmax, gsum=gsum: e.activation(out=ge.t[:], in_=lg.t[:, 0:4], func=AF.Exp, bias=ngmax.t[:, 0:1], accum_out=gsum.t[:, 0:1]),
                   [lg.r, ngmax.r], [ge.r, gsum.r])
            P.emit("dve", lambda e, gsum=gsum: e.reciprocal(out=gsum.t[:], in_=gsum.t[:]), [gsum.r], [gsum.r])
            if CSTOP < 3.4:
                continue
            P.emit("dve", lambda e: e.tensor_scalar_mul(out=esel.t[:], in0=lg.t[:, 4:12], scalar1=goh.t[:, 0:1]), [lg.r, goh.r], [esel.r])
            for g in range(1, 4):
                P.emit("dve", lambda e, g=g: e.scalar_tensor_tensor(out=esel.t[:], in0=lg.t[:, 4 + 8 * g:12 + 8 * g], scalar=goh.t[:, g:g + 1], in1=esel.t[:],
                                                                    op0=ALU.mult, op1=ALU.add), [lg.r, goh.r, esel.r], [esel.r])
            P.emit("dve", lambda e, m1=m1: e.reduce_max(out=m1.t[:], in_=esel.t[:], axis=AX.X), [esel.r], [m1.r])
            P.emit("dve", lambda e, m1=m1: e.tensor_scalar(out=oh1.t[:], in0=esel.t[:], scalar1=m1.t[:, 0:1], scalar2=None, op0=ALU.is_equal), [esel.r, m1.r], [oh1.r])
            P.emit("dve", lambda e: e.scalar_tensor_tensor(out=msk.t[:], in0=oh1.t[:], scalar=-1e30, in1=esel.t[:], op0=ALU.mult, op1=ALU.add), [oh1.r, esel.r], [msk.r])
            P.emit("dve", lambda e, m2=m2: e.reduce_max(out=m2.t[:], in_=msk.t[:], axis=AX.X), [msk.r], [m2.r])
            P.emit("dve", lambda e, m2=m2: e.tensor_scalar(out=oh2.t[:], in0=msk.t[:], scalar1=m2.t[:, 0:1], scalar2=None, op0=ALU.is_equal), [msk.r, m2.r], [oh2.r])
            if CSTOP < 3.5:
                continue
            P.emit("dve", lambda e, m1=m1, m2=m2, dd=dd: e.tensor_tensor(out=dd.t[:], in0=m2.t[:], in1=m1.t[:], op=ALU.subtract), [m1.r, m2.r], [dd.r])
            P.emit("act", lambda e, dd=dd: e.activation(out=dd.t[:], in_=dd.t[:], func=AF.Exp), [dd.r], [dd.r])
            P.emit("dve", lambda e, dd=dd, w1=w1: e.tensor_scalar_add(out=w1.t[:], in0=dd.t[:], scalar1=1.0), [dd.r], [w1.r])
            P.emit("dve", lambda e, w1=w1: e.reciprocal(out=w1.t[:], in_=w1.t[:]), [w1.r], [w1.r])
            P.emit("dve", lambda e, w1=w1, w2=w2, dd=dd: e.tensor_tensor(out=w2.t[:], in0=dd.t[:], in1=w1.t[:], op=ALU.mult), [dd.r, w1.r], [w2.r])
            P.emit("dve", lambda e, w1=w1, gsum=gsum: e.tensor_tensor(out=w1.t[:], in0=w1.t[:], in1=gsum.t[:], op=ALU.mult), [w1.r, gsum.r], [w1.r])
            P.emit("dve", lambda e, w2=w2, gsum=gsum: e.tensor_tensor(out=w2.t[:], in0=w2.t[:], in1=gsum.t[:], op=ALU.mult), [w2.r, gsum.r], [w2.r])
            P.emit("dve", lambda e, w1=w1: e.tensor_scalar_mul(out=eg.t[:], in0=oh1.t[:], scalar1=w1.t[:, 0:1]), [oh1.r, w1.r], [eg.r])
            P.emit("dve", lambda e, w2=w2: e.scalar_tensor_tensor(out=eg.t[:], in0=oh2.t[:], scalar=w2.t[:, 0:1], in1=eg.t[:], op0=ALU.mult, op1=ALU.add),
                   [oh2.r, w2.r, eg.r], [eg.r])
            for g in range(4):
                P.emit("dve", lambda e, g=g, j=j: e.tensor_scalar_mul(out=gate.t[:, j, g * 8:(g + 1) * 8], in0=eg.t[:], scalar1=goh.t[:, g:g + 1]), [eg.r, goh.r], [gate.r])
        for j in range(8 if CSTOP >= 3 else 0):
            P.emit("dve", lambda e, j=j: e.tensor_scalar_mul(out=acc.t[:, j, :], in0=acc.t[:, j, :], scalar1=float(ALPHA)), [accr[j]], [accr[j]])
        if CSTOP >= 3.02:
            P.dma("sp", lng.t[:], io["ln2_g"].partition_broadcast(128), reads=[h1f.r], writes=[lng.r])
            P.dma("sp", lnb.t[:], io["ln2_b"].partition_broadcast(128), writes=[lnb.r])

        for ex in range(NEXP if CSTOP >= 4 else 0):
            wd = wds.next()
            P.dma("pool", wd.t[:], io["w_down"][ex].rearrange("(fc p) d -> p fc d", p=128), writes=[wd.r])
            for fh in range(2):
                wg, wu = wgs.next(), wus.next()
                P.dma("pool", wg.t[:], io["w_gate"][ex][:, fh * 256:(fh + 1) * 256].rearrange("(kc p) f -> p kc f", p=128), writes=[wg.r])
                P.dma("pool", wu.t[:], io["w_up"][ex][:, fh * 256:(fh + 1) * 256].rearrange("(kc p) f -> p kc f", p=128), writes=[wu.r])
                for fc in range(2):
                    for th in range(2):
                        pg, pu = psr.next(), psr.next()
                        for (wt, ps) in ((wg, pg), (wu, pu)):
                            for kc in range(16):
                                P.emit("pe", lambda e, ps=ps, wt=wt, kc=kc, fc=fc, th=th: e.matmul(
                                    ps.t[:], lhsT=wt.t[:, kc, fc * 128:(fc + 1) * 128], rhs=xT.t[:, kc, th * 512:(th + 1) * 512],
                                    start=(kc == 0), stop=(kc == 15)), [wt.r, xT.r], [ps.r])
                        sg = sgs.next()
                        P.emit("act", lambda e, pg=pg, sg=sg: e.activation(out=sg.t[:], in_=pg.t[:], func=AF.Silu), [pg.r], [sg.r])
                        P.emit("dve", lambda e, pu=pu, sg=sg, fh=fh, fc=fc, th=th: e.tensor_tensor(out=hid.t[:, fh * 2 + fc, th * 512:(th + 1) * 512], in0=sg.t[:], in1=pu.t[:], op=ALU.mult),
                               [sg.r, pu.r], [hid.r])
            for j in range(8):
                for n in range(4):
                    ps = psr.next()
                    for fc in range(4):
                        P.emit("pe", lambda e, ps=ps, fc=fc, j=j, n=n, wd=wd: e.matmul(ps.t[:], lhsT=hid.t[:, fc, j * 128:(j + 1) * 128], rhs=wd.t[:, fc, n * 512:(n + 1) * 512],
                                                                                   start=(fc == 0), stop=(fc == 3)), [hid.r, wd.r], [ps.r])
                    a = acc.t[:, j, n * 512:(n + 1) * 512]
                    P.emit("dve", lambda e, a=a, ps=ps, j=j, ex=ex: e.scalar_tensor_tensor(out=a, in0=ps.t[:], scalar=gate.t[:, j, ex:ex + 1], in1=a, op0=ALU.mult, op1=ALU.add),
                           [ps.r, gate.r, accr[j]], [accr[j]])
        if CSTOP >= 5:
            layer_norm_tiles(P, acc, accr, lng, lnb, epst, junk_ap, sm)
        for j in range(8):
            P.dma("sp", io["hout"][j * 128:(j + 1) * 128, :], acc.t[:, j, :], reads=[accr[j]])
        P.finalize()
    nc.all_engine_barrier()


def build_C():
    nc = bass.Bass("TRN2", target_bir_lowering=False)
    io = dict(
        hres=din(nc, "hres", [TL, D]), attn=din(nc, "attn", [TL, 1280]), lruT=din(nc, "lruT", [768, TL]),
        w_o=din(nc, "w_o", [D, D]), ln1_g=din(nc, "ln1_g", [D]), ln1_b=din(nc, "ln1_b", [D]),
        w_gr=din(nc, "w_gr", [D, 36]), b_gr=din(nc, "b_gr", [36]),
        w_gate=din(nc, "w_gate", [NEXP, D, 512]), w_up=din(nc, "w_up", [NEXP, D, 512]), w_down=din(nc, "w_down", [NEXP, 512, D]),
        ln2_g=din(nc, "ln2_g", [D]), ln2_b=din(nc, "ln2_b", [D]), ident=din(nc, "ident", [128, 128]),
        hout=dout(nc, "hout", [TL, D]),
    )
    phase_C(nc, io)
    return nc


def launch_C(h_shards, resB, prm, l):
    nc = build_C()
    lru_all = np.concatenate([resB[n]["lru"] for n in range(6)], axis=0)
    w_gr = np.ascontiguousarray(np.concatenate([prm["w_group"][l], prm["w_router"][l]], axis=1))
    b_gr = np.ascontiguousarray(np.concatenate([prm["b_group"][l], prm["b_router"][l]], axis=0))
    wg = prm["w_gate"][l].reshape(32, D, 512)[:NEXP]
    wu = prm["w_up"][l].reshape(32, D, 512)[:NEXP]
    wdn = prm["w_down"][l].reshape(32, 512, D)[:NEXP]
    in_maps = []
    for c in range(NC):
        in_maps.append(dict(
            hres=h_shards[c], attn=resB[c]["attn"], lruT=np.ascontiguousarray(lru_all[:, own_rows(c)]),
            w_o=prm["w_o"][l], ln1_g=prm["ln1_g"][l], ln1_b=prm["ln1_b"][l], w_gr=w_gr, b_gr=b_gr,
            w_gate=wg, w_up=wu, w_down=wdn, ln2_g=prm["ln2_g"][l], ln2_b=prm["ln2_b"][l], ident=IDENT))
    return run(nc, in_maps)


def build_CA():
    nc = bass.Bass("TRN2", target_bir_lowering=False)
    ioC = dict(
        hres=din(nc, "hres", [TL, D]), attn=din(nc, "attn", [TL, 1280]), lruT=din(nc, "lruT", [768, TL]),
        w_o=din(nc, "w_o", [D, D]), ln1_g=din(nc, "ln1_g", [D]), ln1_b=din(nc, "ln1_b", [D]),
        w_gr=din(nc, "w_gr", [D, 36]), b_gr=din(nc, "b_gr", [36]),
        w_gate=din(nc, "w_gate", [NEXP, D, 512]), w_up=din(nc, "w_up", [NEXP, D, 512]), w_down=din(nc, "w_down", [NEXP, 512, D]),
        ln2_g=din(nc, "ln2_g", [D]), ln2_b=din(nc, "ln2_b", [D]), ident=din(nc, "ident", [128, 128]),
        hout=dout(nc, "hout", [TL, D]),
    )
    phase_C(nc, ioC)
    ioA = dict(
        hin=ioC["hout"], w_in=din(nc, "w_in", [D, N_IN]), b_f=din(nc, "b_f", [6]),
        pos=din(nc, "pos", [128, 8], I32), invf=din(nc, "invf", [128, 8]), ident=ioC["ident"],
        qTf=dout(nc, "qTf", [768, TL], BF16), kTf=dout(nc, "kTf", [768, TL], BF16), vf=dout(nc, "vf", [TL, 768], BF16),
        logf=dout(nc, "logf", [TL, 6]), xrT=dout(nc, "xrT", [768, TL]), grT=dout(nc, "grT", [768, TL]),
        qTd=dout(nc, "qTd", [512, TL], BF16), kTd=dout(nc, "kTd", [512, TL], BF16), vd=dout(nc, "vd", [TL, 512], BF16),
    )
    phase_A(nc, ioA)
    return nc


def launch_CA(h_shards, resB, prm, l, positions):
    nc = build_CA()
    lru_all = np.concatenate([resB[n]["lru"] for n in range(6)], axis=0)
    w_gr = np.ascontiguousarray(np.concatenate([prm["w_group"][l], prm["w_router"][l]], axis=1))
    b_gr = np.ascontiguousarray(np.concatenate([prm["b_group"][l], prm["b_router"][l]], axis=0))
    wg = prm["w_gate"][l].reshape(32, D, 512)[:NEXP]
    wu = prm["w_up"][l].reshape(32, D, 512)[:NEXP]
    wdn = prm["w_down"][l].reshape(32, 512, D)[:NEXP]
    in_maps = []
    for c in range(NC):
        in_maps.append(dict(
            hres=h_shards[c], attn=resB[c]["attn"], lruT=np.ascontiguousarray(lru_all[:, own_rows(c)]),
            w_o=prm["w_o"][l], ln1_g=prm["ln1_g"][l], ln1_b=prm["ln1_b"][l], w_gr=w_gr, b_gr=b_gr,
            w_gate=wg, w_up=wu, w_down=wdn, ln2_g=prm["ln2_g"][l], ln2_b=prm["ln2_b"][l], ident=IDENT,
            w_in=prm["w_in"][l + 1], b_f=prm["b_f"][l + 1],
            pos=np.ascontiguousarray(positions[own_rows(c)].reshape(8, 128).T), invf=INVF))
    return run(nc, in_maps)


def kernel(**inputs):
    prm = {k: np.asarray(v) for k, v in inputs.items()}
    x = prm["x"][0]
    pos = prm["positions"][0]
    h_shards = [np.ascontiguousarray(x[own_rows(c)]) for c in range(NC)]
    resA = launch_A(h_shards, prm["w_in"][0], prm["b_f"][0], pos)
    resB = launch_B(resA, prm, 0)
    resCA = launch_CA(h_shards, resB, prm, 0, pos)
    h_shards = [np.ascontiguousarray(r["hout"]) for r in resCA]
    resB = launch_B(resCA, prm, 1)
    resC = launch_C(h_shards, resB, prm, 1)
    h_shards = [np.ascontiguousarray(r["hout"]) for r in resC]
    out = np.empty((1, SEQ, D), np.float32)
    for c in range(NC):
        out[0, own_rows(c)] = h_shards[c]
    return out
```
